# Optimizing a Trainium2 kernel written in Bass

```python
import jax, jax.numpy as jnp
from jax import lax
import numpy as np

D_MODEL = 2048
BATCH = 8
SEQ = 2048
DEPTH = 1

CHUNK = 64
Q_BLOCK = 128
D_PLE = 256
EPS = 1e-6

POOL_WIDTH = D_MODEL // 2
POOL_WINDOWS = (2, 4, 8, 16)
POOL_GROUPS = len(POOL_WINDOWS)
POOL_GROUP_WIDTH = POOL_WIDTH // POOL_GROUPS

V_HEAD = 128
MLA_HEADS = (D_MODEL // 2) // V_HEAD
MLA_WIDTH = MLA_HEADS * V_HEAD
QK_NOPE = 128
QK_ROPE = 64
QK_HEAD = QK_NOPE + QK_ROPE
Q_LORA = 512
KV_LORA = 512
ROPE_THETA = 10000.0

MIX_WIDTH = POOL_WIDTH + MLA_WIDTH
IN_COLS = POOL_WIDTH + Q_LORA + KV_LORA + QK_ROPE

PEER_HEADS = 8
PEER_NKEYS = 128
PEER_EXPERTS = PEER_NKEYS * PEER_NKEYS
PEER_DKEY = 256
PEER_HALF = PEER_DKEY // 2
PEER_TOPK = 16
PEER_TOK_BLOCK = 128

kernel_name = "hybrid_pool_mla_peer_ple"


def rms_norm(x, g):
    xf = x.astype(jnp.float32)
    y = xf * lax.rsqrt(jnp.mean(xf * xf, axis=-1, keepdims=True) + EPS)
    return (y * g.astype(jnp.float32)).astype(x.dtype)


def apply_rope(x, cos, sin):
    half = QK_ROPE // 2
    xf = x.astype(jnp.float32)
    x1, x2 = xf[..., :half], xf[..., half:]
    return jnp.concatenate([x1 * cos - x2 * sin, x2 * cos + x1 * sin], axis=-1).astype(x.dtype)


def pool_mixer(xp, w_pool, pool_scale):
    B, S, _ = xp.shape
    xf = xp.reshape(B, S, POOL_GROUPS, POOL_GROUP_WIDTH).astype(jnp.float32)
    cs = jnp.cumsum(xf, axis=1)
    t = jnp.arange(S)
    outs = []
    for g, w in enumerate(POOL_WINDOWS):
        csg = cs[:, :, g]
        lower = jnp.pad(csg, ((0, 0), (w, 0), (0, 0)))[:, :S]
        cnt = jnp.minimum(t + 1, w).astype(jnp.float32)[None, :, None]
        outs.append((csg - lower) / cnt - xf[:, :, g])
    d = jnp.stack(outs, axis=2).astype(xp.dtype)
    y = jnp.einsum('bsgc,gcd->bsgd', d, w_pool)
    return y.reshape(B, S, POOL_WIDTH) * pool_scale


def mla(c_q, c_kv, k_rope, positions, q_lat_gain, kv_lat_gain, w_uq, w_ukv, q_norm_gain, k_norm_gain):
    B, S, _ = c_q.shape
    q = (rms_norm(c_q, q_lat_gain) @ w_uq).reshape(B, S, MLA_HEADS, QK_HEAD)
    kv = (rms_norm(c_kv, kv_lat_gain) @ w_ukv).reshape(B, S, MLA_HEADS, QK_NOPE + V_HEAD)
    k_nope, v = kv[..., :QK_NOPE], kv[..., QK_NOPE:]
    k = jnp.concatenate([k_nope, jnp.broadcast_to(k_rope[:, :, None, :], (B, S, MLA_HEADS, QK_ROPE))], axis=-1)
    q = rms_norm(q, q_norm_gain)
    k = rms_norm(k, k_norm_gain)
    inv_freq = ROPE_THETA ** (-jnp.arange(0, QK_ROPE, 2, dtype=jnp.float32) / QK_ROPE)
    ang = positions.astype(jnp.float32)[:, :, None] * inv_freq
    cos, sin = jnp.cos(ang)[:, :, None, :], jnp.sin(ang)[:, :, None, :]
    q = jnp.concatenate([q[..., :QK_NOPE], apply_rope(q[..., QK_NOPE:], cos, sin)], axis=-1)
    k = jnp.concatenate([k[..., :QK_NOPE], apply_rope(k[..., QK_NOPE:], cos, sin)], axis=-1)

    nb = S // Q_BLOCK
    qb = q.reshape(B, nb, Q_BLOCK, MLA_HEADS, QK_HEAD).transpose(1, 0, 2, 3, 4)
    k_chunk = jnp.arange(S) // CHUNK
    scale = QK_HEAD ** -0.5

    def attend(args):
        q_blk, blk = args
        q_chunk = (blk * Q_BLOCK + jnp.arange(Q_BLOCK)) // CHUNK
        s = jnp.einsum('bqhd,bkhd->bhqk', q_blk, k, preferred_element_type=jnp.float32) * scale
        s = jnp.where(k_chunk[None, :] <= q_chunk[:, None], s, -jnp.inf)
        pr = jax.nn.softmax(s, axis=-1).astype(v.dtype)
        return jnp.einsum('bhqk,bkhd->bqhd', pr, v)

    o = lax.map(attend, (qb, jnp.arange(nb)))
    return o.transpose(1, 0, 2, 3, 4).reshape(B, S, MLA_WIDTH)


def peer(xn, w_pq, sub_k1, sub_k2, expert_u, expert_v):
    B, S, D = xn.shape
    q = (xn @ w_pq).reshape(B, S, PEER_HEADS, 2, PEER_HALF)
    s1 = jnp.einsum('bshd,hnd->bshn', q[..., 0, :], sub_k1, preferred_element_type=jnp.float32)
    s2 = jnp.einsum('bshd,hnd->bshn', q[..., 1, :], sub_k2, preferred_element_type=jnp.float32)
    v1, i1 = lax.top_k(s1, PEER_TOPK)
    v2, i2 = lax.top_k(s2, PEER_TOPK)
    cand = (v1[..., :, None] + v2[..., None, :]).reshape(B, S, PEER_HEADS, PEER_TOPK * PEER_TOPK)
    vs, ci = lax.top_k(cand, PEER_TOPK)
    e1 = jnp.take_along_axis(i1, ci // PEER_TOPK, axis=-1)
    e2 = jnp.take_along_axis(i2, ci % PEER_TOPK, axis=-1)
    eidx = e1 * PEER_NKEYS + e2
    gates = jax.nn.softmax(vs, axis=-1).astype(xn.dtype)

    T = B * S
    nblk = T // PEER_TOK_BLOCK
    kk = PEER_HEADS * PEER_TOPK
    xt = xn.reshape(nblk, PEER_TOK_BLOCK, D)
    it = eidx.reshape(nblk, PEER_TOK_BLOCK, kk)
    gt = gates.reshape(nblk, PEER_TOK_BLOCK, kk)

    def block(args):
        xb, ib, gb = args
        act = jax.nn.gelu(jnp.einsum('tkd,td->tk', expert_u[ib], xb))
        return jnp.einsum('tk,tkd->td', act * gb, expert_v[ib])

    return lax.map(block, (xt, it, gt)).reshape(B, S, D)


def setup_inputs(seed: int = 0) -> dict:
    key = jax.random.key(seed)
    ks = jax.random.split(key, 24)
    f32 = jnp.float32

    def nrm(k, shape, scale):
        return jax.random.normal(k, shape, f32) * scale

    def gain(k, n):
        return 1.0 + 0.02 * jax.random.normal(k, (DEPTH, n), f32)

    x = jax.random.normal(ks[0], (BATCH, SEQ, D_MODEL), f32)
    p = jax.random.normal(ks[1], (DEPTH, BATCH, SEQ, D_PLE), f32)
    offs = jax.random.randint(ks[2], (BATCH, 1), 0, 4096, dtype=jnp.int32)
    positions = offs + jnp.arange(SEQ, dtype=jnp.int32)[None, :]
    return {
        "x": x,
        "p": p,
        "positions": positions,
        "mix_norm_gain": gain(ks[3], D_MODEL),
        "w_in": nrm(ks[4], (DEPTH, D_MODEL, IN_COLS), D_MODEL ** -0.5),
        "w_pool": nrm(ks[5], (DEPTH, POOL_GROUPS, POOL_GROUP_WIDTH, POOL_GROUP_WIDTH), POOL_GROUP_WIDTH ** -0.5),
        "pool_scale": 1.0 + 0.1 * jax.random.normal(ks[6], (DEPTH, POOL_WIDTH), f32),
        "q_lat_gain": gain(ks[7], Q_LORA),
        "kv_lat_gain": gain(ks[8], KV_LORA),
        "w_uq": nrm(ks[9], (DEPTH, Q_LORA, MLA_HEADS * QK_HEAD), Q_LORA ** -0.5),
        "w_ukv": nrm(ks[10], (DEPTH, KV_LORA, MLA_HEADS * (QK_NOPE + V_HEAD)), KV_LORA ** -0.5),
        "q_norm_gain": gain(ks[11], QK_HEAD),
        "k_norm_gain": gain(ks[12], QK_HEAD),
        "w_out": nrm(ks[13], (DEPTH, MIX_WIDTH, D_MODEL), MIX_WIDTH ** -0.5),
        "ffn_norm_gain": gain(ks[14], D_MODEL),
        "w_pq": nrm(ks[15], (DEPTH, D_MODEL, PEER_HEADS * PEER_DKEY), D_MODEL ** -0.5),
        "sub_k1": nrm(ks[16], (DEPTH, PEER_HEADS, PEER_NKEYS, PEER_HALF), PEER_HALF ** -0.5),
        "sub_k2": nrm(ks[17], (DEPTH, PEER_HEADS, PEER_NKEYS, PEER_HALF), PEER_HALF ** -0.5),
        "expert_u": nrm(ks[18], (DEPTH, PEER_EXPERTS, D_MODEL), D_MODEL ** -0.5),
        "expert_v": nrm(ks[19], (DEPTH, PEER_EXPERTS, D_MODEL), PEER_HEADS ** -0.5),
        "ple_norm_gain": gain(ks[20], D_MODEL),
        "w_ple_gate": nrm(ks[21], (DEPTH, D_MODEL, D_MODEL), D_MODEL ** -0.5),
        "w_ple_proj": nrm(ks[22], (DEPTH, D_PLE, D_MODEL), D_PLE ** -0.5),
    }


def reference(x, p, positions, mix_norm_gain, w_in, w_pool, pool_scale, q_lat_gain, kv_lat_gain,
              w_uq, w_ukv, q_norm_gain, k_norm_gain, w_out, ffn_norm_gain, w_pq, sub_k1, sub_k2,
              expert_u, expert_v, ple_norm_gain, w_ple_gate, w_ple_proj):
    h = x
    c0 = POOL_WIDTH
    c1 = c0 + Q_LORA
    c2 = c1 + KV_LORA
    for i in range(DEPTH):
        z = rms_norm(h, mix_norm_gain[i]) @ w_in[i]
        y_pool = pool_mixer(z[..., :c0], w_pool[i], pool_scale[i])
        y_mla = mla(z[..., c0:c1], z[..., c1:c2], z[..., c2:], positions,
                    q_lat_gain[i], kv_lat_gain[i], w_uq[i], w_ukv[i], q_norm_gain[i], k_norm_gain[i])
        h = h + jnp.concatenate([y_pool, y_mla], axis=-1) @ w_out[i]
        h = h + peer(rms_norm(h, ffn_norm_gain[i]), w_pq[i], sub_k1[i], sub_k2[i], expert_u[i], expert_v[i])
        gate = jax.nn.sigmoid(rms_norm(h, ple_norm_gain[i]) @ w_ple_gate[i])
        h = h + gate * (p[i] @ w_ple_proj[i])
    return h
```

```python
import math
from contextlib import ExitStack

import numpy as np
import concourse.bass as bass
import concourse.mybir as mybir
from concourse.bass_utils import run_bass_kernel_spmd

F32 = mybir.dt.float32
BF16 = mybir.dt.bfloat16
I32 = mybir.dt.int32
U32 = mybir.dt.uint32
AF = mybir.ActivationFunctionType
ALU = mybir.AluOpType
AX = mybir.AxisListType

T = 2048
D = 2048
EPS = 1e-6
TG = 256
NG = T // TG
NCST = 88
DBG_CUT = 99
TWO_PI = 2.0 * math.pi
CW1 = 6.28125
_c2 = np.array([TWO_PI - CW1], np.float32).view(np.uint32) & np.uint32(0xFFFFF000)
CW2 = float(_c2.view(np.float32)[0])
CW3 = float(TWO_PI - CW1 - CW2)


class Res:
    __slots__ = ("name", "w", "rd")

    def __init__(self, name):
        self.name = name
        self.w = None
        self.rd = []


class Op:
    __slots__ = ("eng", "fn", "deps", "signal", "sigval", "isdma", "sem", "semval", "prev")


class Prog:
    ENG = {"pe": "tensor", "act": "scalar", "dve": "vector", "pool": "gpsimd", "sp": "sync"}

    def __init__(self, nc, st, ring=8):
        self.nc = nc
        self.sems = {e: st.enter_context(nc.semaphore("s_" + e)) for e in self.ENG}
        self.cnt = {e: 0 for e in self.ENG}
        self.ring = ring
        self.rings = {q: [st.enter_context(nc.semaphore("d_%s%d" % (q, i))) for i in range(ring)]
                      for q in ("sp", "pool", "act")}
        self.ringcnt = {q: [0] * ring for q in self.rings}
        self.ringpos = {q: 0 for q in self.rings}
        self.ops = []
        self.waited = {}
        self.allres = []

    def res(self, name):
        r = Res(name)
        self.allres.append(r)
        return r

    def _add(self, eng, fn, reads, writes, isdma, indep):
        o = Op()
        o.eng = eng
        o.fn = fn
        o.isdma = isdma
        o.signal = False
        o.sigval = None
        deps = []
        for r in reads:
            if r.w is not None:
                deps.append(r.w)
        for w in writes:
            if w.w is not None:
                deps.append(w.w)
            deps.extend(w.rd)
        seen = set()
        out = []
        for d in deps:
            if id(d) in seen:
                continue
            seen.add(id(d))
            if (not d.isdma) and (not isdma) and d.eng == eng and (eng == "pe" or indep):
                continue
            out.append(d)
            if not d.isdma:
                d.signal = True
        o.deps = out
        if isdma:
            q = eng
            slot = self.ringpos[q] % self.ring
            self.ringpos[q] += 1
            o.sem = self.rings[q][slot]
            o.prev = self.ringcnt[q][slot]
            self.ringcnt[q][slot] += 16
            o.semval = self.ringcnt[q][slot]
        for w in writes:
            w.w = o
            w.rd = []
        for r in reads:
            if r in writes:
                continue
            if not isdma:
                r.rd = [x for x in r.rd if x.isdma or x.eng != eng]
            r.rd.append(o)
        self.ops.append(o)
        return o

    _defer = None

    def defer_begin(self):
        self._defer = []

    def defer_end(self):
        d = self._defer
        self._defer = None
        return d

    def op(self, eng, fn, reads=(), writes=(), indep=False):
        if self._defer is not None:
            self._defer.append(lambda: self._add(eng, fn, list(reads), list(writes), False, indep))
            return None
        return self._add(eng, fn, list(reads), list(writes), False, indep)

    def dma(self, q, fn, reads=(), writes=()):
        if self._defer is not None:
            self._defer.append(lambda: self._add(q, fn, list(reads), list(writes), True, False))
            return None
        return self._add(q, fn, list(reads), list(writes), True, False)

    def _wait(self, engobj, e, sem, key, val):
        if val <= 0:
            return
        k = (e, key)
        if self.waited.get(k, 0) >= val:
            return
        self.waited[k] = val
        engobj.wait_ge(sem, val)

    def emit(self):
        ops = self.ops
        self.ops = []
        for o in ops:
            if (not o.isdma) and o.signal:
                self.cnt[o.eng] += 1
                o.sigval = self.cnt[o.eng]
        with self.nc.Block() as block:
            for e, attr in self.ENG.items():
                mine = [o for o in ops if o.eng == e]
                if not mine:
                    continue

                def body(engobj, mine=mine, e=e):
                    for o in mine:
                        for d in o.deps:
                            if d.isdma:
                                self._wait(engobj, e, d.sem, id(d.sem), d.semval)
                            else:
                                self._wait(engobj, e, self.sems[d.eng], d.eng, d.sigval)
                        if o.isdma:
                            self._wait(engobj, e, o.sem, id(o.sem), o.prev)
                            o.fn(engobj).then_inc(o.sem, 16)
                        else:
                            ins = o.fn(engobj)
                            if o.signal:
                                ins.then_inc(self.sems[e], 1)
                    if e in self.rings:
                        for i, s in enumerate(self.rings[e]):
                            self._wait(engobj, e, s, id(s), self.ringcnt[e][i])

                getattr(block, attr)(body)
        for r in self.allres:
            r.w = None
            r.rd = []


def build(stop_after=None, dbg=()):
    nc = bass.Bass("TRN2", target_bir_lowering=False)

    def din(name, shape, dt=F32):
        return nc.dram_tensor(name, list(shape), dt, kind="ExternalInput").ap()

    def dscr(name, shape, dt):
        kind = "ExternalOutput" if name in dbg else "Internal"
        return nc.dram_tensor(name, list(shape), dt, kind=kind).ap()

    x = din("x", [T, D])
    pin = din("p", [T, 256])
    pos = din("pos", [1, T], I32)
    cst = din("cst", [128, NCST])
    mats = din("mats", [128, 6 * 128])
    maskc = din("maskc", [128, 2 * TG])
    Win = din("Win", [17, 128, 2048])
    Wpool = din("Wpool", [128, 2048])
    Wuq = din("Wuq", [128, 4 * 1536])
    Wuk = din("Wuk", [128, 4096])
    Wv = din("Wv", [128, 4096])
    Wout = din("Wout", [4, 128, 8192])
    Wpq = din("Wpq", [16, 128, 2048])
    subk = din("subk", [128, 2048])
    UT = din("UT", [128, 128, 2048])
    EV = din("EV", [256, 128, 1024])
    Wg = din("Wg", [4, 128, 8192])
    Wpp = din("Wpp", [128, 4096])
    out = nc.dram_tensor("out", [T, D], F32, kind="ExternalOutput").ap()

    Winb = dscr("Winb", [17, 128, 2048], BF16)
    Woutb = dscr("Woutb", [4, 128, 8192], BF16)
    Wpqb = dscr("Wpqb", [16, 128, 2048], BF16)
    Wgb = dscr("Wgb", [4, 128, 8192], BF16)
    UTb = dscr("UTb", [128, 128, 2048], BF16)
    EVb = dscr("EVb", [256, 128, 1024], BF16)
    CQ = dscr("CQ", [NG, 128, 4 * TG], BF16)
    YP = dscr("YP", [NG, 128, 8 * TG], BF16)
    H1 = dscr("H1", [T, D], F32)
    H2 = dscr("H2", [T, D], F32)
    XN2 = dscr("XN2", [NG, 128, 16 * TG], BF16)
    IRs = dscr("IRs", [3, 128, T], F32)

    st0 = ExitStack()
    with st0:
        P = Prog(nc, st0)

        def sbuf(st, name, shape, dt):
            return st.enter_context(nc.sbuf_tensor(name, list(shape), dt))

        def psum(st, name, shape, dt=F32):
            return st.enter_context(nc.psum_tensor(name, list(shape), dt))

        cst_sb = sbuf(st0, "cst_sb", [128, NCST], F32)
        mats_sb = sbuf(st0, "mats_sb", [128, 6 * 128], F32)
        ident_bf = sbuf(st0, "ident_bf", [128, 128], BF16)
        ones_bf = sbuf(st0, "ones_bf", [128, 128], BF16)
        iota_bf = sbuf(st0, "iota_bf", [128, 128], BF16)
        stKV = ExitStack()
        cosT = sbuf(stKV, "cosT", [128, T], F32)
        sinT = sbuf(stKV, "sinT", [128, T], F32)
        R_const = P.res("const")
        R_rope = P.res("rope")
        ident_f = mats_sb[:, 0:128]
        ones_f = mats_sb[:, 128:256]
        Llo_f = mats_sb[:, 256:384]
        Lhi_f = mats_sb[:, 384:512]
        perm_f = mats_sb[:, 512:640]
        iota_f = mats_sb[:, 640:768]

        RWin = [P.res("Win%d" % c) for c in range(17)]
        RWout = [P.res("Wout%d" % c) for c in range(4)]
        RWpq = [P.res("Wpq%d" % c) for c in range(16)]
        RWg = [P.res("Wg%d" % c) for c in range(4)]
        pro_list = []
        for c in range(17):
            pro_list.append((Winb[c], Win[c], RWin[c]))
        for nb in range(4):
            for q in range(4):
                pro_list.append((Woutb[nb, 32 * q:32 * q + 32, :], Wout[nb, 32 * q:32 * q + 32, :], RWout[nb]))
        for c in range(16):
            pro_list.append((Wpqb[c], Wpq[c], RWpq[c]))
        for nb in range(4):
            for q in range(4):
                pro_list.append((Wgb[nb, 32 * q:32 * q + 32, :], Wg[nb, 32 * q:32 * q + 32, :], RWg[nb]))
        for i in range(128):
            pro_list.append((UTb[i], UT[i], None))
            pro_list.append((EVb[2 * i:2 * i + 2], EV[2 * i:2 * i + 2], None))
        pro_state = {"i": 0}

        def pro_some(n):
            for _ in range(n):
                if pro_state["i"] >= len(pro_list):
                    return
                o_, i_, r_ = pro_list[pro_state["i"]]
                pro_state["i"] += 1
                P.dma("pool", lambda e, o_=o_, i_=i_: e.dma_start(out=o_, in_=i_, max_dma_last_dim=4096),
                      writes=([r_] if r_ is not None else []))

        with ExitStack() as st:
            posi = sbuf(st, "posi", [128, T], I32)
            ang = sbuf(st, "ang", [128, T], F32)
            kf = sbuf(st, "kf", [128, T], F32)
            ki = sbuf(st, "ki", [128, T], I32)
            rr = sbuf(st, "rr", [128, T], F32)
            r2 = sbuf(st, "r2", [128, T], F32)
            Rp, Ra, Rk, Rki, Rr, Rr2 = [P.res(n) for n in ("posi", "ang", "kf", "ki", "rr", "r2")]
            P.dma("sp", lambda e: e.dma_start(out=cst_sb[:], in_=cst), writes=[R_const])
            P.dma("sp", lambda e: e.dma_start(out=mats_sb[:], in_=mats), writes=[R_const])
            P.dma("sp", lambda e: e.dma_start(out=posi[:], in_=pos.to_broadcast([128, T])), writes=[Rp])
            P.op("dve", lambda e: e.tensor_copy(out=ident_bf[:], in_=ident_f), reads=[R_const], writes=[R_const])
            P.op("dve", lambda e: e.tensor_copy(out=ones_bf[:], in_=ones_f), reads=[R_const], writes=[R_const])
            P.op("dve", lambda e: e.tensor_copy(out=iota_bf[:], in_=iota_f), reads=[R_const], writes=[R_const])
            P.op("dve", lambda e: e.tensor_copy(out=ang[:], in_=posi[:]), reads=[Rp], writes=[Ra])
            P.op("dve", lambda e: e.tensor_scalar(out=ang[:], in0=ang[:], scalar1=cst_sb[:, 68:69], scalar2=None,
                                                  op0=ALU.mult), reads=[Ra, R_const], writes=[Ra])
            P.op("dve", lambda e: e.tensor_scalar(out=kf[:], in0=ang[:], scalar1=1.0 / TWO_PI, scalar2=None,
                                                  op0=ALU.mult), reads=[Ra], writes=[Rk])
            P.op("dve", lambda e: e.tensor_copy(out=ki[:], in_=kf[:]), reads=[Rk], writes=[Rki])
            P.op("dve", lambda e: e.tensor_copy(out=kf[:], in_=ki[:]), reads=[Rki], writes=[Rk])
            P.op("dve", lambda e: e.scalar_tensor_tensor(out=rr[:], in0=kf[:], scalar=-CW1, in1=ang[:],
                                                         op0=ALU.mult, op1=ALU.add), reads=[Rk, Ra], writes=[Rr])
            P.op("dve", lambda e: e.scalar_tensor_tensor(out=r2[:], in0=kf[:], scalar=-CW2, in1=rr[:],
                                                         op0=ALU.mult, op1=ALU.add), reads=[Rk, Rr], writes=[Rr2])
            P.op("dve", lambda e: e.scalar_tensor_tensor(out=rr[:], in0=kf[:], scalar=-CW3, in1=r2[:],
                                                         op0=ALU.mult, op1=ALU.add), reads=[Rk, Rr2], writes=[Rr])
            def wrap_sin(dst, shift, sign_col):
                P.op("dve", lambda e: e.tensor_scalar(out=r2[:], in0=rr[:], scalar1=shift, scalar2=None, op0=ALU.add),
                     reads=[Rr], writes=[Rr2])
                P.op("dve", lambda e: e.tensor_scalar(out=kf[:], in0=r2[:], scalar1=math.pi, scalar2=-TWO_PI,
                                                      op0=ALU.is_gt, op1=ALU.mult), reads=[Rr2], writes=[Rk])
                P.op("dve", lambda e: e.tensor_tensor(out=r2[:], in0=r2[:], in1=kf[:], op=ALU.add),
                     reads=[Rr2, Rk], writes=[Rr2])
                P.op("dve", lambda e: e.tensor_scalar(out=kf[:], in0=r2[:], scalar1=-math.pi, scalar2=TWO_PI,
                                                      op0=ALU.is_lt, op1=ALU.mult), reads=[Rr2], writes=[Rk])
                P.op("dve", lambda e: e.tensor_tensor(out=r2[:], in0=r2[:], in1=kf[:], op=ALU.add),
                     reads=[Rr2, Rk], writes=[Rr2])
                P.op("dve", lambda e: e.tensor_scalar(out=r2[:], in0=r2[:], scalar1=math.pi, scalar2=-math.pi,
                                                      op0=ALU.min, op1=ALU.max), reads=[Rr2], writes=[Rr2])
                P.op("act", lambda e: e.activation(out=dst[:], in_=r2[:], func=AF.Sin), reads=[Rr2], writes=[R_rope])
                if sign_col is not None:
                    P.op("dve", lambda e: e.tensor_scalar(out=dst[:], in0=dst[:], scalar1=cst_sb[:, sign_col:sign_col + 1],
                                                          scalar2=None, op0=ALU.mult),
                         reads=[R_rope, R_const], writes=[R_rope])

            wrap_sin(sinT, 0.0, 69)
            wrap_sin(cosT, math.pi / 2, None)
            pro_some(17)
            P.emit()

        def rmsnorm_multi(items, g0, all_act=False):
            for (src, Rsrc, dstfn, Rdst, bufs) in items:
                xs, Rxs, ss, ms, Rst, pst, Rpst = bufs
                P.op("act", lambda e, xs=xs, src=src, ss=ss: e.activation(out=xs[:], in_=src, func=AF.Square,
                                                                          accum_out=ss[:, 0:1]),
                     reads=[Rsrc], writes=[Rxs, Rst])
            for (src, Rsrc, dstfn, Rdst, bufs) in items:
                xs, Rxs, ss, ms, Rst, pst, Rpst = bufs
                P.op("act", lambda e, ss=ss, ms=ms: e.activation(out=ms[:, 0:1], in_=ss[:, 0:1], func=AF.Ln,
                                                                 scale=1.0 / D, bias=cst_sb[:, 86:87]),
                     reads=[Rst, R_const], writes=[Rst])
            for (src, Rsrc, dstfn, Rdst, bufs) in items:
                xs, Rxs, ss, ms, Rst, pst, Rpst = bufs
                P.op("act", lambda e, ms=ms: e.activation(out=ms[:, 2:3], in_=ms[:, 0:1], func=AF.Exp, scale=-0.5),
                     reads=[Rst], writes=[Rst])
            for (src, Rsrc, dstfn, Rdst, bufs) in items:
                xs, Rxs, ss, ms, Rst, pst, Rpst = bufs
                P.op("act", lambda e, xs=xs, src=src, ms=ms: e.activation(out=xs[:], in_=src, func=AF.Copy,
                                                                          scale=ms[:, 2:3]),
                     reads=[Rsrc, Rst], writes=[Rxs])
            for half in range(2):
                for (src, Rsrc, dstfn, Rdst, bufs) in items:
                    xs, Rxs, ss, ms, Rst, pst, Rpst = bufs
                    for j in range(8):
                        dc = half * 8 + j
                        P.op("pe", lambda e, dc=dc, j=j, half=half, pst=pst, xs=xs: e.transpose(
                            out=pst[half][:, j * 128:(j + 1) * 128], in_=xs[:, dc * 128:(dc + 1) * 128],
                            identity=ident_bf[:]), reads=[Rxs, R_const], writes=[Rpst[half]])
                for (src, Rsrc, dstfn, Rdst, bufs) in items:
                    xs, Rxs, ss, ms, Rst, pst, Rpst = bufs
                    for j in range(8):
                        dc = half * 8 + j
                        if j % 2 == 0 and not all_act:
                            P.op("dve", lambda e, dc=dc, j=j, half=half, pst=pst, dstfn=dstfn: e.tensor_scalar(
                                out=dstfn(dc), in0=pst[half][:, j * 128:(j + 1) * 128],
                                scalar1=cst_sb[:, g0 + dc:g0 + dc + 1], scalar2=None, op0=ALU.mult),
                                reads=[Rpst[half], R_const], writes=[Rdst], indep=True)
                        else:
                            P.op("act", lambda e, dc=dc, j=j, half=half, pst=pst, dstfn=dstfn: e.activation(
                                out=dstfn(dc), in_=pst[half][:, j * 128:(j + 1) * 128], func=AF.Copy,
                                scale=cst_sb[:, g0 + dc:g0 + dc + 1]),
                                reads=[Rpst[half], R_const], writes=[Rdst], indep=True)

        def rmsnorm_T(src, Rsrc, g0, dstfn, Rdst, bufs, tag):
            rmsnorm_multi([(src, Rsrc, dstfn, Rdst, bufs)], g0)

        def rms_bufs(st, tag):
            xs = sbuf(st, "rn_xs" + tag, [128, D], BF16)
            ss = sbuf(st, "rn_ss" + tag, [128, 1], F32)
            ms = sbuf(st, "rn_ms" + tag, [128, 4], F32)
            pst = [psum(st, "rn_ps%d%s" % (h, tag), [128, 1024], BF16) for h in range(2)]
            return (xs, P.res("rn_xs" + tag), ss, ms, P.res("rn_st" + tag), pst,
                    [P.res("rn_ps0" + tag), P.res("rn_ps1" + tag)])

        KT = sbuf(stKV, "KT", [128, 8, T], BF16)
        KRT = sbuf(stKV, "KRT", [128, T], BF16)
        Vsb = sbuf(stKV, "Vsb", [128, 16, 1024], BF16)
        rkcol = sbuf(stKV, "rkcol", [128, 16, 8], F32)
        R_K = [P.res("K%d" % g) for g in range(NG)]
        R_V = [P.res("V%d" % g) for g in range(NG)]

        with ExitStack() as st:
            wuk_sb = sbuf(st, "wuk_sb", [128, 4, 1024], BF16)
            wv_sb = sbuf(st, "wv_sb", [128, 4, 1024], BF16)
            wpool_sb = sbuf(st, "wpool_sb", [128, 4, 2, 256], BF16)
            R_w = P.res("m1w")
            P.dma("pool", lambda e: e.dma_start(out=wuk_sb[:], in_=Wuk.rearrange("p (a b) -> p a b", a=4),
                                                max_dma_last_dim=4096), writes=[R_w])
            P.dma("pool", lambda e: e.dma_start(out=wv_sb[:], in_=Wv.rearrange("p (a b) -> p a b", a=4),
                                                max_dma_last_dim=4096), writes=[R_w])
            P.dma("pool", lambda e: e.dma_start(out=wpool_sb[:], in_=Wpool.rearrange("p (a b c) -> p a b c", a=4, b=2),
                                                max_dma_last_dim=1024), writes=[R_w])
            xt = [sbuf(st, "m1_xt%d" % i, [128, D], F32) for i in range(2)]
            Rxt = [P.res("m1_xt%d" % i) for i in range(2)]
            rbs = [rms_bufs(st, "m1_%d" % i) for i in range(2)]
            xnT = sbuf(st, "xnT", [128, 16, TG], BF16)
            RxnT = P.res("xnT")
            wbuf = [sbuf(st, "m1_wb%d" % i, [128, 16, 128], BF16) for i in range(6)]
            Rwb = [P.res("m1_wb%d" % i) for i in range(6)]
            psz = [psum(st, "psz%d" % i, [128, 512], F32) for i in range(2)]
            Rpsz = [P.res("psz%d" % i) for i in range(2)]
            pz_ring = [psz[0][:, 0:TG], psz[1][:, 0:TG]]
            Rpz_ring = [Rpsz[0], Rpsz[1]]
            for k_ in range(2):
                for h_ in range(2):
                    pz_ring.append(rbs[k_][5][h_][:].bitcast(F32)[:, 0:TG])
                    Rpz_ring.append(rbs[k_][6][h_])
            psm = [psum(st, "psm%d" % i, [128, 512], F32) for i in range(2)]
            Rpsm = [P.res("psm%d" % i) for i in range(2)]
            halo = sbuf(st, "halo", [128, 8, 16], F32)
            Rhalo = [P.res("halo%d" % c) for c in range(8)]
            zpad = [sbuf(st, "zpad%d" % i, [128, 16 + TG], F32) for i in range(2)]
            Rzp = [P.res("zpad%d" % i) for i in range(2)]
            abuf = [sbuf(st, "abuf%d" % i, [128, 16 + TG], F32) for i in range(2)]
            Rab = [P.res("abuf%d" % i) for i in range(2)]
            fix16 = sbuf(st, "fix16", [128, 16], F32)
            Rfix = P.res("fix16")
            dbuf = sbuf(st, "dbuf", [128, 2, TG], BF16)
            Rdb = [P.res("dbuf%d" % i) for i in range(2)]
            ypo = sbuf(st, "ypo", [128, 8, TG], BF16)
            Rypo = P.res("ypo")
            zlat = sbuf(st, "zlat", [128, 4, TG], F32)
            Rzlat = P.res("zlat")
            sq = [sbuf(st, "m1_sq%d" % i, [128, TG], F32) for i in range(2)]
            Rsq = [P.res("m1_sq%d" % i) for i in range(2)]
            rlat = sbuf(st, "rlat", [128, TG], F32)
            Rrlat = P.res("rlat")
            cqn = sbuf(st, "cqn", [128, 4, TG], BF16)
            Rcqn = P.res("cqn")
            ckvn = sbuf(st, "ckvn", [128, 4, TG], BF16)
            Rckvn = P.res("ckvn")
            krg = sbuf(st, "krg", [128, TG], F32)
            krsq = sbuf(st, "krsq", [128, TG], F32)
            Rkr = P.res("kr")
            t1 = sbuf(st, "m1_t1", [128, TG], F32)
            t2 = sbuf(st, "m1_t2", [128, TG], F32)
            Rt1, Rt2 = P.res("m1_t1"), P.res("m1_t2")
            colb = sbuf(st, "colb", [128, 3, 16], F32)
            Rcolb = P.res("colb")

            P.op("dve", lambda e: e.memset(halo[:], 0.0), writes=Rhalo)
            dq = []

            def dq_tick():
                for it in dq:
                    it[0] -= 1
                while dq and dq[0][0] <= 0:
                    for f_ in dq.pop(0)[1]:
                        f_()

            def dq_flush():
                while dq:
                    for f_ in dq.pop(0)[1]:
                        f_()

            wcnt = [0]
            pcnt = [0]
            mcnt = [0]

            for g in range(NG):
                t0 = g * TG
                items = []
                for tt in range(TG // 128):
                    k = tt % 2
                    row0 = t0 + tt * 128
                    P.dma("sp", lambda e, k=k, row0=row0: e.dma_start(out=xt[k][:], in_=x[row0:row0 + 128, :]),
                          writes=[Rxt[k]])
                    items.append((xt[k][:], Rxt[k], (lambda dc, tt=tt: xnT[:, dc, tt * 128:(tt + 1) * 128]), RxnT, rbs[k]))
                rmsnorm_multi(items, 0)
                pro_some(8)
                for c in range(17):
                    wk = wcnt[0] % 6
                    wcnt[0] += 1
                    P.dma("sp", lambda e, wk=wk, c=c: e.dma_start(
                        out=wbuf[wk][:], in_=Winb[c].rearrange("p (a b) -> p a b", a=16)),
                        reads=[RWin[c]], writes=[Rwb[wk]])
                    pk = pcnt[0] % len(pz_ring)
                    pcnt[0] += 1
                    pz = pz_ring[pk]
                    for dc in range(16):
                        P.op("pe", lambda e, wk=wk, dc=dc, pz=pz: e.matmul(
                            out=pz, lhsT=wbuf[wk][:, dc, :], rhs=xnT[:, dc, :], start=(dc == 0), stop=(dc == 15)),
                            reads=[Rwb[wk], RxnT], writes=[Rpz_ring[pk]])
                    dq_tick()
                    if c < 8:
                        grp = c // 2
                        S = grp + 1
                        w = 2 ** S
                        zk = c % 2
                        zp = zpad[zk]
                        P.op("act", lambda e, zp=zp, pz=pz: e.activation(out=zp[:, 16:16 + TG], in_=pz, func=AF.Copy),
                             reads=[Rpz_ring[pk]], writes=[Rzp[zk]])
                        P.op("dve", lambda e, zp=zp, c=c: e.tensor_copy(out=zp[:, 0:16], in_=halo[:, c, :]),
                             reads=[Rhalo[c]], writes=[Rzp[zk]])
                        P.op("dve", lambda e, zp=zp, c=c: e.tensor_copy(out=halo[:, c, :], in_=zp[:, TG:TG + 16]),
                             reads=[Rzp[zk]], writes=[Rhalo[c]])
                        prev, Rprev = zp, Rzp[zk]
                        L = 16 + TG
                        for s in range(1, S + 1):
                            sh = 2 ** (s - 1)
                            lo = 2 ** s - 1
                            ab = abuf[s % 2]
                            P.op("dve", lambda e, ab=ab, prev=prev, lo=lo, sh=sh, L=L: e.tensor_tensor(
                                out=ab[:, lo:L], in0=prev[:, lo:L], in1=prev[:, lo - sh:L - sh], op=ALU.add),
                                reads=[Rprev], writes=[Rab[s % 2]])
                            prev, Rprev = ab, Rab[s % 2]
                        P.op("dve", lambda e, prev=prev, zp=zp, zk=zk, w=w: e.scalar_tensor_tensor(
                            out=dbuf[:, zk, :], in0=prev[:, 16:16 + TG], scalar=1.0 / w, in1=zp[:, 16:16 + TG],
                            op0=ALU.mult, op1=ALU.subtract), reads=[Rprev, Rzp[zk]], writes=[Rdb[zk]])
                        if g == 0:
                            P.op("dve", lambda e, prev=prev, w=w: e.tensor_tensor(
                                out=fix16[:, 0:w - 1], in0=prev[:, 16:16 + w - 1], in1=cst_sb[:, 70:70 + w - 1],
                                op=ALU.mult), reads=[Rprev, R_const], writes=[Rfix])
                            P.op("dve", lambda e, zp=zp, zk=zk, w=w: e.tensor_tensor(
                                out=dbuf[:, zk, 0:w - 1], in0=fix16[:, 0:w - 1], in1=zp[:, 16:16 + w - 1],
                                op=ALU.subtract), reads=[Rfix, Rzp[zk]], writes=[Rdb[zk]])
                        if c % 2 == 1:
                            P.defer_begin()
                            for oc in range(2):
                                mk = mcnt[0] % 2
                                mcnt[0] += 1
                                pm = psm[mk][:, 0:TG]
                                for ic in range(2):
                                    P.op("pe", lambda e, pm=pm, grp=grp, ic=ic, oc=oc: e.matmul(
                                        out=pm, lhsT=wpool_sb[:, grp, ic, oc * 128:(oc + 1) * 128], rhs=dbuf[:, ic, :],
                                        start=(ic == 0), stop=(ic == 1)), reads=[R_w, Rdb[ic]], writes=[Rpsm[mk]])
                                cc = 2 * grp + oc
                                P.op("act", lambda e, pm=pm, cc=cc: e.activation(
                                    out=ypo[:, cc, :], in_=pm, func=AF.Copy, scale=cst_sb[:, 56 + cc:57 + cc]),
                                    reads=[Rpsm[mk], R_const], writes=[Rypo], indep=True)
                            dq.append([1, P.defer_end()])
                    elif c < 16:
                        j = (c - 8) % 4
                        isq = c < 12
                        sk = c % 2
                        P.op("act", lambda e, j=j, pz=pz: e.activation(out=zlat[:, j, :], in_=pz, func=AF.Copy),
                             reads=[Rpz_ring[pk]], writes=[Rzlat], indep=True)
                        P.op("act", lambda e, sk=sk, pz=pz: e.activation(out=sq[sk][:], in_=pz, func=AF.Square),
                             reads=[Rpz_ring[pk]], writes=[Rsq[sk]])
                        P.defer_begin()
                        if j == 0:
                            smk = mcnt[0] % 2
                            mcnt[0] += 1
                        pm = psm[smk][:, 0:TG]
                        P.op("pe", lambda e, pm=pm, sk=sk, j=j: e.matmul(out=pm, lhsT=ones_f, rhs=sq[sk][:],
                                                                          start=(j == 0), stop=(j == 3)),
                             reads=[Rsq[sk], R_const], writes=[Rpsm[smk]])
                        if j == 3:
                            P.op("dve", lambda e, pm=pm: e.tensor_scalar(out=rlat[:], in0=pm, scalar1=1.0 / 512,
                                                                           scalar2=EPS, op0=ALU.mult, op1=ALU.add),
                                 reads=[Rpsm[smk]], writes=[Rrlat])
                            P.op("act", lambda e: e.activation(out=rlat[:], in_=rlat[:], func=AF.Sqrt),
                                 reads=[Rrlat], writes=[Rrlat])
                            P.op("dve", lambda e: e.reciprocal(out=rlat[:], in_=rlat[:]), reads=[Rrlat], writes=[Rrlat])
                            dst, Rd, gc = (cqn, Rcqn, 48) if isq else (ckvn, Rckvn, 52)
                            for jj in range(4):
                                P.op("dve", lambda e, dst=dst, jj=jj, gc=gc: e.scalar_tensor_tensor(
                                    out=dst[:, jj, :], in0=zlat[:, jj, :], scalar=cst_sb[:, gc + jj:gc + jj + 1],
                                    in1=rlat[:], op0=ALU.mult, op1=ALU.mult),
                                    reads=[Rzlat, Rrlat, R_const], writes=[Rd], indep=(jj > 0))
                            if isq:
                                P.dma("sp", lambda e, g=g: e.dma_start(
                                    out=CQ[g].rearrange("p (a b) -> p a b", a=4), in_=cqn[:]), reads=[Rcqn])
                        dq.append([1, P.defer_end()])
                    else:
                        P.op("act", lambda e, pz=pz: e.activation(out=krg[:], in_=pz, func=AF.Copy,
                                                                    scale=cst_sb[:, 67:68]),
                             reads=[Rpz_ring[pk], R_const], writes=[Rkr])
                        P.op("act", lambda e, pz=pz: e.activation(out=krsq[:], in_=pz, func=AF.Square),
                             reads=[Rpz_ring[pk]], writes=[Rkr])
                dq_flush()
                P.dma("sp", lambda e, g=g: e.dma_start(out=YP[g].rearrange("p (a b) -> p a b", a=8), in_=ypo[:]),
                      reads=[Rypo])
                pro_some(6)
                mkc = mcnt[0] % 2
                mcnt[0] += 1
                pcol = psm[mkc]
                ntt = TG // 128
                for h in range(8):
                    pk = pcnt[0] % len(pz_ring)
                    pcnt[0] += 1
                    pz = pz_ring[pk]
                    for fc in range(4):
                        P.op("pe", lambda e, pz=pz, fc=fc, h=h: e.matmul(
                            out=pz, lhsT=wuk_sb[:, fc, h * 128:(h + 1) * 128], rhs=ckvn[:, fc, :],
                            start=(fc == 0), stop=(fc == 3)), reads=[R_w, Rckvn], writes=[Rpz_ring[pk]])
                    dq_tick()
                    P.op("act", lambda e, pz=pz, h=h, t0=t0: e.activation(
                        out=KT[:, h, t0:t0 + TG], in_=pz, func=AF.Copy, scale=cst_sb[:, 66:67]),
                        reads=[Rpz_ring[pk], R_const], writes=[R_K[g]], indep=True)
                    sk = h % 2
                    P.op("act", lambda e, pz=pz, sk=sk: e.activation(out=sq[sk][:], in_=pz, func=AF.Square),
                         reads=[Rpz_ring[pk]], writes=[Rsq[sk]])
                    P.defer_begin()
                    for tt in range(ntt):
                        col = (h * ntt + tt) * 2
                        P.op("pe", lambda e, sk=sk, tt=tt, col=col: e.matmul(
                            out=pcol[:, col:col + 2], lhsT=sq[sk][:, tt * 128:(tt + 1) * 128], rhs=ones_f[:, 0:2],
                            start=True, stop=False), reads=[Rsq[sk], R_const], writes=[Rpsm[mkc]])
                        P.op("pe", lambda e, tt=tt, col=col: e.matmul(
                            out=pcol[:, col:col + 2], lhsT=krsq[0:64, tt * 128:(tt + 1) * 128], rhs=ones_f[0:64, 0:2],
                            start=False, stop=True), reads=[Rkr, R_const], writes=[Rpsm[mkc]])
                    dq.append([1, P.defer_end()])
                dq_flush()
                nc_ = 8 * ntt
                P.op("dve", lambda e, nc_=nc_: e.tensor_scalar(
                    out=colb[:, 0, 0:nc_], in0=pcol[:, 0:2 * nc_].rearrange("p (a b) -> p a b", b=2)[:, :, 0],
                    scalar1=1.0, scalar2=192.0 * EPS, op0=ALU.mult, op1=ALU.add), reads=[Rpsm[mkc]], writes=[Rcolb])
                P.op("act", lambda e, nc_=nc_: e.activation(out=colb[:, 1, 0:nc_], in_=colb[:, 0, 0:nc_], func=AF.Sqrt),
                     reads=[Rcolb], writes=[Rcolb])
                P.op("dve", lambda e, nc_=nc_: e.reciprocal(out=colb[:, 2, 0:nc_], in_=colb[:, 1, 0:nc_]),
                     reads=[Rcolb], writes=[Rcolb])
                P.op("dve", lambda e, g=g, ntt=ntt, nc_=nc_: e.tensor_copy(
                    out=rkcol[:, g * ntt:(g + 1) * ntt, :].rearrange("p t h -> p h t"),
                    in_=colb[:, 2, 0:nc_].rearrange("p (h t) -> p h t", t=ntt)), reads=[Rcolb], writes=[R_K[g]])
                mk = mcnt[0] % 2
                mcnt[0] += 1
                pm = psm[mk][:, 0:TG]
                P.op("pe", lambda e, pm=pm: e.matmul(out=pm, lhsT=perm_f, rhs=krg[:], start=True, stop=True),
                     reads=[Rkr, R_const], writes=[Rpsm[mk]])
                P.op("dve", lambda e, t0=t0: e.tensor_tensor(out=t1[:], in0=krg[:], in1=cosT[:, t0:t0 + TG], op=ALU.mult),
                     reads=[Rkr, R_rope], writes=[Rt1])
                P.op("dve", lambda e, pm=pm, t0=t0: e.tensor_tensor(out=t2[:], in0=pm, in1=sinT[:, t0:t0 + TG],
                                                                     op=ALU.mult),
                     reads=[Rpsm[mk], R_rope], writes=[Rt2])
                P.op("dve", lambda e, t0=t0: e.tensor_tensor(out=KRT[:, t0:t0 + TG], in0=t1[:], in1=t2[:], op=ALU.add),
                     reads=[Rt1, Rt2], writes=[R_K[g]])
                for tt in range(ntt):
                    for nb in range(2):
                        mk = mcnt[0] % 2
                        mcnt[0] += 1
                        for fc in range(4):
                            P.op("pe", lambda e, mk=mk, fc=fc, tt=tt, nb=nb: e.matmul(
                                out=psm[mk][:], lhsT=ckvn[:, fc, tt * 128:(tt + 1) * 128],
                                rhs=wv_sb[:, fc, nb * 512:(nb + 1) * 512], start=(fc == 0), stop=(fc == 3)),
                                reads=[Rckvn, R_w], writes=[Rpsm[mk]])
                        P.op("act", lambda e, mk=mk, tt=tt, nb=nb, g=g, ntt=ntt: e.activation(
                            out=Vsb[:, g * ntt + tt, nb * 512:(nb + 1) * 512], in_=psm[mk][:], func=AF.Copy),
                            reads=[Rpsm[mk]], writes=[R_V[g]], indep=True)
            P.emit()
        if stop_after == "M1":
            stKV.close()
            return nc, None, st0

        with ExitStack() as st:
            wuq_sb = sbuf(st, "wuq_sb", [128, 4, 1536], BF16)
            mask_f = sbuf(st, "mask_f", [128, 2, TG], F32)
            mask_b = sbuf(st, "mask_b", [128, 2, TG], BF16)
            R_w = P.res("m2w")
            P.dma("pool", lambda e: e.dma_start(out=wuq_sb[:], in_=Wuq.rearrange("p (a b) -> p a b", a=4),
                                                max_dma_last_dim=2048), writes=[R_w])
            P.dma("sp", lambda e: e.dma_start(out=mask_f[:], in_=maskc.rearrange("p (a b) -> p a b", a=2)),
                  writes=[R_w])
            P.op("dve", lambda e: e.tensor_copy(out=mask_b[:], in_=mask_f[:]), reads=[R_w], writes=[R_w])
            cqn = sbuf(st, "m2_cqn", [128, 4, TG], BF16)
            Rcqn = P.res("m2_cqn")
            ymix = sbuf(st, "ymix", [128, 16, TG], BF16)
            Rypool = P.res("ymix_pool")
            Rymla = P.res("ymix_mla")
            psq = [psum(st, "psq%d" % i, [128, 512], F32) for i in range(2)]
            Rpsq = [P.res("psq%d" % i) for i in range(2)]
            pss = [psum(st, "pss%d" % i, [128, 512], F32) for i in range(2)]
            Rpss = [P.res("pss%d" % i) for i in range(2)]
            pso = psum(st, "pso", [128, 512], F32)
            psd = psum(st, "psd", [128, 512], F32)
            Rpso, Rpsd = P.res("pso"), P.res("psd")
            psh = [psum(st, "psh%d" % i, [128, 512], F32) for i in range(2)]
            Rpsh = [P.res("psh%d" % i) for i in range(2)]
            qtmp = [sbuf(st, "qtmp%d" % i, [128, TG], F32) for i in range(2)]
            sqq = [sbuf(st, "sqq%d" % i, [128, TG], F32) for i in range(2)]
            rq = [sbuf(st, "rq%d" % i, [128, TG], F32) for i in range(2)]
            Rqtmp = [P.res("qtmp%d" % i) for i in range(2)]
            Rsqq = [P.res("sqq%d" % i) for i in range(2)]
            Rrq = [P.res("rq%d" % i) for i in range(2)]
            qrg = sbuf(st, "qrg", [128, TG], F32)
            sqr = sbuf(st, "sqr", [128, TG], F32)
            Rqrg, Rsqr = P.res("qrg"), P.res("sqr")
            t1 = sbuf(st, "m2_t1", [128, TG], F32)
            t2 = sbuf(st, "m2_t2", [128, TG], F32)
            Rt1, Rt2 = P.res("m2_t1"), P.res("m2_t2")
            QTs = [sbuf(st, "QT%d" % i, [128, TG], BF16) for i in range(4)]
            RQTs = [P.res("QT%d" % i) for i in range(4)]
            QRTs = [sbuf(st, "QRT%d" % i, [128, TG], BF16) for i in range(2)]
            RQRTs = [P.res("QRT%d" % i) for i in range(2)]
            pT = [sbuf(st, "pT%d" % i, [128, TG], BF16) for i in range(3)]
            RpT = [P.res("pT%d" % i) for i in range(3)]
            rec = sbuf(st, "rec", [128, TG], F32)
            Rrec = P.res("rec")
            wob = [sbuf(st, "wob%d" % i, [128, 16, 512], BF16) for i in range(3)]
            Rwob = [P.res("wob%d" % i) for i in range(3)]
            xb = [sbuf(st, "m2_xb%d" % i, [128, 512], F32) for i in range(3)]
            Rxb = [P.res("m2_xb%d" % i) for i in range(3)]
            hb = [sbuf(st, "m2_hb%d" % i, [128, 512], F32) for i in range(3)]
            Rhb = [P.res("m2_hb%d" % i) for i in range(3)]
            qcnt = [0]
            scnt = [0]
            ptc = [0]
            wcnt = [0]
            hcnt = [0]
            ntt = TG // 128

            for g in range(NG):
                t0 = g * TG
                P.dma("sp", lambda e, g=g: e.dma_start(out=cqn[:], in_=CQ[g].rearrange("p (a b) -> p a b", a=4)),
                      writes=[Rcqn])
                P.dma("sp", lambda e, g=g: e.dma_start(out=ymix[:, 0:8, :], in_=YP[g].rearrange("p (a b) -> p a b", a=8)),
                      writes=[Rypool])
                pro_some(8)
                nkt = ntt * (g + 1)
                def prep(j, t0=t0):
                    QT, RQT = QTs[2 * (j % 2):2 * (j % 2) + 2], RQTs[2 * (j % 2):2 * (j % 2) + 2]
                    QRT, RQRT = QRTs[j % 2], RQRTs[j % 2]
                    qk = qcnt[0] % 2
                    qcnt[0] += 1
                    pqr = psq[qk][:, 0:TG]
                    for fc in range(4):
                        P.op("pe", lambda e, pqr=pqr, fc=fc, j=j: e.matmul(
                            out=pqr, lhsT=wuq_sb[:, fc, 1024 + j * 128:1024 + (j + 1) * 128], rhs=cqn[:, fc, :],
                            start=(fc == 0), stop=(fc == 3)), reads=[R_w, Rcqn], writes=[Rpsq[qk]])
                    P.op("act", lambda e, pqr=pqr: e.activation(out=qrg[:], in_=pqr, func=AF.Copy, scale=cst_sb[:, 65:66]),
                         reads=[Rpsq[qk], R_const], writes=[Rqrg])
                    P.op("act", lambda e, pqr=pqr: e.activation(out=sqr[:], in_=pqr, func=AF.Square),
                         reads=[Rpsq[qk]], writes=[Rsqr])
                    for hh in range(2):
                        h = 2 * j + hh
                        qk2 = qcnt[0] % 2
                        qcnt[0] += 1
                        pq = psq[qk2][:, 0:TG]
                        for fc in range(4):
                            P.op("pe", lambda e, pq=pq, fc=fc, h=h: e.matmul(
                                out=pq, lhsT=wuq_sb[:, fc, h * 128:(h + 1) * 128], rhs=cqn[:, fc, :],
                                start=(fc == 0), stop=(fc == 3)), reads=[R_w, Rcqn], writes=[Rpsq[qk2]])
                        P.op("act", lambda e, pq=pq, hh=hh: e.activation(out=qtmp[hh][:], in_=pq, func=AF.Copy,
                                                                           scale=cst_sb[:, 64:65]),
                             reads=[Rpsq[qk2], R_const], writes=[Rqtmp[hh]])
                        P.op("act", lambda e, pq=pq, hh=hh: e.activation(out=sqq[hh][:], in_=pq, func=AF.Square),
                             reads=[Rpsq[qk2]], writes=[Rsqq[hh]])
                        P.op("pe", lambda e, pq=pq, hh=hh: e.matmul(out=pq, lhsT=ones_f, rhs=sqq[hh][:], start=True,
                                                                      stop=False),
                             reads=[Rsqq[hh], R_const, Rqtmp[hh]], writes=[Rpsq[qk2]])
                        P.op("pe", lambda e, pq=pq, hh=hh: e.matmul(out=pq, lhsT=(Llo_f if hh == 0 else Lhi_f),
                                                                      rhs=sqr[:], start=False, stop=True),
                             reads=[Rsqr, R_const], writes=[Rpsq[qk2]])
                        P.op("dve", lambda e, pq=pq, hh=hh: e.tensor_scalar(out=rq[hh][:], in0=pq, scalar1=1.0 / 192,
                                                                              scalar2=EPS, op0=ALU.mult, op1=ALU.add),
                             reads=[Rpsq[qk2]], writes=[Rrq[hh]])
                        P.op("act", lambda e, hh=hh: e.activation(out=rq[hh][:], in_=rq[hh][:], func=AF.Sqrt),
                             reads=[Rrq[hh]], writes=[Rrq[hh]])
                        P.op("dve", lambda e, hh=hh: e.reciprocal(out=rq[hh][:], in_=rq[hh][:]),
                             reads=[Rrq[hh]], writes=[Rrq[hh]])
                        P.op("dve", lambda e, hh=hh: e.tensor_tensor(out=QT[hh][:], in0=qtmp[hh][:], in1=rq[hh][:],
                                                                       op=ALU.mult),
                             reads=[Rqtmp[hh], Rrq[hh]], writes=[RQT[hh]])
                    qk3 = qcnt[0] % 2
                    qcnt[0] += 1
                    pr = psq[qk3][:, 0:TG]
                    P.op("pe", lambda e, pr=pr: e.matmul(out=pr, lhsT=perm_f, rhs=qrg[:], start=True, stop=True),
                         reads=[Rqrg, R_const], writes=[Rpsq[qk3]])
                    P.op("dve", lambda e, t0=t0: e.tensor_tensor(out=t1[:], in0=qrg[:], in1=cosT[:, t0:t0 + TG],
                                                                  op=ALU.mult), reads=[Rqrg, R_rope], writes=[Rt1])
                    P.op("dve", lambda e, pr=pr, t0=t0: e.tensor_tensor(out=t2[:], in0=pr, in1=sinT[:, t0:t0 + TG],
                                                                         op=ALU.mult),
                         reads=[Rpsq[qk3], R_rope], writes=[Rt2])
                    P.op("dve", lambda e: e.tensor_tensor(out=t1[:], in0=t1[:], in1=t2[:], op=ALU.add),
                         reads=[Rt1, Rt2], writes=[Rt1])
                    P.op("dve", lambda e: e.tensor_tensor(out=QRT[0:64, :], in0=t1[0:64, :], in1=rq[0][0:64, :],
                                                          op=ALU.mult), reads=[Rt1, Rrq[0]], writes=[RQRT])
                    P.op("dve", lambda e: e.tensor_tensor(out=QRT[64:128, :], in0=t1[64:128, :], in1=rq[1][64:128, :],
                                                          op=ALU.mult), reads=[Rt1, Rrq[1]], writes=[RQRT], indep=True)

                prep(0)
                for j in range(4):
                    QT, RQT = QTs[2 * (j % 2):2 * (j % 2) + 2], RQTs[2 * (j % 2):2 * (j % 2) + 2]
                    QRT, RQRT = QRTs[j % 2], RQRTs[j % 2]
                    pending = []
                    if j + 1 < 4:
                        P.defer_begin()
                        prep(j + 1)
                        pending = P.defer_end()
                    kstep = -(-len(pending) // max(1, 2 * nkt - 1))
                    for hh in range(2):
                        h = 2 * j + hh
                        hp = 64 * hh
                        def emit_S(kt, h=h, hh=hh, hp=hp, QT=QT, QRT=QRT):
                            sk = scnt[0] % 2
                            scnt[0] += 1
                            ps_ = pss[sk][:, 0:TG]
                            gk = kt // ntt
                            P.op("pe", lambda e, ps_=ps_, h=h, kt=kt, hh=hh: e.matmul(
                                out=ps_, lhsT=KT[:, h, kt * 128:(kt + 1) * 128], rhs=QT[hh][:], start=True, stop=False),
                                reads=[R_K[gk], RQT[hh]], writes=[Rpss[sk]])
                            P.op("pe", lambda e, ps_=ps_, kt=kt, hp=hp: e.matmul(
                                out=ps_, lhsT=KRT[hp:hp + 64, kt * 128:(kt + 1) * 128], rhs=QRT[hp:hp + 64, :],
                                start=False, stop=True), reads=[R_K[gk], RQRT], writes=[Rpss[sk]])
                            return sk, ps_

                        nxt = emit_S(0)
                        for kt in range(nkt):
                            sk, ps_ = nxt
                            gk = kt // ntt
                            if kt + 1 < nkt:
                                nxt = emit_S(kt + 1)
                            pk_ = ptc[0] % 3
                            ptc[0] += 1
                            P.op("act", lambda e, ps_=ps_, pk_=pk_, kt=kt, h=h: e.activation(
                                out=pT[pk_][:], in_=ps_, func=AF.Exp, scale=rkcol[:, kt, h:h + 1]),
                                reads=[Rpss[sk], R_K[gk]], writes=[RpT[pk_]])
                            if kt >= ntt * g:
                                jm = kt - ntt * g
                                P.op("dve", lambda e, pk_=pk_, jm=jm: e.tensor_tensor(
                                    out=pT[pk_][:], in0=pT[pk_][:], in1=mask_b[:, jm, :], op=ALU.mult),
                                    reads=[RpT[pk_], R_w], writes=[RpT[pk_]])
                            P.op("pe", lambda e, pk_=pk_, kt=kt, h=h, nkt=nkt: e.matmul(
                                out=pso[:, 0:TG], lhsT=Vsb[:, kt, h * 128:(h + 1) * 128], rhs=pT[pk_][:],
                                start=(kt == 0), stop=(kt == nkt - 1)), reads=[R_V[gk], RpT[pk_]], writes=[Rpso])
                            P.op("pe", lambda e, pk_=pk_, kt=kt, nkt=nkt: e.matmul(
                                out=psd[:, 0:TG], lhsT=ones_bf[:], rhs=pT[pk_][:],
                                start=(kt == 0), stop=(kt == nkt - 1)), reads=[R_const, RpT[pk_]], writes=[Rpsd])
                            for _ in range(kstep):
                                if pending:
                                    pending.pop(0)()
                        P.op("dve", lambda e: e.reciprocal(out=rec[:], in_=psd[:, 0:TG]), reads=[Rpsd], writes=[Rrec])
                        P.op("dve", lambda e, h=h: e.tensor_tensor(out=ymix[:, 8 + h, :], in0=pso[:, 0:TG], in1=rec[:],
                                                                    op=ALU.mult),
                             reads=[Rpso, Rrec], writes=[Rymla])
                    while pending:
                        pending.pop(0)()
                for nb in range(4):
                    wk = wcnt[0] % 3
                    wcnt[0] += 1
                    P.dma("sp", lambda e, wk=wk, nb=nb: e.dma_start(
                        out=wob[wk][:], in_=Woutb[nb].rearrange("p (a b) -> p a b", a=16)),
                        reads=[RWout[nb]], writes=[Rwob[wk]])
                    for tt in range(ntt):
                        hk = hcnt[0] % 3
                        hk2 = hcnt[0] % 2
                        hcnt[0] += 1
                        row0 = t0 + tt * 128
                        P.dma("sp", lambda e, hk=hk, row0=row0, nb=nb: e.dma_start(
                            out=xb[hk][:], in_=x[row0:row0 + 128, nb * 512:(nb + 1) * 512]), writes=[Rxb[hk]])
                        for fc in range(16):
                            P.op("pe", lambda e, hk2=hk2, fc=fc, tt=tt, wk=wk: e.matmul(
                                out=psh[hk2][:], lhsT=ymix[:, fc, tt * 128:(tt + 1) * 128], rhs=wob[wk][:, fc, :],
                                start=(fc == 0), stop=(fc == 15)),
                                reads=[Rypool, Rymla, Rwob[wk]], writes=[Rpsh[hk2]])
                        P.op("dve", lambda e, hk=hk, hk2=hk2: e.tensor_tensor(out=hb[hk][:], in0=psh[hk2][:],
                                                                                in1=xb[hk][:], op=ALU.add),
                             reads=[Rpsh[hk2], Rxb[hk]], writes=[Rhb[hk]])
                        P.dma("sp", lambda e, hk=hk, row0=row0, nb=nb: e.dma_start(
                            out=H1[row0:row0 + 128, nb * 512:(nb + 1) * 512], in_=hb[hk][:]), reads=[Rhb[hk]])
            P.emit()
        stKV.close()
        if stop_after == "M2":
            return nc, None, st0

        ntt = TG // 128
        IRv = IRs.rearrange("k p t -> p k t")
        with ExitStack() as st:
            subk_sb = sbuf(st, "subk_sb", [128, 16, 128], F32)
            iota16 = sbuf(st, "iota16", [128, 16], F32)
            R_w = P.res("f1w")
            P.dma("sp", lambda e: e.dma_start(out=subk_sb[:], in_=subk.rearrange("p (a b) -> p a b", a=16)), writes=[R_w])
            P.op("dve", lambda e: e.tensor_copy(out=iota16[:], in_=iota_f[:, 0:16]), reads=[R_const], writes=[R_w])
            ht = [sbuf(st, "f1_ht%d" % i, [128, D], F32) for i in range(2)]
            Rht = [P.res("f1_ht%d" % i) for i in range(2)]
            rbs = [rms_bufs(st, "f1_%d" % i) for i in range(2)]
            xn2 = sbuf(st, "xn2", [128, 16, TG], BF16)
            Rxn2 = P.res("xn2")
            wbuf = [sbuf(st, "f1_wb%d" % i, [128, 16, 128], BF16) for i in range(4)]
            Rwb = [P.res("f1_wb%d" % i) for i in range(4)]
            psq = [psum(st, "f1_psq%d" % i, [128, 512], F32) for i in range(2)]
            Rpsq = [P.res("f1_psq%d" % i) for i in range(2)]
            pssc = [psum(st, "f1_pssc%d" % i, [128, 512], F32) for i in range(2)]
            Rpssc = [P.res("f1_pssc%d" % i) for i in range(2)]
            qpTs = [sbuf(st, "qpT%d" % i, [128, 16, TG], F32) for i in range(2)]
            RqpTs = [P.res("qpT%d" % i) for i in range(2)]
            sc = sbuf(st, "sc", [128, 16, 128], F32)
            sc2 = sbuf(st, "sc2", [128, 16, 128], F32)
            Rsc = [P.res("sc%d" % i) for i in range(16)]
            Rsc2 = [P.res("sc2_%d" % i) for i in range(16)]
            v16 = sbuf(st, "v16", [128, 16, 16], F32)
            i16 = sbuf(st, "i16", [128, 16, 16], U32)
            i16f = sbuf(st, "i16f", [128, 16, 16], F32)
            Rv16 = [P.res("v16_%d" % i) for i in range(16)]
            Ri16 = [P.res("i16_%d" % i) for i in range(16)]
            Ri16f = P.res("i16f")
            cand = sbuf(st, "cand", [128, 8, 256], F32)
            cand2 = sbuf(st, "cand2", [128, 8, 256], F32)
            Rcand = [P.res("cand%d" % i) for i in range(8)]
            Rcand2 = [P.res("cand2_%d" % i) for i in range(8)]
            vs = sbuf(st, "vs", [128, 8, 16], F32)
            ci = sbuf(st, "ci", [128, 8, 16], U32)
            Rvs = [P.res("vs%d" % i) for i in range(8)]
            Rci = [P.res("ci%d" % i) for i in range(8)]
            abi = sbuf(st, "abi", [128, 2, 128], U32)
            abf = sbuf(st, "abf", [128, 2, 128], F32)
            Rabi, Rabf = P.res("abi"), P.res("abf")
            eqb = [sbuf(st, "eqb%d" % i, [128, 8, 16, 16], F32) for i in range(2)]
            Reqb = [P.res("eqb%d" % i) for i in range(2)]
            tris = [sbuf(st, "tri%d" % i, [128, 3, 128], F32) for i in range(4)]
            Rtris = [P.res("tri%d" % i) for i in range(4)]
            gsm = sbuf(st, "gsm", [128, 2, 8], F32)
            Rgsm = P.res("gsm")
            pstr = pssc[1]
            Rpstr = Rpssc[1]
            trT = sbuf(st, "trT", [128, 3, 128], F32)
            RtrT = P.res("trT")
            wcnt = [0]
            qcnt = [0]
            tcnt = [0]

            def top16(src, Rsrc, src2, Rsrc2, vout, Rvout, iout, Riout, n):
                for k in range(n):
                    P.op("dve", lambda e, k=k: e.max(out=vout(k)[:, 0:8], in_=src(k)), reads=[Rsrc[k]], writes=[Rvout[k]])
                for k in range(n):
                    P.op("dve", lambda e, k=k: e.max_index(out=iout(k)[:, 0:8], in_max=vout(k)[:, 0:8], in_values=src(k)),
                         reads=[Rsrc[k], Rvout[k]], writes=[Riout[k]])
                for k in range(n):
                    P.op("dve", lambda e, k=k: e.match_replace(out=src2(k), in_to_replace=vout(k)[:, 0:8],
                                                               in_values=src(k), imm_value=-1e30),
                         reads=[Rsrc[k], Rvout[k]], writes=[Rsrc2[k]])
                for k in range(n):
                    P.op("dve", lambda e, k=k: e.max(out=vout(k)[:, 8:16], in_=src2(k)), reads=[Rsrc2[k]],
                         writes=[Rvout[k]])
                for k in range(n):
                    P.op("dve", lambda e, k=k: e.max_index(out=iout(k)[:, 8:16], in_max=vout(k)[:, 8:16],
                                                           in_values=src2(k)),
                         reads=[Rsrc2[k], Rvout[k]], writes=[Riout[k]])

            def f1_front(g):
                t0 = g * TG
                qpT, RqpT = qpTs[g % 2], RqpTs[g % 2]
                items = []
                for tt in range(ntt):
                    k = tt % 2
                    row0 = t0 + tt * 128
                    P.dma("sp", lambda e, k=k, row0=row0: e.dma_start(out=ht[k][:], in_=H1[row0:row0 + 128, :]),
                          writes=[Rht[k]])
                    items.append((ht[k][:], Rht[k], (lambda dc, tt=tt: xn2[:, dc, tt * 128:(tt + 1) * 128]), Rxn2, rbs[k]))
                rmsnorm_multi(items, 16, all_act=True)
                P.dma("sp", lambda e, g=g: e.dma_start(out=XN2[g].rearrange("p (a b) -> p a b", a=16), in_=xn2[:]),
                      reads=[Rxn2])
                pro_some(40)
                for c in range(16):
                    wk = wcnt[0] % 4
                    wcnt[0] += 1
                    P.dma("sp", lambda e, wk=wk, c=c: e.dma_start(
                        out=wbuf[wk][:], in_=Wpqb[c].rearrange("p (a b) -> p a b", a=16)),
                        reads=[RWpq[c]], writes=[Rwb[wk]])
                    qk = qcnt[0] % 2
                    qcnt[0] += 1
                    pq = psq[qk][:, 0:TG]
                    for dc in range(16):
                        P.op("pe", lambda e, wk=wk, dc=dc, pq=pq: e.matmul(
                            out=pq, lhsT=wbuf[wk][:, dc, :], rhs=xn2[:, dc, :], start=(dc == 0), stop=(dc == 15)),
                            reads=[Rwb[wk], Rxn2], writes=[Rpsq[qk]])
                    P.op("act", lambda e, pq=pq, c=c: e.activation(out=qpT[:, c, :], in_=pq, func=AF.Copy),
                         reads=[Rpsq[qk]], writes=[RqpT], indep=True)

            def f1_main(g, tt):
                t0 = g * TG
                qpT, RqpT = qpTs[g % 2], RqpTs[g % 2]
                tri, Rtri = tris[2 * (g % 2) + tt], Rtris[2 * (g % 2) + tt]
                if True:
                    for c in range(16):
                        bq = (c // 4) % 2
                        P.op("pe", lambda e, c=c, tt=tt, bq=bq: e.matmul(
                            out=pssc[bq][:, (c % 4) * 128:(c % 4 + 1) * 128],
                            lhsT=qpT[:, c, tt * 128:(tt + 1) * 128], rhs=subk_sb[:, c, :], start=True, stop=True),
                            reads=[RqpT, R_w], writes=[Rpssc[bq]])
                        if c % 4 == 3:
                            b4 = c // 4
                            P.op("dve", lambda e, b4=b4, bq=bq: e.tensor_copy(
                                out=sc[:, 4 * b4:4 * b4 + 4, :], in_=pssc[bq][:].rearrange("p (a b) -> p a b", a=4)),
                                reads=[Rpssc[bq]], writes=Rsc[4 * b4:4 * b4 + 4])
                    top16(lambda k: sc[:, k, :], Rsc, lambda k: sc2[:, k, :], Rsc2,
                          lambda k: v16[:, k, :], Rv16, lambda k: i16[:, k, :], Ri16, 16)
                    P.op("dve", lambda e: e.tensor_copy(out=i16f[:], in_=i16[:]), reads=Ri16, writes=[Ri16f])
                    v16v = v16[:].rearrange("p (h s) a -> p h s a", s=2)
                    i16v = i16f[:].rearrange("p (h s) a -> p h s a", s=2)
                    P.op("dve", lambda e, v16v=v16v: e.tensor_tensor(
                        out=cand[:].rearrange("p h (a b) -> p h a b", a=16),
                        in0=v16v[:, :, 0, :].unsqueeze(3).to_broadcast([128, 8, 16, 16]),
                        in1=v16v[:, :, 1, :].unsqueeze(2).to_broadcast([128, 8, 16, 16]), op=ALU.add),
                        reads=Rv16, writes=Rcand)
                    top16(lambda k: cand[:, k, :], Rcand, lambda k: cand2[:, k, :], Rcand2,
                          lambda k: vs[:, k, :], Rvs, lambda k: ci[:, k, :], Rci, 8)
                    civ = ci[:].rearrange("p h r -> p (h r)")
                    P.op("dve", lambda e, civ=civ: e.tensor_single_scalar(out=abi[:, 0, :], in_=civ, scalar=4,
                                                                          op=ALU.logical_shift_right),
                         reads=Rci, writes=[Rabi])
                    P.op("dve", lambda e, civ=civ: e.tensor_single_scalar(out=abi[:, 1, :], in_=civ, scalar=15,
                                                                          op=ALU.bitwise_and),
                         reads=Rci, writes=[Rabi], indep=True)
                    P.op("dve", lambda e: e.tensor_copy(out=abf[:], in_=abi[:]), reads=[Rabi], writes=[Rabf])
                    for s_ in range(2):
                        eb = eqb[s_]
                        P.op("dve", lambda e, eb=eb, s_=s_: e.tensor_tensor(
                            out=eb[:],
                            in0=iota16[:].unsqueeze(1).unsqueeze(1).to_broadcast([128, 8, 16, 16]),
                            in1=abf[:, s_, :].rearrange("p (h r) -> p h r", h=8).unsqueeze(3).to_broadcast([128, 8, 16, 16]),
                            op=ALU.is_equal), reads=[Rabf, R_w], writes=[Reqb[s_]])
                        P.op("dve", lambda e, eb=eb, s_=s_, i16v=i16v: e.tensor_tensor(
                            out=eb[:], in0=eb[:],
                            in1=i16v[:, :, s_, :].unsqueeze(2).to_broadcast([128, 8, 16, 16]), op=ALU.mult),
                            reads=[Reqb[s_], Ri16f], writes=[Reqb[s_]])
                        P.op("dve", lambda e, eb=eb, s_=s_: e.tensor_reduce(
                            out=tri[:, s_, :].rearrange("p (h r) -> p h r", h=8), in_=eb[:], axis=AX.X, op=ALU.add),
                            reads=[Reqb[s_]], writes=[Rtri])
                    gv = tri[:, 2, :].rearrange("p (h r) -> p h r", h=8)
                    P.op("dve", lambda e, gv=gv: e.tensor_tensor(
                        out=gv, in0=vs[:], in1=vs[:, :, 0:1].to_broadcast([128, 8, 16]), op=ALU.subtract),
                        reads=Rvs, writes=[Rtri])

            def f1_tail(g, tt):
                t0 = g * TG
                tri, Rtri = tris[2 * (g % 2) + tt], Rtris[2 * (g % 2) + tt]
                if True:
                    gv = tri[:, 2, :].rearrange("p (h r) -> p h r", h=8)
                    P.op("act", lambda e, gv=gv: e.activation(out=gv, in_=gv, func=AF.Exp), reads=[Rtri], writes=[Rtri])
                    P.op("dve", lambda e, gv=gv: e.tensor_reduce(out=gsm[:, 0, :], in_=gv, axis=AX.X, op=ALU.add),
                         reads=[Rtri], writes=[Rgsm])
                    P.op("dve", lambda e: e.reciprocal(out=gsm[:, 1, :], in_=gsm[:, 0, :]), reads=[Rgsm], writes=[Rgsm])
                    P.op("dve", lambda e, gv=gv: e.tensor_tensor(
                        out=gv, in0=gv, in1=gsm[:, 1, :].unsqueeze(2).to_broadcast([128, 8, 16]), op=ALU.mult),
                        reads=[Rtri, Rgsm], writes=[Rtri])
                    for q_ in range(3):
                        P.op("pe", lambda e, q_=q_: e.transpose(out=pstr[:, q_ * 128:(q_ + 1) * 128], in_=tri[:, q_, :],
                                                                identity=ident_f), reads=[Rtri, R_const], writes=[Rpstr])
                    P.op("act", lambda e: e.activation(out=trT[:], in_=pstr[:, 0:384].rearrange("p (a b) -> p a b", a=3),
                                                       func=AF.Copy), reads=[Rpstr], writes=[RtrT])
                    row0 = t0 + tt * 128
                    P.dma("sp", lambda e, row0=row0: e.dma_start(out=IRv[:, :, row0:row0 + 128], in_=trT[:]),
                          reads=[RtrT])

            f1_front(0)
            if NG > 1:
                f1_front(1)
            for g in range(NG):
                for tt in range(ntt):
                    f1_main(g, tt)
                if g + 2 < NG:
                    f1_front(g + 2)
                if g >= 1:
                    for tt in range(ntt):
                        f1_tail(g - 1, tt)
            for tt in range(ntt):
                f1_tail(NG - 1, tt)
            P.emit()
        if stop_after == "F1":
            return nc, None, st0

        with ExitStack() as st:
            GT = sbuf(st, "GT", [128, TG, 128], BF16)
            RGT = [P.res("GT%d" % i) for i in range(128)]
            xn2 = sbuf(st, "f2_xn2", [128, 16, TG], BF16)
            Rxn2 = P.res("f2_xn2")
            trg = sbuf(st, "trg", [128, 3, TG], F32)
            Rtrg = P.res("trg")
            NSUB = 32
            Pb = [sbuf(st, "Pb%d" % i, [128, NSUB, 128], BF16) for i in range(2)]
            Qb = [sbuf(st, "Qb%d" % i, [128, NSUB, 128], BF16) for i in range(2)]
            RPb = [P.res("Pb%d" % i) for i in range(2)]
            RQb = [P.res("Qb%d" % i) for i in range(2)]
            psg = [psum(st, "psg%d" % i, [128, 512], F32) for i in range(2)]
            Rpsg = [P.res("psg%d" % i) for i in range(2)]
            pss = [psum(st, "f2_pss%d" % i, [128, 512], F32) for i in range(2)]
            Rpss = [P.res("f2_pss%d" % i) for i in range(2)]
            pso = [psum(st, "f2_pso%d" % i, [128, 512], F32) for i in range(4)]
            Rpso = [P.res("f2_pso%d" % i) for i in range(4)]
            ub = [sbuf(st, "ub%d" % i, [128, 16, 128], BF16) for i in range(10)]
            Rub = [P.res("ub%d" % i) for i in range(10)]
            vb = [sbuf(st, "vb%d" % i, [128, 2, 1024], BF16) for i in range(6)]
            Rvb = [P.res("vb%d" % i) for i in range(6)]
            gl = [sbuf(st, "gl%d" % i, [128, TG], F32) for i in range(3)]
            Rgl = [P.res("gl%d" % i) for i in range(3)]
            hb = [sbuf(st, "f2_hb%d" % i, [128, 512], F32) for i in range(3)]
            Rhb = [P.res("f2_hb%d" % i) for i in range(3)]
            ob = [sbuf(st, "f2_ob%d" % i, [128, 512], F32) for i in range(3)]
            Rob = [P.res("f2_ob%d" % i) for i in range(3)]
            gcnt = [0]
            scnt = [0]
            ucnt = [0]
            vcnt = [0]
            glc = [0]
            ocnt = [0]
            hcnt = [0]
            sbc = [0]
            for g in range(NG):
                t0 = g * TG
                P.dma("sp", lambda e, g=g: e.dma_start(out=xn2[:], in_=XN2[g].rearrange("p (a b) -> p a b", a=16)),
                      writes=[Rxn2])
                P.dma("sp", lambda e, t0=t0: e.dma_start(out=trg[:], in_=IRv[:, :, t0:t0 + TG]), writes=[Rtrg])
                for sub in range(TG // NSUB):
                    bk = sbc[0] % 2
                    sbc[0] += 1
                    for tl in range(NSUB):
                        t = sub * NSUB + tl
                        P.op("dve", lambda e, bk=bk, tl=tl, t=t: e.tensor_scalar(
                            out=Pb[bk][:, tl, :], in0=iota_bf[:], scalar1=trg[:, 0, t:t + 1], scalar2=trg[:, 2, t:t + 1],
                            op0=ALU.is_equal, op1=ALU.mult), reads=[Rtrg, R_const], writes=[RPb[bk]], indep=True)
                        P.op("dve", lambda e, bk=bk, tl=tl, t=t: e.tensor_scalar(
                            out=Qb[bk][:, tl, :], in0=iota_bf[:], scalar1=trg[:, 1, t:t + 1], scalar2=None,
                            op0=ALU.is_equal), reads=[Rtrg, R_const], writes=[RQb[bk]], indep=True)
                    for q4 in range(NSUB // 4):
                        gk = gcnt[0] % 2
                        gcnt[0] += 1
                        tok0 = sub * NSUB + q4 * 4
                        for u in range(4):
                            tl = q4 * 4 + u
                            P.op("pe", lambda e, gk=gk, u=u, bk=bk, tl=tl: e.matmul(
                                out=psg[gk][:, u * 128:(u + 1) * 128], lhsT=Qb[bk][:, tl, :], rhs=Pb[bk][:, tl, :],
                                start=True, stop=True), reads=[RPb[bk], RQb[bk]], writes=[Rpsg[gk]])
                        P.op("act", lambda e, gk=gk, tok0=tok0: e.activation(
                            out=GT[:, tok0:tok0 + 4, :],
                            in_=psg[gk][:].rearrange("p (t i) -> p t i", t=4), func=AF.Copy),
                            reads=[Rpsg[gk]], writes=RGT, indep=True)
                for i in range(128):
                    uk = ucnt[0] % 10
                    ucnt[0] += 1
                    P.dma("sp", lambda e, uk=uk, i=i: e.dma_start(out=ub[uk][:], in_=UTb[i].rearrange("p (a b) -> p a b", a=16)),
                          writes=[Rub[uk]])
                    sk = scnt[0] % 2
                    scnt[0] += 1
                    ps_ = pss[sk][:, 0:TG]
                    for dc in range(16):
                        P.op("pe", lambda e, ps_=ps_, uk=uk, dc=dc: e.matmul(
                            out=ps_, lhsT=ub[uk][:, dc, :], rhs=xn2[:, dc, :], start=(dc == 0), stop=(dc == 15)),
                            reads=[Rub[uk], Rxn2], writes=[Rpss[sk]])
                    lk = glc[0] % 3
                    glc[0] += 1
                    P.op("act", lambda e, ps_=ps_, lk=lk: e.activation(out=gl[lk][:], in_=ps_, func=AF.Gelu_apprx_tanh),
                         reads=[Rpss[sk]], writes=[Rgl[lk]])
                    P.op("dve", lambda e, lk=lk, i=i: e.tensor_tensor(out=GT[:, :, i], in0=gl[lk][:], in1=GT[:, :, i],
                                                                        op=ALU.mult),
                         reads=[Rgl[lk], RGT[i]], writes=[RGT[i]])
                for nbp in range(2):
                    for i2 in range(64):
                        vk = vcnt[0] % 6
                        vcnt[0] += 1
                        r0 = nbp * 128 + 2 * i2
                        P.dma("sp", lambda e, vk=vk, r0=r0: e.dma_start(
                            out=vb[vk][:], in_=EVb[r0:r0 + 2].rearrange("i e n -> e i n")), writes=[Rvb[vk]])
                        for ii in range(2):
                            i = 2 * i2 + ii
                            for nbl in range(2):
                                for tt in range(ntt):
                                    pk = 2 * nbl + tt
                                    P.op("pe", lambda e, pk=pk, tt=tt, i=i, ii=ii, vk=vk, nbl=nbl: e.matmul(
                                        out=pso[pk][:], lhsT=GT[:, tt * 128:(tt + 1) * 128, i],
                                        rhs=vb[vk][:, ii, nbl * 512:(nbl + 1) * 512],
                                        start=(i == 0), stop=(i == 127)), reads=[RGT[i], Rvb[vk]], writes=[Rpso[pk]])
                    for nbl in range(2):
                        nb = 2 * nbp + nbl
                        for tt in range(ntt):
                            pk = 2 * nbl + tt
                            hk = hcnt[0] % 3
                            hcnt[0] += 1
                            row0 = t0 + tt * 128
                            P.dma("sp", lambda e, hk=hk, row0=row0, nb=nb: e.dma_start(
                                out=hb[hk][:], in_=H1[row0:row0 + 128, nb * 512:(nb + 1) * 512]), writes=[Rhb[hk]])
                            P.op("dve", lambda e, hk=hk, pk=pk: e.tensor_tensor(
                                out=ob[hk][:], in0=pso[pk][:], in1=hb[hk][:], op=ALU.add),
                                reads=[Rpso[pk], Rhb[hk]], writes=[Rob[hk]])
                            P.dma("sp", lambda e, hk=hk, row0=row0, nb=nb: e.dma_start(
                                out=H2[row0:row0 + 128, nb * 512:(nb + 1) * 512], in_=ob[hk][:]), reads=[Rob[hk]])
            P.emit()
        if stop_after == "F2":
            return nc, None, st0

        with ExitStack() as st:
            NTILE = T // 128
            wpp_sb = sbuf(st, "wpp_sb", [128, 2, 2048], BF16)
            R_w = P.res("gw")
            P.dma("pool", lambda e: e.dma_start(out=wpp_sb[:], in_=Wpp.rearrange("p (a b) -> p a b", a=2),
                                                max_dma_last_dim=4096), writes=[R_w])
            ht = [sbuf(st, "g_ht%d" % i, [128, D], F32) for i in range(2)]
            Rht = [P.res("g_ht%d" % i) for i in range(2)]
            rbs = [rms_bufs(st, "g%d" % i) for i in range(2)]
            xn3 = sbuf(st, "xn3", [128, 16, T], BF16)
            Rxn3 = [P.res("xn3_%d" % i) for i in range(NTILE)]
            pt_f = sbuf(st, "pt_f", [128, 256], F32)
            pt_b = sbuf(st, "pt_b", [128, 256], BF16)
            Rptf, Rptb = P.res("pt_f"), P.res("pt_b")
            pTb = sbuf(st, "pTb", [128, 2, T], BF16)
            RpTb = [P.res("pTb%d" % i) for i in range(NTILE)]
            pstp = psum(st, "g_pstp", [128, 1024], BF16)
            Rpstp = P.res("g_pstp")
            wgb = [sbuf(st, "wgb%d" % i, [128, 16, 512], BF16) for i in range(3)]
            Rwgb = [P.res("wgb%d" % i) for i in range(3)]
            psgt = [psum(st, "g_psg%d" % i, [128, 512], F32) for i in range(2)]
            Rpsgt = [P.res("g_psg%d" % i) for i in range(2)]
            pspp = psum(st, "g_psp", [128, 512], F32)
            Rpspp = P.res("g_psp")
            sg = [sbuf(st, "sg%d" % i, [128, 512], F32) for i in range(2)]
            Rsg = [P.res("sg%d" % i) for i in range(2)]
            hb = [sbuf(st, "g_hb%d" % i, [128, 512], F32) for i in range(3)]
            Rhb = [P.res("g_hb%d" % i) for i in range(3)]
            ob = [sbuf(st, "g_ob%d" % i, [128, 512], F32) for i in range(3)]
            Rob = [P.res("g_ob%d" % i) for i in range(3)]
            kcnt = [0]
            ocnt = [0]

            def g_front2(t_first):
                items = []
                for t in (t_first, t_first + 1):
                    k = t % 2
                    row0 = t * 128
                    P.dma("sp", lambda e, k=k, row0=row0: e.dma_start(out=ht[k][:], in_=H2[row0:row0 + 128, :]),
                          writes=[Rht[k]])
                    items.append((ht[k][:], Rht[k], (lambda dc, row0=row0: xn3[:, dc, row0:row0 + 128]), Rxn3[t], rbs[k]))
                rmsnorm_multi(items, 32)
                for t in (t_first, t_first + 1):
                    row0 = t * 128
                    P.dma("sp", lambda e, row0=row0: e.dma_start(out=pt_f[:], in_=pin[row0:row0 + 128, :]), writes=[Rptf])
                    P.op("act", lambda e: e.activation(out=pt_b[:], in_=pt_f[:], func=AF.Copy), reads=[Rptf], writes=[Rptb])
                    for fc in range(2):
                        P.op("pe", lambda e, fc=fc: e.transpose(out=pstp[:, fc * 128:(fc + 1) * 128],
                                                                in_=pt_b[:, fc * 128:(fc + 1) * 128], identity=ident_bf[:]),
                             reads=[Rptb, R_const], writes=[Rpstp])
                    P.op("dve", lambda e, row0=row0: e.tensor_copy(
                        out=pTb[:, :, row0:row0 + 128], in_=pstp[:, 0:256].rearrange("p (a b) -> p a b", a=2)),
                        reads=[Rpstp], writes=[RpTb[t]])

            def g_back(t, nb, wk):
                row0 = t * 128
                kk = kcnt[0] % 2
                kcnt[0] += 1
                okk = ocnt[0] % 3
                ocnt[0] += 1
                P.dma("sp", lambda e: e.dma_start(out=hb[okk][:], in_=H2[row0:row0 + 128, nb * 512:(nb + 1) * 512]),
                      writes=[Rhb[okk]])
                for fc in range(16):
                    P.op("pe", lambda e, fc=fc: e.matmul(
                        out=psgt[kk][:], lhsT=xn3[:, fc, row0:row0 + 128], rhs=wgb[wk][:, fc, :],
                        start=(fc == 0), stop=(fc == 15)), reads=[Rxn3[t], Rwgb[wk]], writes=[Rpsgt[kk]])
                for fc in range(2):
                    P.op("pe", lambda e, fc=fc: e.matmul(
                        out=pspp[:], lhsT=pTb[:, fc, row0:row0 + 128], rhs=wpp_sb[:, fc, nb * 512:(nb + 1) * 512],
                        start=(fc == 0), stop=(fc == 1)), reads=[RpTb[t], R_w], writes=[Rpspp])
                P.op("act", lambda e: e.activation(out=sg[kk][:], in_=psgt[kk][:], func=AF.Sigmoid),
                     reads=[Rpsgt[kk]], writes=[Rsg[kk]])
                P.op("dve", lambda e: e.tensor_tensor(out=sg[kk][:], in0=sg[kk][:], in1=pspp[:], op=ALU.mult),
                     reads=[Rsg[kk], Rpspp], writes=[Rsg[kk]])
                P.op("dve", lambda e: e.tensor_tensor(out=ob[okk][:], in0=sg[kk][:], in1=hb[okk][:], op=ALU.add),
                     reads=[Rsg[kk], Rhb[okk]], writes=[Rob[okk]])
                P.dma("sp", lambda e: e.dma_start(out=out[row0:row0 + 128, nb * 512:(nb + 1) * 512], in_=ob[okk][:]),
                      reads=[Rob[okk]])

            def load_w(nb):
                wk = nb % 3
                P.dma("sp", lambda e: e.dma_start(out=wgb[wk][:], in_=Wgb[nb].rearrange("p (a b) -> p a b", a=16)),
                      reads=[RWg[nb]], writes=[Rwgb[wk]])

            load_w(0)
            g_front2(0)
            load_w(1)
            load_w(2)
            for t in range(0, NTILE, 2):
                if t + 2 < NTILE:
                    g_front2(t + 2)
                g_back(t, 0, 0)
                g_back(t + 1, 0, 0)
            for nb in range(1, 4):
                if nb + 2 < 4:
                    load_w(nb + 2)
                for t in range(NTILE):
                    g_back(t, nb, nb % 3)
            P.emit()
        return nc, None, st0


def _host_layout(inp, b):
    f = np.float32
    d = {}
    d["x"] = np.ascontiguousarray(inp["x"][b], f)
    d["p"] = np.ascontiguousarray(inp["p"][0, b], f)
    d["pos"] = np.ascontiguousarray(inp["positions"][b].reshape(1, T).astype(np.int32))
    return d


_SHARED = {}


def _shared_layout(inp):
    f = np.float32
    s = {}
    cst = np.zeros((128, NCST), f)

    def colmajor(v, n):
        return np.asarray(v, f).reshape(n, 128).T

    cst[:, 0:16] = colmajor(inp["mix_norm_gain"][0], 16)
    cst[:, 16:32] = colmajor(inp["ffn_norm_gain"][0], 16)
    cst[:, 32:48] = colmajor(inp["ple_norm_gain"][0], 16)
    cst[:, 48:52] = colmajor(inp["q_lat_gain"][0], 4)
    cst[:, 52:56] = colmajor(inp["kv_lat_gain"][0], 4)
    cst[:, 56:64] = colmajor(inp["pool_scale"][0], 8)
    qg = np.asarray(inp["q_norm_gain"][0], f)
    kg = np.asarray(inp["k_norm_gain"][0], f)
    cst[:, 64] = qg[0:128]
    cst[:, 65] = np.tile(qg[128:192], 2)
    cst[:, 66] = kg[0:128]
    cst[:, 67] = np.tile(kg[128:192], 2)
    inv_freq = (np.float32(10000.0) ** (-np.arange(0, 64, 2, dtype=np.float32) / np.float32(64))).astype(f)
    cst[:, 68] = np.tile(inv_freq, 4)
    cst[:, 69] = np.tile(np.concatenate([-np.ones(32, f), np.ones(32, f)]), 2)
    cst[:, 70:86] = (1.0 / np.arange(1, 17, dtype=f))[None, :]
    cst[:, 86] = EPS
    s["cst"] = cst
    mats = np.zeros((128, 6, 128), f)
    mats[:, 0, :] = np.eye(128, dtype=f)
    mats[:, 1, :] = 1.0
    mats[0:64, 2, :] = 1.0
    mats[64:128, 3, :] = 1.0
    for m in range(128):
        partner = m + 32 if (m % 64) < 32 else m - 32
        mats[partner, 4, m] = 1.0
    mats[:, 5, :] = np.arange(128, dtype=f)[None, :]
    s["mats"] = mats.reshape(128, 768)
    ntt = TG // 128
    mk = np.zeros((128, ntt, TG), f)
    kk = np.arange(128)[:, None]
    qq = np.arange(TG)[None, :]
    for j in range(ntt):
        mk[:, j, :] = ((qq // 64) >= ((128 * j + kk) // 64)).astype(f)
    s["maskc"] = mk.reshape(128, ntt * TG)
    w_in = np.asarray(inp["w_in"][0], f)
    w_ext = np.concatenate([w_in, w_in[:, 2048:2112]], axis=1)
    s["Win"] = np.ascontiguousarray(w_ext.reshape(16, 128, 17, 128).transpose(2, 1, 0, 3)).reshape(17, 128, 2048)
    wp = np.asarray(inp["w_pool"][0], f)
    s["Wpool"] = np.ascontiguousarray(wp.reshape(4, 2, 128, 256).transpose(2, 0, 1, 3)).reshape(128, 2048)
    wuq = np.asarray(inp["w_uq"][0], f).reshape(512, 8, 192)
    wuq_r = np.concatenate([wuq[:, :, 0:128].reshape(512, 1024), wuq[:, :, 128:192].reshape(512, 512)], axis=1)
    s["Wuq"] = np.ascontiguousarray(wuq_r.reshape(4, 128, 1536).transpose(1, 0, 2)).reshape(128, 4 * 1536)
    wukv = np.asarray(inp["w_ukv"][0], f).reshape(512, 8, 256)
    wuk = wukv[:, :, 0:128].reshape(512, 1024)
    wv = wukv[:, :, 128:256].reshape(512, 1024)
    s["Wuk"] = np.ascontiguousarray(wuk.reshape(4, 128, 1024).transpose(1, 0, 2)).reshape(128, 4096)
    s["Wv"] = np.ascontiguousarray(wv.reshape(4, 128, 1024).transpose(1, 0, 2)).reshape(128, 4096)
    wo = np.asarray(inp["w_out"][0], f)
    s["Wout"] = np.ascontiguousarray(wo.reshape(16, 128, 4, 512).transpose(2, 1, 0, 3)).reshape(4, 128, 8192)
    wpq = np.asarray(inp["w_pq"][0], f)
    s["Wpq"] = np.ascontiguousarray(wpq.reshape(16, 128, 16, 128).transpose(2, 1, 0, 3)).reshape(16, 128, 2048)
    sk = np.stack([np.asarray(inp["sub_k1"][0], f), np.asarray(inp["sub_k2"][0], f)], axis=1)
    s["subk"] = np.ascontiguousarray(sk.transpose(3, 0, 1, 2)).reshape(128, 2048)
    eu = np.asarray(inp["expert_u"][0], f)
    s["UT"] = np.ascontiguousarray(eu.reshape(128, 128, 16, 128).transpose(0, 3, 2, 1)).reshape(128, 128, 2048)
    ev = np.asarray(inp["expert_v"][0], f)
    evl = np.ascontiguousarray(ev.reshape(128, 128, 2, 1024).transpose(2, 0, 1, 3))
    s["EV"] = evl.reshape(256, 128, 1024)
    wg = np.asarray(inp["w_ple_gate"][0], f)
    s["Wg"] = np.ascontiguousarray(wg.reshape(16, 128, 4, 512).transpose(2, 1, 0, 3)).reshape(4, 128, 8192)
    wpp = np.asarray(inp["w_ple_proj"][0], f)
    s["Wpp"] = np.ascontiguousarray(wpp.reshape(2, 128, 2048).transpose(1, 0, 2)).reshape(128, 4096)
    return s


def kernel(**inputs):
    shared = _shared_layout(inputs)
    nc, _, _ = build()
    in_maps = []
    for b in range(8):
        m = dict(shared)
        m.update(_host_layout(inputs, b))
        in_maps.append(m)
    res = run_bass_kernel_spmd(nc, in_maps, core_ids=list(range(8)))
    return np.stack([r["out"] for r in res.results], axis=0).astype(np.float32)
```

```python
import math
from contextlib import ExitStack

import numpy as np
import concourse.bass as bass
import concourse.mybir as mybir
from concourse.bass_utils import run_bass_kernel_spmd

F32 = mybir.dt.float32
BF16 = mybir.dt.bfloat16
I32 = mybir.dt.int32
U32 = mybir.dt.uint32
AF = mybir.ActivationFunctionType
ALU = mybir.AluOpType
AX = mybir.AxisListType

T = 2048
D = 2048
EPS = 1e-6
TG = 256
NG = T // TG
NCST = 88
DBG_CUT = 99
TWO_PI = 2.0 * math.pi
CW1 = 6.28125
_c2 = np.array([TWO_PI - CW1], np.float32).view(np.uint32) & np.uint32(0xFFFFF000)
CW2 = float(_c2.view(np.float32)[0])
CW3 = float(TWO_PI - CW1 - CW2)


class Res:
    __slots__ = ("name", "w", "rd")

    def __init__(self, name):
        self.name = name
        self.w = None
        self.rd = []


class Op:
    __slots__ = ("eng", "fn", "deps", "signal", "sigval", "isdma", "sem", "semval", "prev")


class Prog:
    ENG = {"pe": "tensor", "act": "scalar", "dve": "vector", "pool": "gpsimd", "sp": "sync"}

    def __init__(self, nc, st, ring=8):
        self.nc = nc
        self.sems = {e: st.enter_context(nc.semaphore("s_" + e)) for e in self.ENG}
        self.cnt = {e: 0 for e in self.ENG}
        self.ring = ring
        self.rings = {q: [st.enter_context(nc.semaphore("d_%s%d" % (q, i))) for i in range(ring)]
                      for q in ("sp", "pool", "act")}
        self.ringcnt = {q: [0] * ring for q in self.rings}
        self.ringpos = {q: 0 for q in self.rings}
        self.ops = []
        self.waited = {}
        self.allres = []

    def res(self, name):
        r = Res(name)
        self.allres.append(r)
        return r

    def _add(self, eng, fn, reads, writes, isdma, indep):
        o = Op()
        o.eng = eng
        o.fn = fn
        o.isdma = isdma
        o.signal = False
        o.sigval = None
        deps = []
        for r in reads:
            if r.w is not None:
                deps.append(r.w)
        for w in writes:
            if w.w is not None:
                deps.append(w.w)
            deps.extend(w.rd)
        seen = set()
        out = []
        for d in deps:
            if id(d) in seen:
                continue
            seen.add(id(d))
            if (not d.isdma) and (not isdma) and d.eng == eng and (eng == "pe" or indep):
                continue
            out.append(d)
            if not d.isdma:
                d.signal = True
        o.deps = out
        if isdma:
            q = eng
            slot = self.ringpos[q] % self.ring
            self.ringpos[q] += 1
            o.sem = self.rings[q][slot]
            o.prev = self.ringcnt[q][slot]
            self.ringcnt[q][slot] += 16
            o.semval = self.ringcnt[q][slot]
        for w in writes:
            w.w = o
            w.rd = []
        for r in reads:
            if r in writes:
                continue
            if not isdma:
                r.rd = [x for x in r.rd if x.isdma or x.eng != eng]
            r.rd.append(o)
        self.ops.append(o)
        return o

    _defer = None

    def defer_begin(self):
        self._defer = []

    def defer_end(self):
        d = self._defer
        self._defer = None
        return d

    def op(self, eng, fn, reads=(), writes=(), indep=False):
        if self._defer is not None:
            self._defer.append(lambda: self._add(eng, fn, list(reads), list(writes), False, indep))
            return None
        return self._add(eng, fn, list(reads), list(writes), False, indep)

    def dma(self, q, fn, reads=(), writes=()):
        if self._defer is not None:
            self._defer.append(lambda: self._add(q, fn, list(reads), list(writes), True, False))
            return None
        return self._add(q, fn, list(reads), list(writes), True, False)

    def _wait(self, engobj, e, sem, key, val):
        if val <= 0:
            return
        k = (e, key)
        if self.waited.get(k, 0) >= val:
            return
        self.waited[k] = val
        engobj.wait_ge(sem, val)

    def emit(self):
        ops = self.ops
        self.ops = []
        for o in ops:
            if (not o.isdma) and o.signal:
                self.cnt[o.eng] += 1
                o.sigval = self.cnt[o.eng]
        with self.nc.Block() as block:
            for e, attr in self.ENG.items():
                mine = [o for o in ops if o.eng == e]
                if not mine:
                    continue

                def body(engobj, mine=mine, e=e):
                    for o in mine:
                        for d in o.deps:
                            if d.isdma:
                                self._wait(engobj, e, d.sem, id(d.sem), d.semval)
                            else:
                                self._wait(engobj, e, self.sems[d.eng], d.eng, d.sigval)
                        if o.isdma:
                            self._wait(engobj, e, o.sem, id(o.sem), o.prev)
                            o.fn(engobj).then_inc(o.sem, 16)
                        else:
                            ins = o.fn(engobj)
                            if o.signal:
                                ins.then_inc(self.sems[e], 1)
                    if e in self.rings:
                        for i, s in enumerate(self.rings[e]):
                            self._wait(engobj, e, s, id(s), self.ringcnt[e][i])

                getattr(block, attr)(body)
        for r in self.allres:
            r.w = None
            r.rd = []


def build(stop_after=None, dbg=()):
    nc = bass.Bass("TRN2", target_bir_lowering=False)

    def din(name, shape, dt=F32):
        return nc.dram_tensor(name, list(shape), dt, kind="ExternalInput").ap()

    def dscr(name, shape, dt):
        kind = "ExternalOutput" if name in dbg else "Internal"
        return nc.dram_tensor(name, list(shape), dt, kind=kind).ap()

    x = din("x", [T, D])
    pin = din("p", [T, 256])
    pos = din("pos", [1, T], I32)
    cst = din("cst", [128, NCST])
    mats = din("mats", [128, 6 * 128])
    maskc = din("maskc", [128, 2 * TG])
    Win = din("Win", [17, 128, 2048])
    Wpool = din("Wpool", [128, 2048])
    Wuq = din("Wuq", [128, 4 * 1536])
    Wuk = din("Wuk", [128, 4096])
    Wv = din("Wv", [128, 4096])
    Wout = din("Wout", [4, 128, 8192])
    Wpq = din("Wpq", [16, 128, 2048])
    subk = din("subk", [128, 2048])
    UT = din("UT", [128, 128, 2048])
    EV = din("EV", [256, 128, 1024])
    Wg = din("Wg", [4, 128, 8192])
    Wpp = din("Wpp", [128, 4096])
    out = nc.dram_tensor("out", [T, D], F32, kind="ExternalOutput").ap()

    Winb = dscr("Winb", [17, 128, 2048], BF16)
    Woutb = dscr("Woutb", [4, 128, 8192], BF16)
    Wpqb = dscr("Wpqb", [16, 128, 2048], BF16)
    Wgb = dscr("Wgb", [4, 128, 8192], BF16)
    UTb = dscr("UTb", [128, 128, 2048], BF16)
    EVb = dscr("EVb", [256, 128, 1024], BF16)
    CQ = dscr("CQ", [NG, 128, 4 * TG], BF16)
    YP = dscr("YP", [NG, 128, 8 * TG], BF16)
    H1 = dscr("H1", [T, D], F32)
    H2 = dscr("H2", [T, D], F32)
    XN2 = dscr("XN2", [NG, 128, 16 * TG], BF16)
    IRs = dscr("IRs", [3, 128, T], F32)

    st0 = ExitStack()
    with st0:
        P = Prog(nc, st0)

        def sbuf(st, name, shape, dt):
            return st.enter_context(nc.sbuf_tensor(name, list(shape), dt))

        def psum(st, name, shape, dt=F32):
            return st.enter_context(nc.psum_tensor(name, list(shape), dt))

        cst_sb = sbuf(st0, "cst_sb", [128, NCST], F32)
        mats_sb = sbuf(st0, "mats_sb", [128, 6 * 128], F32)
        ident_bf = sbuf(st0, "ident_bf", [128, 128], BF16)
        ones_bf = sbuf(st0, "ones_bf", [128, 128], BF16)
        iota_bf = sbuf(st0, "iota_bf", [128, 128], BF16)
        stKV = ExitStack()
        cosT = sbuf(stKV, "cosT", [128, T], F32)
        sinT = sbuf(stKV, "sinT", [128, T], F32)
        R_const = P.res("const")
        R_rope = P.res("rope")
        ident_f = mats_sb[:, 0:128]
        ones_f = mats_sb[:, 128:256]
        Llo_f = mats_sb[:, 256:384]
        Lhi_f = mats_sb[:, 384:512]
        perm_f = mats_sb[:, 512:640]
        iota_f = mats_sb[:, 640:768]

        RWin = [P.res("Win%d" % c) for c in range(17)]
        RWout = [P.res("Wout%d" % c) for c in range(4)]
        RWpq = [P.res("Wpq%d" % c) for c in range(16)]
        RWg = [P.res("Wg%d" % c) for c in range(4)]
        pro_list = []
        for c in range(17):
            pro_list.append((Winb[c], Win[c], RWin[c]))
        for nb in range(4):
            for q in range(4):
                pro_list.append((Woutb[nb, 32 * q:32 * q + 32, :], Wout[nb, 32 * q:32 * q + 32, :], RWout[nb]))
        for c in range(16):
            pro_list.append((Wpqb[c], Wpq[c], RWpq[c]))
        for nb in range(4):
            for q in range(4):
                pro_list.append((Wgb[nb, 32 * q:32 * q + 32, :], Wg[nb, 32 * q:32 * q + 32, :], RWg[nb]))
        for i in range(128):
            pro_list.append((UTb[i], UT[i], None))
            pro_list.append((EVb[2 * i:2 * i + 2], EV[2 * i:2 * i + 2], None))
        pro_state = {"i": 0}

        def pro_some(n):
            for _ in range(n):
                if pro_state["i"] >= len(pro_list):
                    return
                o_, i_, r_ = pro_list[pro_state["i"]]
                pro_state["i"] += 1
                P.dma("pool", lambda e, o_=o_, i_=i_: e.dma_start(out=o_, in_=i_, max_dma_last_dim=4096),
                      writes=([r_] if r_ is not None else []))

        with ExitStack() as st:
            posi = sbuf(st, "posi", [128, T], I32)
            ang = sbuf(st, "ang", [128, T], F32)
            kf = sbuf(st, "kf", [128, T], F32)
            ki = sbuf(st, "ki", [128, T], I32)
            rr = sbuf(st, "rr", [128, T], F32)
            r2 = sbuf(st, "r2", [128, T], F32)
            Rp, Ra, Rk, Rki, Rr, Rr2 = [P.res(n) for n in ("posi", "ang", "kf", "ki", "rr", "r2")]
            P.dma("sp", lambda e: e.dma_start(out=cst_sb[:], in_=cst), writes=[R_const])
            P.dma("sp", lambda e: e.dma_start(out=mats_sb[:], in_=mats), writes=[R_const])
            P.dma("sp", lambda e: e.dma_start(out=posi[:], in_=pos.to_broadcast([128, T])), writes=[Rp])
            P.op("dve", lambda e: e.tensor_copy(out=ident_bf[:], in_=ident_f), reads=[R_const], writes=[R_const])
            P.op("dve", lambda e: e.tensor_copy(out=ones_bf[:], in_=ones_f), reads=[R_const], writes=[R_const])
            P.op("dve", lambda e: e.tensor_copy(out=iota_bf[:], in_=iota_f), reads=[R_const], writes=[R_const])
            P.op("dve", lambda e: e.tensor_copy(out=ang[:], in_=posi[:]), reads=[Rp], writes=[Ra])
            P.op("dve", lambda e: e.tensor_scalar(out=ang[:], in0=ang[:], scalar1=cst_sb[:, 68:69], scalar2=None,
                                                  op0=ALU.mult), reads=[Ra, R_const], writes=[Ra])
            P.op("dve", lambda e: e.tensor_scalar(out=kf[:], in0=ang[:], scalar1=1.0 / TWO_PI, scalar2=None,
                                                  op0=ALU.mult), reads=[Ra], writes=[Rk])
            P.op("dve", lambda e: e.tensor_copy(out=ki[:], in_=kf[:]), reads=[Rk], writes=[Rki])
            P.op("dve", lambda e: e.tensor_copy(out=kf[:], in_=ki[:]), reads=[Rki], writes=[Rk])
            P.op("dve", lambda e: e.scalar_tensor_tensor(out=rr[:], in0=kf[:], scalar=-CW1, in1=ang[:],
                                                         op0=ALU.mult, op1=ALU.add), reads=[Rk, Ra], writes=[Rr])
            P.op("dve", lambda e: e.scalar_tensor_tensor(out=r2[:], in0=kf[:], scalar=-CW2, in1=rr[:],
                                                         op0=ALU.mult, op1=ALU.add), reads=[Rk, Rr], writes=[Rr2])
            P.op("dve", lambda e: e.scalar_tensor_tensor(out=rr[:], in0=kf[:], scalar=-CW3, in1=r2[:],
                                                         op0=ALU.mult, op1=ALU.add), reads=[Rk, Rr2], writes=[Rr])
            def wrap_sin(dst, shift, sign_col):
                P.op("dve", lambda e: e.tensor_scalar(out=r2[:], in0=rr[:], scalar1=shift, scalar2=None, op0=ALU.add),
                     reads=[Rr], writes=[Rr2])
                P.op("dve", lambda e: e.tensor_scalar(out=kf[:], in0=r2[:], scalar1=math.pi, scalar2=-TWO_PI,
                                                      op0=ALU.is_gt, op1=ALU.mult), reads=[Rr2], writes=[Rk])
                P.op("dve", lambda e: e.tensor_tensor(out=r2[:], in0=r2[:], in1=kf[:], op=ALU.add),
                     reads=[Rr2, Rk], writes=[Rr2])
                P.op("dve", lambda e: e.tensor_scalar(out=kf[:], in0=r2[:], scalar1=-math.pi, scalar2=TWO_PI,
                                                      op0=ALU.is_lt, op1=ALU.mult), reads=[Rr2], writes=[Rk])
                P.op("dve", lambda e: e.tensor_tensor(out=r2[:], in0=r2[:], in1=kf[:], op=ALU.add),
                     reads=[Rr2, Rk], writes=[Rr2])
                P.op("dve", lambda e: e.tensor_scalar(out=r2[:], in0=r2[:], scalar1=math.pi, scalar2=-math.pi,
                                                      op0=ALU.min, op1=ALU.max), reads=[Rr2], writes=[Rr2])
                P.op("act", lambda e: e.activation(out=dst[:], in_=r2[:], func=AF.Sin), reads=[Rr2], writes=[R_rope])
                if sign_col is not None:
                    P.op("dve", lambda e: e.tensor_scalar(out=dst[:], in0=dst[:], scalar1=cst_sb[:, sign_col:sign_col + 1],
                                                          scalar2=None, op0=ALU.mult),
                         reads=[R_rope, R_const], writes=[R_rope])

            wrap_sin(sinT, 0.0, 69)
            wrap_sin(cosT, math.pi / 2, None)
            pro_some(17)
            P.emit()

        def rmsnorm_multi(items, g0, all_act=False):
            for (src, Rsrc, dstfn, Rdst, bufs) in items:
                xs, Rxs, ss, ms, Rst, pst, Rpst = bufs
                P.op("act", lambda e, xs=xs, src=src, ss=ss: e.activation(out=xs[:], in_=src, func=AF.Square,
                                                                          accum_out=ss[:, 0:1]),
                     reads=[Rsrc], writes=[Rxs, Rst])
            for (src, Rsrc, dstfn, Rdst, bufs) in items:
                xs, Rxs, ss, ms, Rst, pst, Rpst = bufs
                P.op("act", lambda e, ss=ss, ms=ms: e.activation(out=ms[:, 0:1], in_=ss[:, 0:1], func=AF.Ln,
                                                                 scale=1.0 / D, bias=cst_sb[:, 86:87]),
                     reads=[Rst, R_const], writes=[Rst])
            for (src, Rsrc, dstfn, Rdst, bufs) in items:
                xs, Rxs, ss, ms, Rst, pst, Rpst = bufs
                P.op("act", lambda e, ms=ms: e.activation(out=ms[:, 2:3], in_=ms[:, 0:1], func=AF.Exp, scale=-0.5),
                     reads=[Rst], writes=[Rst])
            for (src, Rsrc, dstfn, Rdst, bufs) in items:
                xs, Rxs, ss, ms, Rst, pst, Rpst = bufs
                P.op("act", lambda e, xs=xs, src=src, ms=ms: e.activation(out=xs[:], in_=src, func=AF.Copy,
                                                                          scale=ms[:, 2:3]),
                     reads=[Rsrc, Rst], writes=[Rxs])
            for half in range(2):
                for (src, Rsrc, dstfn, Rdst, bufs) in items:
                    xs, Rxs, ss, ms, Rst, pst, Rpst = bufs
                    for j in range(8):
                        dc = half * 8 + j
                        P.op("pe", lambda e, dc=dc, j=j, half=half, pst=pst, xs=xs: e.transpose(
                            out=pst[half][:, j * 128:(j + 1) * 128], in_=xs[:, dc * 128:(dc + 1) * 128],
                            identity=ident_bf[:]), reads=[Rxs, R_const], writes=[Rpst[half]])
                for (src, Rsrc, dstfn, Rdst, bufs) in items:
                    xs, Rxs, ss, ms, Rst, pst, Rpst = bufs
                    for j in range(8):
                        dc = half * 8 + j
                        if j % 2 == 0 and not all_act:
                            P.op("dve", lambda e, dc=dc, j=j, half=half, pst=pst, dstfn=dstfn: e.tensor_scalar(
                                out=dstfn(dc), in0=pst[half][:, j * 128:(j + 1) * 128],
                                scalar1=cst_sb[:, g0 + dc:g0 + dc + 1], scalar2=None, op0=ALU.mult),
                                reads=[Rpst[half], R_const], writes=[Rdst], indep=True)
                        else:
                            P.op("act", lambda e, dc=dc, j=j, half=half, pst=pst, dstfn=dstfn: e.activation(
                                out=dstfn(dc), in_=pst[half][:, j * 128:(j + 1) * 128], func=AF.Copy,
                                scale=cst_sb[:, g0 + dc:g0 + dc + 1]),
                                reads=[Rpst[half], R_const], writes=[Rdst], indep=True)

        def rmsnorm_T(src, Rsrc, g0, dstfn, Rdst, bufs, tag):
            rmsnorm_multi([(src, Rsrc, dstfn, Rdst, bufs)], g0)

        def rms_bufs(st, tag):
            xs = sbuf(st, "rn_xs" + tag, [128, D], BF16)
            ss = sbuf(st, "rn_ss" + tag, [128, 1], F32)
            ms = sbuf(st, "rn_ms" + tag, [128, 4], F32)
            pst = [psum(st, "rn_ps%d%s" % (h, tag), [128, 1024], BF16) for h in range(2)]
            return (xs, P.res("rn_xs" + tag), ss, ms, P.res("rn_st" + tag), pst,
                    [P.res("rn_ps0" + tag), P.res("rn_ps1" + tag)])

        KT = sbuf(stKV, "KT", [128, 8, T], BF16)
        KRT = sbuf(stKV, "KRT", [128, T], BF16)
        Vsb = sbuf(stKV, "Vsb", [128, 16, 1024], BF16)
        rkcol = sbuf(stKV, "rkcol", [128, 16, 8], F32)
        R_K = [P.res("K%d" % g) for g in range(NG)]
        R_V = [P.res("V%d" % g) for g in range(NG)]

        with ExitStack() as st:
            wuk_sb = sbuf(st, "wuk_sb", [128, 4, 1024], BF16)
            wv_sb = sbuf(st, "wv_sb", [128, 4, 1024], BF16)
            wpool_sb = sbuf(st, "wpool_sb", [128, 4, 2, 256], BF16)
            R_w = P.res("m1w")
            P.dma("pool", lambda e: e.dma_start(out=wuk_sb[:], in_=Wuk.rearrange("p (a b) -> p a b", a=4),
                                                max_dma_last_dim=4096), writes=[R_w])
            P.dma("pool", lambda e: e.dma_start(out=wv_sb[:], in_=Wv.rearrange("p (a b) -> p a b", a=4),
                                                max_dma_last_dim=4096), writes=[R_w])
            P.dma("pool", lambda e: e.dma_start(out=wpool_sb[:], in_=Wpool.rearrange("p (a b c) -> p a b c", a=4, b=2),
                                                max_dma_last_dim=1024), writes=[R_w])
            xt = [sbuf(st, "m1_xt%d" % i, [128, D], F32) for i in range(2)]
            Rxt = [P.res("m1_xt%d" % i) for i in range(2)]
            rbs = [rms_bufs(st, "m1_%d" % i) for i in range(2)]
            xnT = sbuf(st, "xnT", [128, 16, TG], BF16)
            RxnT = P.res("xnT")
            wbuf = [sbuf(st, "m1_wb%d" % i, [128, 16, 128], BF16) for i in range(6)]
            Rwb = [P.res("m1_wb%d" % i) for i in range(6)]
            psz = [psum(st, "psz%d" % i, [128, 512], F32) for i in range(2)]
            Rpsz = [P.res("psz%d" % i) for i in range(2)]
            pz_ring = [psz[0][:, 0:TG], psz[1][:, 0:TG]]
            Rpz_ring = [Rpsz[0], Rpsz[1]]
            for k_ in range(2):
                for h_ in range(2):
                    pz_ring.append(rbs[k_][5][h_][:].bitcast(F32)[:, 0:TG])
                    Rpz_ring.append(rbs[k_][6][h_])
            psm = [psum(st, "psm%d" % i, [128, 512], F32) for i in range(2)]
            Rpsm = [P.res("psm%d" % i) for i in range(2)]
            halo = sbuf(st, "halo", [128, 8, 16], F32)
            Rhalo = [P.res("halo%d" % c) for c in range(8)]
            zpad = [sbuf(st, "zpad%d" % i, [128, 16 + TG], F32) for i in range(2)]
            Rzp = [P.res("zpad%d" % i) for i in range(2)]
            abuf = [sbuf(st, "abuf%d" % i, [128, 16 + TG], F32) for i in range(2)]
            Rab = [P.res("abuf%d" % i) for i in range(2)]
            fix16 = sbuf(st, "fix16", [128, 16], F32)
            Rfix = P.res("fix16")
            dbuf = sbuf(st, "dbuf", [128, 2, TG], BF16)
            Rdb = [P.res("dbuf%d" % i) for i in range(2)]
            ypo = sbuf(st, "ypo", [128, 8, TG], BF16)
            Rypo = P.res("ypo")
            zlat = sbuf(st, "zlat", [128, 4, TG], F32)
            Rzlat = P.res("zlat")
            sq = [sbuf(st, "m1_sq%d" % i, [128, TG], F32) for i in range(2)]
            Rsq = [P.res("m1_sq%d" % i) for i in range(2)]
            rlat = sbuf(st, "rlat", [128, TG], F32)
            Rrlat = P.res("rlat")
            cqn = sbuf(st, "cqn", [128, 4, TG], BF16)
            Rcqn = P.res("cqn")
            ckvn = sbuf(st, "ckvn", [128, 4, TG], BF16)
            Rckvn = P.res("ckvn")
            krg = sbuf(st, "krg", [128, TG], F32)
            krsq = sbuf(st, "krsq", [128, TG], F32)
            Rkr = P.res("kr")
            t1 = sbuf(st, "m1_t1", [128, TG], F32)
            t2 = sbuf(st, "m1_t2", [128, TG], F32)
            Rt1, Rt2 = P.res("m1_t1"), P.res("m1_t2")
            colb = sbuf(st, "colb", [128, 3, 16], F32)
            Rcolb = P.res("colb")

            P.op("dve", lambda e: e.memset(halo[:], 0.0), writes=Rhalo)
            dq = []

            def dq_tick():
                for it in dq:
                    it[0] -= 1
                while dq and dq[0][0] <= 0:
                    for f_ in dq.pop(0)[1]:
                        f_()

            def dq_flush():
                while dq:
                    for f_ in dq.pop(0)[1]:
                        f_()

            wcnt = [0]
            pcnt = [0]
            mcnt = [0]

            for g in range(NG):
                t0 = g * TG
                items = []
                for tt in range(TG // 128):
                    k = tt % 2
                    row0 = t0 + tt * 128
                    P.dma("sp", lambda e, k=k, row0=row0: e.dma_start(out=xt[k][:], in_=x[row0:row0 + 128, :]),
                          writes=[Rxt[k]])
                    items.append((xt[k][:], Rxt[k], (lambda dc, tt=tt: xnT[:, dc, tt * 128:(tt + 1) * 128]), RxnT, rbs[k]))
                rmsnorm_multi(items, 0)
                pro_some(8)
                for c in range(17):
                    wk = wcnt[0] % 6
                    wcnt[0] += 1
                    P.dma("sp", lambda e, wk=wk, c=c: e.dma_start(
                        out=wbuf[wk][:], in_=Winb[c].rearrange("p (a b) -> p a b", a=16)),
                        reads=[RWin[c]], writes=[Rwb[wk]])
                    pk = pcnt[0] % len(pz_ring)
                    pcnt[0] += 1
                    pz = pz_ring[pk]
                    for dc in range(16):
                        P.op("pe", lambda e, wk=wk, dc=dc, pz=pz: e.matmul(
                            out=pz, lhsT=wbuf[wk][:, dc, :], rhs=xnT[:, dc, :], start=(dc == 0), stop=(dc == 15)),
                            reads=[Rwb[wk], RxnT], writes=[Rpz_ring[pk]])
                    dq_tick()
                    if c < 8:
                        grp = c // 2
                        S = grp + 1
                        w = 2 ** S
                        zk = c % 2
                        zp = zpad[zk]
                        P.op("act", lambda e, zp=zp, pz=pz: e.activation(out=zp[:, 16:16 + TG], in_=pz, func=AF.Copy),
                             reads=[Rpz_ring[pk]], writes=[Rzp[zk]])
                        P.op("dve", lambda e, zp=zp, c=c: e.tensor_copy(out=zp[:, 0:16], in_=halo[:, c, :]),
                             reads=[Rhalo[c]], writes=[Rzp[zk]])
                        P.op("dve", lambda e, zp=zp, c=c: e.tensor_copy(out=halo[:, c, :], in_=zp[:, TG:TG + 16]),
                             reads=[Rzp[zk]], writes=[Rhalo[c]])
                        prev, Rprev = zp, Rzp[zk]
                        L = 16 + TG
                        for s in range(1, S + 1):
                            sh = 2 ** (s - 1)
                            lo = 2 ** s - 1
                            ab = abuf[s % 2]
                            P.op("dve", lambda e, ab=ab, prev=prev, lo=lo, sh=sh, L=L: e.tensor_tensor(
                                out=ab[:, lo:L], in0=prev[:, lo:L], in1=prev[:, lo - sh:L - sh], op=ALU.add),
                                reads=[Rprev], writes=[Rab[s % 2]])
                            prev, Rprev = ab, Rab[s % 2]
                        P.op("dve", lambda e, prev=prev, zp=zp, zk=zk, w=w: e.scalar_tensor_tensor(
                            out=dbuf[:, zk, :], in0=prev[:, 16:16 + TG], scalar=1.0 / w, in1=zp[:, 16:16 + TG],
                            op0=ALU.mult, op1=ALU.subtract), reads=[Rprev, Rzp[zk]], writes=[Rdb[zk]])
                        if g == 0:
                            P.op("dve", lambda e, prev=prev, w=w: e.tensor_tensor(
                                out=fix16[:, 0:w - 1], in0=prev[:, 16:16 + w - 1], in1=cst_sb[:, 70:70 + w - 1],
                                op=ALU.mult), reads=[Rprev, R_const], writes=[Rfix])
                            P.op("dve", lambda e, zp=zp, zk=zk, w=w: e.tensor_tensor(
                                out=dbuf[:, zk, 0:w - 1], in0=fix16[:, 0:w - 1], in1=zp[:, 16:16 + w - 1],
                                op=ALU.subtract), reads=[Rfix, Rzp[zk]], writes=[Rdb[zk]])
                        if c % 2 == 1:
                            P.defer_begin()
                            for oc in range(2):
                                mk = mcnt[0] % 2
                                mcnt[0] += 1
                                pm = psm[mk][:, 0:TG]
                                for ic in range(2):
                                    P.op("pe", lambda e, pm=pm, grp=grp, ic=ic, oc=oc: e.matmul(
                                        out=pm, lhsT=wpool_sb[:, grp, ic, oc * 128:(oc + 1) * 128], rhs=dbuf[:, ic, :],
                                        start=(ic == 0), stop=(ic == 1)), reads=[R_w, Rdb[ic]], writes=[Rpsm[mk]])
                                cc = 2 * grp + oc
                                P.op("act", lambda e, pm=pm, cc=cc: e.activation(
                                    out=ypo[:, cc, :], in_=pm, func=AF.Copy, scale=cst_sb[:, 56 + cc:57 + cc]),
                                    reads=[Rpsm[mk], R_const], writes=[Rypo], indep=True)
                            dq.append([1, P.defer_end()])
                    elif c < 16:
                        j = (c - 8) % 4
                        isq = c < 12
                        sk = c % 2
                        P.op("act", lambda e, j=j, pz=pz: e.activation(out=zlat[:, j, :], in_=pz, func=AF.Copy),
                             reads=[Rpz_ring[pk]], writes=[Rzlat], indep=True)
                        P.op("act", lambda e, sk=sk, pz=pz: e.activation(out=sq[sk][:], in_=pz, func=AF.Square),
                             reads=[Rpz_ring[pk]], writes=[Rsq[sk]])
                        P.defer_begin()
                        if j == 0:
                            smk = mcnt[0] % 2
                            mcnt[0] += 1
                        pm = psm[smk][:, 0:TG]
                        P.op("pe", lambda e, pm=pm, sk=sk, j=j: e.matmul(out=pm, lhsT=ones_f, rhs=sq[sk][:],
                                                                          start=(j == 0), stop=(j == 3)),
                             reads=[Rsq[sk], R_const], writes=[Rpsm[smk]])
                        if j == 3:
                            P.op("act", lambda e, pm=pm: e.activation(out=rlat[:], in_=pm, func=AF.Ln, scale=1.0 / 512,
                                                                        bias=cst_sb[:, 86:87]),
                                 reads=[Rpsm[smk], R_const], writes=[Rrlat])
                            P.op("act", lambda e: e.activation(out=rlat[:], in_=rlat[:], func=AF.Exp, scale=-0.5),
                                 reads=[Rrlat], writes=[Rrlat])
                            dst, Rd, gc = (cqn, Rcqn, 48) if isq else (ckvn, Rckvn, 52)
                            for jj in range(4):
                                P.op("dve", lambda e, dst=dst, jj=jj, gc=gc: e.scalar_tensor_tensor(
                                    out=dst[:, jj, :], in0=zlat[:, jj, :], scalar=cst_sb[:, gc + jj:gc + jj + 1],
                                    in1=rlat[:], op0=ALU.mult, op1=ALU.mult),
                                    reads=[Rzlat, Rrlat, R_const], writes=[Rd], indep=(jj > 0))
                            if isq:
                                P.dma("sp", lambda e, g=g: e.dma_start(
                                    out=CQ[g].rearrange("p (a b) -> p a b", a=4), in_=cqn[:]), reads=[Rcqn])
                        dq.append([1, P.defer_end()])
                    else:
                        P.op("act", lambda e, pz=pz: e.activation(out=krg[:], in_=pz, func=AF.Copy,
                                                                    scale=cst_sb[:, 67:68]),
                             reads=[Rpz_ring[pk], R_const], writes=[Rkr])
                        P.op("act", lambda e, pz=pz: e.activation(out=krsq[:], in_=pz, func=AF.Square),
                             reads=[Rpz_ring[pk]], writes=[Rkr])
                dq_flush()
                P.dma("sp", lambda e, g=g: e.dma_start(out=YP[g].rearrange("p (a b) -> p a b", a=8), in_=ypo[:]),
                      reads=[Rypo])
                pro_some(6)
                mkc = mcnt[0] % 2
                mcnt[0] += 1
                pcol = psm[mkc]
                ntt = TG // 128
                for h in range(8):
                    pk = pcnt[0] % len(pz_ring)
                    pcnt[0] += 1
                    pz = pz_ring[pk]
                    for fc in range(4):
                        P.op("pe", lambda e, pz=pz, fc=fc, h=h: e.matmul(
                            out=pz, lhsT=wuk_sb[:, fc, h * 128:(h + 1) * 128], rhs=ckvn[:, fc, :],
                            start=(fc == 0), stop=(fc == 3)), reads=[R_w, Rckvn], writes=[Rpz_ring[pk]])
                    dq_tick()
                    P.op("act", lambda e, pz=pz, h=h, t0=t0: e.activation(
                        out=KT[:, h, t0:t0 + TG], in_=pz, func=AF.Copy, scale=cst_sb[:, 66:67]),
                        reads=[Rpz_ring[pk], R_const], writes=[R_K[g]], indep=True)
                    sk = h % 2
                    P.op("act", lambda e, pz=pz, sk=sk: e.activation(out=sq[sk][:], in_=pz, func=AF.Square),
                         reads=[Rpz_ring[pk]], writes=[Rsq[sk]])
                    P.defer_begin()
                    for tt in range(ntt):
                        col = (h * ntt + tt) * 2
                        P.op("pe", lambda e, sk=sk, tt=tt, col=col: e.matmul(
                            out=pcol[:, col:col + 2], lhsT=sq[sk][:, tt * 128:(tt + 1) * 128], rhs=ones_f[:, 0:2],
                            start=True, stop=False), reads=[Rsq[sk], R_const], writes=[Rpsm[mkc]])
                        P.op("pe", lambda e, tt=tt, col=col: e.matmul(
                            out=pcol[:, col:col + 2], lhsT=krsq[0:64, tt * 128:(tt + 1) * 128], rhs=ones_f[0:64, 0:2],
                            start=False, stop=True), reads=[Rkr, R_const], writes=[Rpsm[mkc]])
                    dq.append([1, P.defer_end()])
                dq_flush()
                nc_ = 8 * ntt
                P.op("dve", lambda e, nc_=nc_: e.tensor_scalar(
                    out=colb[:, 0, 0:nc_], in0=pcol[:, 0:2 * nc_].rearrange("p (a b) -> p a b", b=2)[:, :, 0],
                    scalar1=1.0, scalar2=192.0 * EPS, op0=ALU.mult, op1=ALU.add), reads=[Rpsm[mkc]], writes=[Rcolb])
                P.op("act", lambda e, nc_=nc_: e.activation(out=colb[:, 1, 0:nc_], in_=colb[:, 0, 0:nc_], func=AF.Ln),
                     reads=[Rcolb], writes=[Rcolb])
                P.op("act", lambda e, nc_=nc_: e.activation(out=colb[:, 2, 0:nc_], in_=colb[:, 1, 0:nc_], func=AF.Exp,
                                                            scale=-0.5),
                     reads=[Rcolb], writes=[Rcolb])
                P.op("dve", lambda e, g=g, ntt=ntt, nc_=nc_: e.tensor_copy(
                    out=rkcol[:, g * ntt:(g + 1) * ntt, :].rearrange("p t h -> p h t"),
                    in_=colb[:, 2, 0:nc_].rearrange("p (h t) -> p h t", t=ntt)), reads=[Rcolb], writes=[R_K[g]])
                mk = mcnt[0] % 2
                mcnt[0] += 1
                pm = psm[mk][:, 0:TG]
                P.op("pe", lambda e, pm=pm: e.matmul(out=pm, lhsT=perm_f, rhs=krg[:], start=True, stop=True),
                     reads=[Rkr, R_const], writes=[Rpsm[mk]])
                P.op("dve", lambda e, t0=t0: e.tensor_tensor(out=t1[:], in0=krg[:], in1=cosT[:, t0:t0 + TG], op=ALU.mult),
                     reads=[Rkr, R_rope], writes=[Rt1])
                P.op("dve", lambda e, pm=pm, t0=t0: e.tensor_tensor(out=t2[:], in0=pm, in1=sinT[:, t0:t0 + TG],
                                                                     op=ALU.mult),
                     reads=[Rpsm[mk], R_rope], writes=[Rt2])
                P.op("dve", lambda e, t0=t0: e.tensor_tensor(out=KRT[:, t0:t0 + TG], in0=t1[:], in1=t2[:], op=ALU.add),
                     reads=[Rt1, Rt2], writes=[R_K[g]])
                for tt in range(ntt):
                    for nb in range(2):
                        mk = mcnt[0] % 2
                        mcnt[0] += 1
                        for fc in range(4):
                            P.op("pe", lambda e, mk=mk, fc=fc, tt=tt, nb=nb: e.matmul(
                                out=psm[mk][:], lhsT=ckvn[:, fc, tt * 128:(tt + 1) * 128],
                                rhs=wv_sb[:, fc, nb * 512:(nb + 1) * 512], start=(fc == 0), stop=(fc == 3)),
                                reads=[Rckvn, R_w], writes=[Rpsm[mk]])
                        P.op("act", lambda e, mk=mk, tt=tt, nb=nb, g=g, ntt=ntt: e.activation(
                            out=Vsb[:, g * ntt + tt, nb * 512:(nb + 1) * 512], in_=psm[mk][:], func=AF.Copy),
                            reads=[Rpsm[mk]], writes=[R_V[g]], indep=True)
            P.emit()
        if stop_after == "M1":
            stKV.close()
            return nc, None, st0

        with ExitStack() as st:
            wuq_sb = sbuf(st, "wuq_sb", [128, 4, 1536], BF16)
            mask_f = sbuf(st, "mask_f", [128, 2, TG], F32)
            mask_b = sbuf(st, "mask_b", [128, 2, TG], BF16)
            R_w = P.res("m2w")
            P.dma("pool", lambda e: e.dma_start(out=wuq_sb[:], in_=Wuq.rearrange("p (a b) -> p a b", a=4),
                                                max_dma_last_dim=2048), writes=[R_w])
            P.dma("sp", lambda e: e.dma_start(out=mask_f[:], in_=maskc.rearrange("p (a b) -> p a b", a=2)),
                  writes=[R_w])
            P.op("dve", lambda e: e.tensor_copy(out=mask_b[:], in_=mask_f[:]), reads=[R_w], writes=[R_w])
            cqn = sbuf(st, "m2_cqn", [128, 4, TG], BF16)
            Rcqn = P.res("m2_cqn")
            ymix = sbuf(st, "ymix", [128, 16, TG], BF16)
            Rypool = P.res("ymix_pool")
            Rymla = P.res("ymix_mla")
            psq = [psum(st, "psq%d" % i, [128, 512], F32) for i in range(2)]
            Rpsq = [P.res("psq%d" % i) for i in range(2)]
            pss = [psum(st, "pss%d" % i, [128, 512], F32) for i in range(2)]
            Rpss = [P.res("pss%d" % i) for i in range(2)]
            pso = psum(st, "pso", [128, 512], F32)
            psd = psum(st, "psd", [128, 512], F32)
            Rpso, Rpsd = P.res("pso"), P.res("psd")
            psh = [psum(st, "psh%d" % i, [128, 512], F32) for i in range(2)]
            Rpsh = [P.res("psh%d" % i) for i in range(2)]
            qtmp = [sbuf(st, "qtmp%d" % i, [128, TG], F32) for i in range(2)]
            sqq = [sbuf(st, "sqq%d" % i, [128, TG], F32) for i in range(2)]
            rq = [sbuf(st, "rq%d" % i, [128, TG], F32) for i in range(2)]
            Rqtmp = [P.res("qtmp%d" % i) for i in range(2)]
            Rsqq = [P.res("sqq%d" % i) for i in range(2)]
            Rrq = [P.res("rq%d" % i) for i in range(2)]
            qrg = sbuf(st, "qrg", [128, TG], F32)
            sqr = sbuf(st, "sqr", [128, TG], F32)
            Rqrg, Rsqr = P.res("qrg"), P.res("sqr")
            t1 = sbuf(st, "m2_t1", [128, TG], F32)
            t2 = sbuf(st, "m2_t2", [128, TG], F32)
            Rt1, Rt2 = P.res("m2_t1"), P.res("m2_t2")
            QTs = [sbuf(st, "QT%d" % i, [128, TG], BF16) for i in range(4)]
            RQTs = [P.res("QT%d" % i) for i in range(4)]
            QRTs = [sbuf(st, "QRT%d" % i, [128, TG], BF16) for i in range(2)]
            RQRTs = [P.res("QRT%d" % i) for i in range(2)]
            pT = [sbuf(st, "pT%d" % i, [128, TG], BF16) for i in range(3)]
            RpT = [P.res("pT%d" % i) for i in range(3)]
            rec = sbuf(st, "rec", [128, TG], F32)
            Rrec = P.res("rec")
            wob = [sbuf(st, "wob%d" % i, [128, 16, 512], BF16) for i in range(3)]
            Rwob = [P.res("wob%d" % i) for i in range(3)]
            xb = [sbuf(st, "m2_xb%d" % i, [128, 512], F32) for i in range(3)]
            Rxb = [P.res("m2_xb%d" % i) for i in range(3)]
            hb = [sbuf(st, "m2_hb%d" % i, [128, 512], F32) for i in range(3)]
            Rhb = [P.res("m2_hb%d" % i) for i in range(3)]
            qcnt = [0]
            scnt = [0]
            ptc = [0]
            wcnt = [0]
            hcnt = [0]
            ntt = TG // 128

            for g in range(NG):
                t0 = g * TG
                P.dma("sp", lambda e, g=g: e.dma_start(out=cqn[:], in_=CQ[g].rearrange("p (a b) -> p a b", a=4)),
                      writes=[Rcqn])
                P.dma("sp", lambda e, g=g: e.dma_start(out=ymix[:, 0:8, :], in_=YP[g].rearrange("p (a b) -> p a b", a=8)),
                      writes=[Rypool])
                pro_some(8)
                nkt = ntt * (g + 1)
                def prep(j, t0=t0):
                    QT, RQT = QTs[2 * (j % 2):2 * (j % 2) + 2], RQTs[2 * (j % 2):2 * (j % 2) + 2]
                    QRT, RQRT = QRTs[j % 2], RQRTs[j % 2]
                    qk = qcnt[0] % 2
                    qcnt[0] += 1
                    pqr = psq[qk][:, 0:TG]
                    for fc in range(4):
                        P.op("pe", lambda e, pqr=pqr, fc=fc, j=j: e.matmul(
                            out=pqr, lhsT=wuq_sb[:, fc, 1024 + j * 128:1024 + (j + 1) * 128], rhs=cqn[:, fc, :],
                            start=(fc == 0), stop=(fc == 3)), reads=[R_w, Rcqn], writes=[Rpsq[qk]])
                    P.op("act", lambda e, pqr=pqr: e.activation(out=qrg[:], in_=pqr, func=AF.Copy, scale=cst_sb[:, 65:66]),
                         reads=[Rpsq[qk], R_const], writes=[Rqrg])
                    P.op("act", lambda e, pqr=pqr: e.activation(out=sqr[:], in_=pqr, func=AF.Square),
                         reads=[Rpsq[qk]], writes=[Rsqr])
                    for hh in range(2):
                        h = 2 * j + hh
                        qk2 = qcnt[0] % 2
                        qcnt[0] += 1
                        pq = psq[qk2][:, 0:TG]
                        for fc in range(4):
                            P.op("pe", lambda e, pq=pq, fc=fc, h=h: e.matmul(
                                out=pq, lhsT=wuq_sb[:, fc, h * 128:(h + 1) * 128], rhs=cqn[:, fc, :],
                                start=(fc == 0), stop=(fc == 3)), reads=[R_w, Rcqn], writes=[Rpsq[qk2]])
                        P.op("act", lambda e, pq=pq, hh=hh: e.activation(out=qtmp[hh][:], in_=pq, func=AF.Copy,
                                                                           scale=cst_sb[:, 64:65]),
                             reads=[Rpsq[qk2], R_const], writes=[Rqtmp[hh]])
                        P.op("act", lambda e, pq=pq, hh=hh: e.activation(out=sqq[hh][:], in_=pq, func=AF.Square),
                             reads=[Rpsq[qk2]], writes=[Rsqq[hh]])
                        P.op("pe", lambda e, pq=pq, hh=hh: e.matmul(out=pq, lhsT=ones_f, rhs=sqq[hh][:], start=True,
                                                                      stop=False),
                             reads=[Rsqq[hh], R_const, Rqtmp[hh]], writes=[Rpsq[qk2]])
                        P.op("pe", lambda e, pq=pq, hh=hh: e.matmul(out=pq, lhsT=(Llo_f if hh == 0 else Lhi_f),
                                                                      rhs=sqr[:], start=False, stop=True),
                             reads=[Rsqr, R_const], writes=[Rpsq[qk2]])
                        P.op("act", lambda e, pq=pq, hh=hh: e.activation(out=rq[hh][:], in_=pq, func=AF.Ln,
                                                                           scale=1.0 / 192, bias=cst_sb[:, 86:87]),
                             reads=[Rpsq[qk2], R_const], writes=[Rrq[hh]])
                        P.op("act", lambda e, hh=hh: e.activation(out=rq[hh][:], in_=rq[hh][:], func=AF.Exp, scale=-0.5),
                             reads=[Rrq[hh]], writes=[Rrq[hh]])
                        P.op("dve", lambda e, hh=hh: e.tensor_tensor(out=QT[hh][:], in0=qtmp[hh][:], in1=rq[hh][:],
                                                                       op=ALU.mult),
                             reads=[Rqtmp[hh], Rrq[hh]], writes=[RQT[hh]])
                    qk3 = qcnt[0] % 2
                    qcnt[0] += 1
                    pr = psq[qk3][:, 0:TG]
                    P.op("pe", lambda e, pr=pr: e.matmul(out=pr, lhsT=perm_f, rhs=qrg[:], start=True, stop=True),
                         reads=[Rqrg, R_const], writes=[Rpsq[qk3]])
                    P.op("dve", lambda e, t0=t0: e.tensor_tensor(out=t1[:], in0=qrg[:], in1=cosT[:, t0:t0 + TG],
                                                                  op=ALU.mult), reads=[Rqrg, R_rope], writes=[Rt1])
                    P.op("dve", lambda e, pr=pr, t0=t0: e.tensor_tensor(out=t2[:], in0=pr, in1=sinT[:, t0:t0 + TG],
                                                                         op=ALU.mult),
                         reads=[Rpsq[qk3], R_rope], writes=[Rt2])
                    P.op("dve", lambda e: e.tensor_tensor(out=t1[:], in0=t1[:], in1=t2[:], op=ALU.add),
                         reads=[Rt1, Rt2], writes=[Rt1])
                    P.op("dve", lambda e: e.tensor_tensor(out=QRT[0:64, :], in0=t1[0:64, :], in1=rq[0][0:64, :],
                                                          op=ALU.mult), reads=[Rt1, Rrq[0]], writes=[RQRT])
                    P.op("dve", lambda e: e.tensor_tensor(out=QRT[64:128, :], in0=t1[64:128, :], in1=rq[1][64:128, :],
                                                          op=ALU.mult), reads=[Rt1, Rrq[1]], writes=[RQRT], indep=True)

                prep(0)
                for j in range(4):
                    QT, RQT = QTs[2 * (j % 2):2 * (j % 2) + 2], RQTs[2 * (j % 2):2 * (j % 2) + 2]
                    QRT, RQRT = QRTs[j % 2], RQRTs[j % 2]
                    pending = []
                    if j + 1 < 4:
                        P.defer_begin()
                        prep(j + 1)
                        pending = P.defer_end()
                    kstep = -(-len(pending) // max(1, 2 * nkt - 1))
                    for hh in range(2):
                        h = 2 * j + hh
                        hp = 64 * hh
                        def emit_S(kt, h=h, hh=hh, hp=hp, QT=QT, QRT=QRT):
                            sk = scnt[0] % 2
                            scnt[0] += 1
                            ps_ = pss[sk][:, 0:TG]
                            gk = kt // ntt
                            P.op("pe", lambda e, ps_=ps_, h=h, kt=kt, hh=hh: e.matmul(
                                out=ps_, lhsT=KT[:, h, kt * 128:(kt + 1) * 128], rhs=QT[hh][:], start=True, stop=False),
                                reads=[R_K[gk], RQT[hh]], writes=[Rpss[sk]])
                            P.op("pe", lambda e, ps_=ps_, kt=kt, hp=hp: e.matmul(
                                out=ps_, lhsT=KRT[hp:hp + 64, kt * 128:(kt + 1) * 128], rhs=QRT[hp:hp + 64, :],
                                start=False, stop=True), reads=[R_K[gk], RQRT], writes=[Rpss[sk]])
                            return sk, ps_

                        nxt = emit_S(0)
                        for kt in range(nkt):
                            sk, ps_ = nxt
                            gk = kt // ntt
                            if kt + 1 < nkt:
                                nxt = emit_S(kt + 1)
                            pk_ = ptc[0] % 3
                            ptc[0] += 1
                            P.op("act", lambda e, ps_=ps_, pk_=pk_, kt=kt, h=h: e.activation(
                                out=pT[pk_][:], in_=ps_, func=AF.Exp, scale=rkcol[:, kt, h:h + 1]),
                                reads=[Rpss[sk], R_K[gk]], writes=[RpT[pk_]])
                            if kt >= ntt * g:
                                jm = kt - ntt * g
                                P.op("dve", lambda e, pk_=pk_, jm=jm: e.tensor_tensor(
                                    out=pT[pk_][:], in0=pT[pk_][:], in1=mask_b[:, jm, :], op=ALU.mult),
                                    reads=[RpT[pk_], R_w], writes=[RpT[pk_]])
                            P.op("pe", lambda e, pk_=pk_, kt=kt, h=h, nkt=nkt: e.matmul(
                                out=pso[:, 0:TG], lhsT=Vsb[:, kt, h * 128:(h + 1) * 128], rhs=pT[pk_][:],
                                start=(kt == 0), stop=(kt == nkt - 1)), reads=[R_V[gk], RpT[pk_]], writes=[Rpso])
                            P.op("pe", lambda e, pk_=pk_, kt=kt, nkt=nkt: e.matmul(
                                out=psd[:, 0:TG], lhsT=ones_bf[:], rhs=pT[pk_][:],
                                start=(kt == 0), stop=(kt == nkt - 1)), reads=[R_const, RpT[pk_]], writes=[Rpsd])
                            for _ in range(kstep):
                                if pending:
                                    pending.pop(0)()
                        P.op("dve", lambda e: e.reciprocal(out=rec[:], in_=psd[:, 0:TG]), reads=[Rpsd], writes=[Rrec])
                        P.op("dve", lambda e, h=h: e.tensor_tensor(out=ymix[:, 8 + h, :], in0=pso[:, 0:TG], in1=rec[:],
                                                                    op=ALU.mult),
                             reads=[Rpso, Rrec], writes=[Rymla])
                    while pending:
                        pending.pop(0)()
                for nb in range(4):
                    wk = wcnt[0] % 3
                    wcnt[0] += 1
                    P.dma("sp", lambda e, wk=wk, nb=nb: e.dma_start(
                        out=wob[wk][:], in_=Woutb[nb].rearrange("p (a b) -> p a b", a=16)),
                        reads=[RWout[nb]], writes=[Rwob[wk]])
                    for tt in range(ntt):
                        hk = hcnt[0] % 3
                        hk2 = hcnt[0] % 2
                        hcnt[0] += 1
                        row0 = t0 + tt * 128
                        P.dma("sp", lambda e, hk=hk, row0=row0, nb=nb: e.dma_start(
                            out=xb[hk][:], in_=x[row0:row0 + 128, nb * 512:(nb + 1) * 512]), writes=[Rxb[hk]])
                        for fc in range(16):
                            P.op("pe", lambda e, hk2=hk2, fc=fc, tt=tt, wk=wk: e.matmul(
                                out=psh[hk2][:], lhsT=ymix[:, fc, tt * 128:(tt + 1) * 128], rhs=wob[wk][:, fc, :],
                                start=(fc == 0), stop=(fc == 15)),
                                reads=[Rypool, Rymla, Rwob[wk]], writes=[Rpsh[hk2]])
                        P.op("dve", lambda e, hk=hk, hk2=hk2: e.tensor_tensor(out=hb[hk][:], in0=psh[hk2][:],
                                                                                in1=xb[hk][:], op=ALU.add),
                             reads=[Rpsh[hk2], Rxb[hk]], writes=[Rhb[hk]])
                        P.dma("sp", lambda e, hk=hk, row0=row0, nb=nb: e.dma_start(
                            out=H1[row0:row0 + 128, nb * 512:(nb + 1) * 512], in_=hb[hk][:]), reads=[Rhb[hk]])
            P.emit()
        stKV.close()
        if stop_after == "M2":
            return nc, None, st0

        ntt = TG // 128
        IRv = IRs.rearrange("k p t -> p k t")
        with ExitStack() as st:
            subk_sb = sbuf(st, "subk_sb", [128, 16, 128], F32)
            iota16 = sbuf(st, "iota16", [128, 16], F32)
            R_w = P.res("f1w")
            P.dma("sp", lambda e: e.dma_start(out=subk_sb[:], in_=subk.rearrange("p (a b) -> p a b", a=16)), writes=[R_w])
            P.op("dve", lambda e: e.tensor_copy(out=iota16[:], in_=iota_f[:, 0:16]), reads=[R_const], writes=[R_w])
            ht = [sbuf(st, "f1_ht%d" % i, [128, D], F32) for i in range(2)]
            Rht = [P.res("f1_ht%d" % i) for i in range(2)]
            rbs = [rms_bufs(st, "f1_%d" % i) for i in range(2)]
            xn2 = sbuf(st, "xn2", [128, 16, TG], BF16)
            Rxn2 = P.res("xn2")
            wbuf = [sbuf(st, "f1_wb%d" % i, [128, 16, 128], BF16) for i in range(4)]
            Rwb = [P.res("f1_wb%d" % i) for i in range(4)]
            psq = [psum(st, "f1_psq%d" % i, [128, 512], F32) for i in range(2)]
            Rpsq = [P.res("f1_psq%d" % i) for i in range(2)]
            pssc = [psum(st, "f1_pssc%d" % i, [128, 512], F32) for i in range(2)]
            Rpssc = [P.res("f1_pssc%d" % i) for i in range(2)]
            qpTs = [sbuf(st, "qpT%d" % i, [128, 16, TG], F32) for i in range(2)]
            RqpTs = [P.res("qpT%d" % i) for i in range(2)]
            sc = sbuf(st, "sc", [128, 16, 128], F32)
            sc2 = sbuf(st, "sc2", [128, 16, 128], F32)
            Rsc = [P.res("sc%d" % i) for i in range(16)]
            Rsc2 = [P.res("sc2_%d" % i) for i in range(16)]
            v16 = sbuf(st, "v16", [128, 16, 16], F32)
            i16 = sbuf(st, "i16", [128, 16, 16], U32)
            i16f = sbuf(st, "i16f", [128, 16, 16], F32)
            Rv16 = [P.res("v16_%d" % i) for i in range(16)]
            Ri16 = [P.res("i16_%d" % i) for i in range(16)]
            Ri16f = P.res("i16f")
            cand = sbuf(st, "cand", [128, 8, 256], F32)
            cand2 = sbuf(st, "cand2", [128, 8, 256], F32)
            Rcand = [P.res("cand%d" % i) for i in range(8)]
            Rcand2 = [P.res("cand2_%d" % i) for i in range(8)]
            vs = sbuf(st, "vs", [128, 8, 16], F32)
            ci = sbuf(st, "ci", [128, 8, 16], U32)
            Rvs = [P.res("vs%d" % i) for i in range(8)]
            Rci = [P.res("ci%d" % i) for i in range(8)]
            abi = sbuf(st, "abi", [128, 2, 128], U32)
            abf = sbuf(st, "abf", [128, 2, 128], F32)
            Rabi, Rabf = P.res("abi"), P.res("abf")
            eqb = [sbuf(st, "eqb%d" % i, [128, 8, 16, 16], F32) for i in range(2)]
            Reqb = [P.res("eqb%d" % i) for i in range(2)]
            tris = [sbuf(st, "tri%d" % i, [128, 3, 128], F32) for i in range(4)]
            Rtris = [P.res("tri%d" % i) for i in range(4)]
            gsm = sbuf(st, "gsm", [128, 2, 8], F32)
            Rgsm = P.res("gsm")
            pstr = pssc[1]
            Rpstr = Rpssc[1]
            trT = sbuf(st, "trT", [128, 3, 128], F32)
            RtrT = P.res("trT")
            wcnt = [0]
            qcnt = [0]
            tcnt = [0]

            def top16(src, Rsrc, src2, Rsrc2, vout, Rvout, iout, Riout, n):
                for k in range(n):
                    P.op("dve", lambda e, k=k: e.max(out=vout(k)[:, 0:8], in_=src(k)), reads=[Rsrc[k]], writes=[Rvout[k]])
                for k in range(n):
                    P.op("dve", lambda e, k=k: e.max_index(out=iout(k)[:, 0:8], in_max=vout(k)[:, 0:8], in_values=src(k)),
                         reads=[Rsrc[k], Rvout[k]], writes=[Riout[k]])
                for k in range(n):
                    P.op("dve", lambda e, k=k: e.match_replace(out=src2(k), in_to_replace=vout(k)[:, 0:8],
                                                               in_values=src(k), imm_value=-1e30),
                         reads=[Rsrc[k], Rvout[k]], writes=[Rsrc2[k]])
                for k in range(n):
                    P.op("dve", lambda e, k=k: e.max(out=vout(k)[:, 8:16], in_=src2(k)), reads=[Rsrc2[k]],
                         writes=[Rvout[k]])
                for k in range(n):
                    P.op("dve", lambda e, k=k: e.max_index(out=iout(k)[:, 8:16], in_max=vout(k)[:, 8:16],
                                                           in_values=src2(k)),
                         reads=[Rsrc2[k], Rvout[k]], writes=[Riout[k]])

            def f1_front(g):
                t0 = g * TG
                qpT, RqpT = qpTs[g % 2], RqpTs[g % 2]
                items = []
                for tt in range(ntt):
                    k = tt % 2
                    row0 = t0 + tt * 128
                    P.dma("sp", lambda e, k=k, row0=row0: e.dma_start(out=ht[k][:], in_=H1[row0:row0 + 128, :]),
                          writes=[Rht[k]])
                    items.append((ht[k][:], Rht[k], (lambda dc, tt=tt: xn2[:, dc, tt * 128:(tt + 1) * 128]), Rxn2, rbs[k]))
                rmsnorm_multi(items, 16, all_act=True)
                P.dma("sp", lambda e, g=g: e.dma_start(out=XN2[g].rearrange("p (a b) -> p a b", a=16), in_=xn2[:]),
                      reads=[Rxn2])
                pro_some(40)
                for c in range(16):
                    wk = wcnt[0] % 4
                    wcnt[0] += 1
                    P.dma("sp", lambda e, wk=wk, c=c: e.dma_start(
                        out=wbuf[wk][:], in_=Wpqb[c].rearrange("p (a b) -> p a b", a=16)),
                        reads=[RWpq[c]], writes=[Rwb[wk]])
                    qk = qcnt[0] % 2
                    qcnt[0] += 1
                    pq = psq[qk][:, 0:TG]
                    for dc in range(16):
                        P.op("pe", lambda e, wk=wk, dc=dc, pq=pq: e.matmul(
                            out=pq, lhsT=wbuf[wk][:, dc, :], rhs=xn2[:, dc, :], start=(dc == 0), stop=(dc == 15)),
                            reads=[Rwb[wk], Rxn2], writes=[Rpsq[qk]])
                    P.op("act", lambda e, pq=pq, c=c: e.activation(out=qpT[:, c, :], in_=pq, func=AF.Copy),
                         reads=[Rpsq[qk]], writes=[RqpT], indep=True)

            def f1_main(g, tt):
                t0 = g * TG
                qpT, RqpT = qpTs[g % 2], RqpTs[g % 2]
                tri, Rtri = tris[2 * (g % 2) + tt], Rtris[2 * (g % 2) + tt]
                if True:
                    for c in range(16):
                        bq = (c // 4) % 2
                        P.op("pe", lambda e, c=c, tt=tt, bq=bq: e.matmul(
                            out=pssc[bq][:, (c % 4) * 128:(c % 4 + 1) * 128],
                            lhsT=qpT[:, c, tt * 128:(tt + 1) * 128], rhs=subk_sb[:, c, :], start=True, stop=True),
                            reads=[RqpT, R_w], writes=[Rpssc[bq]])
                        if c % 4 == 3:
                            b4 = c // 4
                            P.op("dve", lambda e, b4=b4, bq=bq: e.tensor_copy(
                                out=sc[:, 4 * b4:4 * b4 + 4, :], in_=pssc[bq][:].rearrange("p (a b) -> p a b", a=4)),
                                reads=[Rpssc[bq]], writes=Rsc[4 * b4:4 * b4 + 4])
                    top16(lambda k: sc[:, k, :], Rsc, lambda k: sc2[:, k, :], Rsc2,
                          lambda k: v16[:, k, :], Rv16, lambda k: i16[:, k, :], Ri16, 16)
                    P.op("dve", lambda e: e.tensor_copy(out=i16f[:], in_=i16[:]), reads=Ri16, writes=[Ri16f])
                    v16v = v16[:].rearrange("p (h s) a -> p h s a", s=2)
                    i16v = i16f[:].rearrange("p (h s) a -> p h s a", s=2)
                    P.op("dve", lambda e, v16v=v16v: e.tensor_tensor(
                        out=cand[:].rearrange("p h (a b) -> p h a b", a=16),
                        in0=v16v[:, :, 0, :].unsqueeze(3).to_broadcast([128, 8, 16, 16]),
                        in1=v16v[:, :, 1, :].unsqueeze(2).to_broadcast([128, 8, 16, 16]), op=ALU.add),
                        reads=Rv16, writes=Rcand)
                    top16(lambda k: cand[:, k, :], Rcand, lambda k: cand2[:, k, :], Rcand2,
                          lambda k: vs[:, k, :], Rvs, lambda k: ci[:, k, :], Rci, 8)
                    civ = ci[:].rearrange("p h r -> p (h r)")
                    P.op("dve", lambda e, civ=civ: e.tensor_single_scalar(out=abi[:, 0, :], in_=civ, scalar=4,
                                                                          op=ALU.logical_shift_right),
                         reads=Rci, writes=[Rabi])
                    P.op("dve", lambda e, civ=civ: e.tensor_single_scalar(out=abi[:, 1, :], in_=civ, scalar=15,
                                                                          op=ALU.bitwise_and),
                         reads=Rci, writes=[Rabi], indep=True)
                    P.op("dve", lambda e: e.tensor_copy(out=abf[:], in_=abi[:]), reads=[Rabi], writes=[Rabf])
                    for s_ in range(2):
                        eb = eqb[s_]
                        P.op("dve", lambda e, eb=eb, s_=s_: e.tensor_tensor(
                            out=eb[:],
                            in0=iota16[:].unsqueeze(1).unsqueeze(1).to_broadcast([128, 8, 16, 16]),
                            in1=abf[:, s_, :].rearrange("p (h r) -> p h r", h=8).unsqueeze(3).to_broadcast([128, 8, 16, 16]),
                            op=ALU.is_equal), reads=[Rabf, R_w], writes=[Reqb[s_]])
                        P.op("dve", lambda e, eb=eb, s_=s_, i16v=i16v: e.tensor_tensor(
                            out=eb[:], in0=eb[:],
                            in1=i16v[:, :, s_, :].unsqueeze(2).to_broadcast([128, 8, 16, 16]), op=ALU.mult),
                            reads=[Reqb[s_], Ri16f], writes=[Reqb[s_]])
                        P.op("dve", lambda e, eb=eb, s_=s_: e.tensor_reduce(
                            out=tri[:, s_, :].rearrange("p (h r) -> p h r", h=8), in_=eb[:], axis=AX.X, op=ALU.add),
                            reads=[Reqb[s_]], writes=[Rtri])
                    gv = tri[:, 2, :].rearrange("p (h r) -> p h r", h=8)
                    P.op("dve", lambda e, gv=gv: e.tensor_tensor(
                        out=gv, in0=vs[:], in1=vs[:, :, 0:1].to_broadcast([128, 8, 16]), op=ALU.subtract),
                        reads=Rvs, writes=[Rtri])

            def f1_tail(g, tt):
                t0 = g * TG
                tri, Rtri = tris[2 * (g % 2) + tt], Rtris[2 * (g % 2) + tt]
                if True:
                    gv = tri[:, 2, :].rearrange("p (h r) -> p h r", h=8)
                    P.op("act", lambda e, gv=gv: e.activation(out=gv, in_=gv, func=AF.Exp), reads=[Rtri], writes=[Rtri])
                    P.op("dve", lambda e, gv=gv: e.tensor_reduce(out=gsm[:, 0, :], in_=gv, axis=AX.X, op=ALU.add),
                         reads=[Rtri], writes=[Rgsm])
                    P.op("dve", lambda e: e.reciprocal(out=gsm[:, 1, :], in_=gsm[:, 0, :]), reads=[Rgsm], writes=[Rgsm])
                    P.op("dve", lambda e, gv=gv: e.tensor_tensor(
                        out=gv, in0=gv, in1=gsm[:, 1, :].unsqueeze(2).to_broadcast([128, 8, 16]), op=ALU.mult),
                        reads=[Rtri, Rgsm], writes=[Rtri])
                    for q_ in range(3):
                        P.op("pe", lambda e, q_=q_: e.transpose(out=pstr[:, q_ * 128:(q_ + 1) * 128], in_=tri[:, q_, :],
                                                                identity=ident_f), reads=[Rtri, R_const], writes=[Rpstr])
                    P.op("act", lambda e: e.activation(out=trT[:], in_=pstr[:, 0:384].rearrange("p (a b) -> p a b", a=3),
                                                       func=AF.Copy), reads=[Rpstr], writes=[RtrT])
                    row0 = t0 + tt * 128
                    P.dma("sp", lambda e, row0=row0: e.dma_start(out=IRv[:, :, row0:row0 + 128], in_=trT[:]),
                          reads=[RtrT])

            f1_front(0)
            if NG > 1:
                f1_front(1)
            for g in range(NG):
                for tt in range(ntt):
                    f1_main(g, tt)
                if g + 2 < NG:
                    f1_front(g + 2)
                if g >= 1:
                    for tt in range(ntt):
                        f1_tail(g - 1, tt)
            for tt in range(ntt):
                f1_tail(NG - 1, tt)
            P.emit()
        if stop_after == "F1":
            return nc, None, st0

        with ExitStack() as st:
            GT = sbuf(st, "GT", [128, TG, 128], BF16)
            RGT = [P.res("GT%d" % i) for i in range(128)]
            xn2 = sbuf(st, "f2_xn2", [128, 16, TG], BF16)
            Rxn2 = P.res("f2_xn2")
            trg = sbuf(st, "trg", [128, 3, TG], F32)
            Rtrg = P.res("trg")
            NSUB = 32
            Pb = [sbuf(st, "Pb%d" % i, [128, NSUB, 128], BF16) for i in range(2)]
            Qb = [sbuf(st, "Qb%d" % i, [128, NSUB, 128], BF16) for i in range(2)]
            RPb = [P.res("Pb%d" % i) for i in range(2)]
            RQb = [P.res("Qb%d" % i) for i in range(2)]
            psg = [psum(st, "psg%d" % i, [128, 512], F32) for i in range(2)]
            Rpsg = [P.res("psg%d" % i) for i in range(2)]
            pss = [psum(st, "f2_pss%d" % i, [128, 512], F32) for i in range(2)]
            Rpss = [P.res("f2_pss%d" % i) for i in range(2)]
            pso = [psum(st, "f2_pso%d" % i, [128, 512], F32) for i in range(4)]
            Rpso = [P.res("f2_pso%d" % i) for i in range(4)]
            ub = [sbuf(st, "ub%d" % i, [128, 16, 128], BF16) for i in range(10)]
            Rub = [P.res("ub%d" % i) for i in range(10)]
            vb = [sbuf(st, "vb%d" % i, [128, 2, 1024], BF16) for i in range(6)]
            Rvb = [P.res("vb%d" % i) for i in range(6)]
            gl = [sbuf(st, "gl%d" % i, [128, TG], F32) for i in range(3)]
            Rgl = [P.res("gl%d" % i) for i in range(3)]
            hb = [sbuf(st, "f2_hb%d" % i, [128, 512], F32) for i in range(3)]
            Rhb = [P.res("f2_hb%d" % i) for i in range(3)]
            ob = [sbuf(st, "f2_ob%d" % i, [128, 512], F32) for i in range(3)]
            Rob = [P.res("f2_ob%d" % i) for i in range(3)]
            gcnt = [0]
            scnt = [0]
            ucnt = [0]
            vcnt = [0]
            glc = [0]
            ocnt = [0]
            hcnt = [0]
            sbc = [0]
            for g in range(NG):
                t0 = g * TG
                P.dma("sp", lambda e, g=g: e.dma_start(out=xn2[:], in_=XN2[g].rearrange("p (a b) -> p a b", a=16)),
                      writes=[Rxn2])
                P.dma("sp", lambda e, t0=t0: e.dma_start(out=trg[:], in_=IRv[:, :, t0:t0 + TG]), writes=[Rtrg])
                for sub in range(TG // NSUB):
                    bk = sbc[0] % 2
                    sbc[0] += 1
                    for tl in range(NSUB):
                        t = sub * NSUB + tl
                        P.op("dve", lambda e, bk=bk, tl=tl, t=t: e.tensor_scalar(
                            out=Pb[bk][:, tl, :], in0=iota_bf[:], scalar1=trg[:, 0, t:t + 1], scalar2=trg[:, 2, t:t + 1],
                            op0=ALU.is_equal, op1=ALU.mult), reads=[Rtrg, R_const], writes=[RPb[bk]], indep=True)
                        P.op("dve", lambda e, bk=bk, tl=tl, t=t: e.tensor_scalar(
                            out=Qb[bk][:, tl, :], in0=iota_bf[:], scalar1=trg[:, 1, t:t + 1], scalar2=None,
                            op0=ALU.is_equal), reads=[Rtrg, R_const], writes=[RQb[bk]], indep=True)
                    for q4 in range(NSUB // 4):
                        gk = gcnt[0] % 2
                        gcnt[0] += 1
                        tok0 = sub * NSUB + q4 * 4
                        for u in range(4):
                            tl = q4 * 4 + u
                            P.op("pe", lambda e, gk=gk, u=u, bk=bk, tl=tl: e.matmul(
                                out=psg[gk][:, u * 128:(u + 1) * 128], lhsT=Qb[bk][:, tl, :], rhs=Pb[bk][:, tl, :],
                                start=True, stop=True), reads=[RPb[bk], RQb[bk]], writes=[Rpsg[gk]])
                        P.op("act", lambda e, gk=gk, tok0=tok0: e.activation(
                            out=GT[:, tok0:tok0 + 4, :],
                            in_=psg[gk][:].rearrange("p (t i) -> p t i", t=4), func=AF.Copy),
                            reads=[Rpsg[gk]], writes=RGT, indep=True)
                for i in range(128):
                    uk = ucnt[0] % 10
                    ucnt[0] += 1
                    P.dma("sp", lambda e, uk=uk, i=i: e.dma_start(out=ub[uk][:], in_=UTb[i].rearrange("p (a b) -> p a b", a=16)),
                          writes=[Rub[uk]])
                    sk = scnt[0] % 2
                    scnt[0] += 1
                    ps_ = pss[sk][:, 0:TG]
                    for dc in range(16):
                        P.op("pe", lambda e, ps_=ps_, uk=uk, dc=dc: e.matmul(
                            out=ps_, lhsT=ub[uk][:, dc, :], rhs=xn2[:, dc, :], start=(dc == 0), stop=(dc == 15)),
                            reads=[Rub[uk], Rxn2], writes=[Rpss[sk]])
                    lk = glc[0] % 3
                    glc[0] += 1
                    P.op("act", lambda e, ps_=ps_, lk=lk: e.activation(out=gl[lk][:], in_=ps_, func=AF.Gelu_apprx_tanh),
                         reads=[Rpss[sk]], writes=[Rgl[lk]])
                    P.op("dve", lambda e, lk=lk, i=i: e.tensor_tensor(out=GT[:, :, i], in0=gl[lk][:], in1=GT[:, :, i],
                                                                        op=ALU.mult),
                         reads=[Rgl[lk], RGT[i]], writes=[RGT[i]])
                for nbp in range(2):
                    for i2 in range(64):
                        vk = vcnt[0] % 6
                        vcnt[0] += 1
                        r0 = nbp * 128 + 2 * i2
                        P.dma("sp", lambda e, vk=vk, r0=r0: e.dma_start(
                            out=vb[vk][:], in_=EVb[r0:r0 + 2].rearrange("i e n -> e i n")), writes=[Rvb[vk]])
                        for ii in range(2):
                            i = 2 * i2 + ii
                            for nbl in range(2):
                                for tt in range(ntt):
                                    pk = 2 * nbl + tt
                                    P.op("pe", lambda e, pk=pk, tt=tt, i=i, ii=ii, vk=vk, nbl=nbl: e.matmul(
                                        out=pso[pk][:], lhsT=GT[:, tt * 128:(tt + 1) * 128, i],
                                        rhs=vb[vk][:, ii, nbl * 512:(nbl + 1) * 512],
                                        start=(i == 0), stop=(i == 127)), reads=[RGT[i], Rvb[vk]], writes=[Rpso[pk]])
                    for nbl in range(2):
                        nb = 2 * nbp + nbl
                        for tt in range(ntt):
                            pk = 2 * nbl + tt
                            hk = hcnt[0] % 3
                            hcnt[0] += 1
                            row0 = t0 + tt * 128
                            P.dma("sp", lambda e, hk=hk, row0=row0, nb=nb: e.dma_start(
                                out=hb[hk][:], in_=H1[row0:row0 + 128, nb * 512:(nb + 1) * 512]), writes=[Rhb[hk]])
                            P.op("dve", lambda e, hk=hk, pk=pk: e.tensor_tensor(
                                out=ob[hk][:], in0=pso[pk][:], in1=hb[hk][:], op=ALU.add),
                                reads=[Rpso[pk], Rhb[hk]], writes=[Rob[hk]])
                            P.dma("sp", lambda e, hk=hk, row0=row0, nb=nb: e.dma_start(
                                out=H2[row0:row0 + 128, nb * 512:(nb + 1) * 512], in_=ob[hk][:]), reads=[Rob[hk]])
            P.emit()
        if stop_after == "F2":
            return nc, None, st0

        with ExitStack() as st:
            NTILE = T // 128
            wpp_sb = sbuf(st, "wpp_sb", [128, 2, 2048], BF16)
            R_w = P.res("gw")
            P.dma("pool", lambda e: e.dma_start(out=wpp_sb[:], in_=Wpp.rearrange("p (a b) -> p a b", a=2),
                                                max_dma_last_dim=4096), writes=[R_w])
            ht = [sbuf(st, "g_ht%d" % i, [128, D], F32) for i in range(2)]
            Rht = [P.res("g_ht%d" % i) for i in range(2)]
            rbs = [rms_bufs(st, "g%d" % i) for i in range(2)]
            xn3 = sbuf(st, "xn3", [128, 16, T], BF16)
            Rxn3 = [P.res("xn3_%d" % i) for i in range(NTILE)]
            pt_f = sbuf(st, "pt_f", [128, 256], F32)
            pt_b = sbuf(st, "pt_b", [128, 256], BF16)
            Rptf, Rptb = P.res("pt_f"), P.res("pt_b")
            pTb = sbuf(st, "pTb", [128, 2, T], BF16)
            RpTb = [P.res("pTb%d" % i) for i in range(NTILE)]
            pstp = psum(st, "g_pstp", [128, 1024], BF16)
            Rpstp = P.res("g_pstp")
            wgb = [sbuf(st, "wgb%d" % i, [128, 16, 512], BF16) for i in range(3)]
            Rwgb = [P.res("wgb%d" % i) for i in range(3)]
            psgt = [psum(st, "g_psg%d" % i, [128, 512], F32) for i in range(2)]
            Rpsgt = [P.res("g_psg%d" % i) for i in range(2)]
            pspp = psum(st, "g_psp", [128, 512], F32)
            Rpspp = P.res("g_psp")
            sg = [sbuf(st, "sg%d" % i, [128, 512], F32) for i in range(2)]
            Rsg = [P.res("sg%d" % i) for i in range(2)]
            hb = [sbuf(st, "g_hb%d" % i, [128, 512], F32) for i in range(3)]
            Rhb = [P.res("g_hb%d" % i) for i in range(3)]
            ob = [sbuf(st, "g_ob%d" % i, [128, 512], F32) for i in range(3)]
            Rob = [P.res("g_ob%d" % i) for i in range(3)]
            kcnt = [0]
            ocnt = [0]

            def g_front2(t_first):
                items = []
                for t in (t_first, t_first + 1):
                    k = t % 2
                    row0 = t * 128
                    P.dma("sp", lambda e, k=k, row0=row0: e.dma_start(out=ht[k][:], in_=H2[row0:row0 + 128, :]),
                          writes=[Rht[k]])
                    items.append((ht[k][:], Rht[k], (lambda dc, row0=row0: xn3[:, dc, row0:row0 + 128]), Rxn3[t], rbs[k]))
                rmsnorm_multi(items, 32)
                for t in (t_first, t_first + 1):
                    row0 = t * 128
                    P.dma("sp", lambda e, row0=row0: e.dma_start(out=pt_f[:], in_=pin[row0:row0 + 128, :]), writes=[Rptf])
                    P.op("act", lambda e: e.activation(out=pt_b[:], in_=pt_f[:], func=AF.Copy), reads=[Rptf], writes=[Rptb])
                    for fc in range(2):
                        P.op("pe", lambda e, fc=fc: e.transpose(out=pstp[:, fc * 128:(fc + 1) * 128],
                                                                in_=pt_b[:, fc * 128:(fc + 1) * 128], identity=ident_bf[:]),
                             reads=[Rptb, R_const], writes=[Rpstp])
                    P.op("dve", lambda e, row0=row0: e.tensor_copy(
                        out=pTb[:, :, row0:row0 + 128], in_=pstp[:, 0:256].rearrange("p (a b) -> p a b", a=2)),
                        reads=[Rpstp], writes=[RpTb[t]])

            def g_back(t, nb, wk):
                row0 = t * 128
                kk = kcnt[0] % 2
                kcnt[0] += 1
                okk = ocnt[0] % 3
                ocnt[0] += 1
                P.dma("sp", lambda e: e.dma_start(out=hb[okk][:], in_=H2[row0:row0 + 128, nb * 512:(nb + 1) * 512]),
                      writes=[Rhb[okk]])
                for fc in range(16):
                    P.op("pe", lambda e, fc=fc: e.matmul(
                        out=psgt[kk][:], lhsT=xn3[:, fc, row0:row0 + 128], rhs=wgb[wk][:, fc, :],
                        start=(fc == 0), stop=(fc == 15)), reads=[Rxn3[t], Rwgb[wk]], writes=[Rpsgt[kk]])
                for fc in range(2):
                    P.op("pe", lambda e, fc=fc: e.matmul(
                        out=pspp[:], lhsT=pTb[:, fc, row0:row0 + 128], rhs=wpp_sb[:, fc, nb * 512:(nb + 1) * 512],
                        start=(fc == 0), stop=(fc == 1)), reads=[RpTb[t], R_w], writes=[Rpspp])
                P.op("act", lambda e: e.activation(out=sg[kk][:], in_=psgt[kk][:], func=AF.Sigmoid),
                     reads=[Rpsgt[kk]], writes=[Rsg[kk]])
                P.op("dve", lambda e: e.tensor_tensor(out=sg[kk][:], in0=sg[kk][:], in1=pspp[:], op=ALU.mult),
                     reads=[Rsg[kk], Rpspp], writes=[Rsg[kk]])
                P.op("dve", lambda e: e.tensor_tensor(out=ob[okk][:], in0=sg[kk][:], in1=hb[okk][:], op=ALU.add),
                     reads=[Rsg[kk], Rhb[okk]], writes=[Rob[okk]])
                P.dma("sp", lambda e: e.dma_start(out=out[row0:row0 + 128, nb * 512:(nb + 1) * 512], in_=ob[okk][:]),
                      reads=[Rob[okk]])

            def load_w(nb):
                wk = nb % 3
                P.dma("sp", lambda e: e.dma_start(out=wgb[wk][:], in_=Wgb[nb].rearrange("p (a b) -> p a b", a=16)),
                      reads=[RWg[nb]], writes=[Rwgb[wk]])

            load_w(0)
            g_front2(0)
            load_w(1)
            load_w(2)
            for t in range(0, NTILE, 2):
                if t + 2 < NTILE:
                    g_front2(t + 2)
                g_back(t, 0, 0)
                g_back(t + 1, 0, 0)
            for nb in range(1, 4):
                if nb + 2 < 4:
                    load_w(nb + 2)
                for t in range(NTILE):
                    g_back(t, nb, nb % 3)
            P.emit()
        return nc, None, st0


def _host_layout(inp, b):
    f = np.float32
    d = {}
    d["x"] = np.ascontiguousarray(inp["x"][b], f)
    d["p"] = np.ascontiguousarray(inp["p"][0, b], f)
    d["pos"] = np.ascontiguousarray(inp["positions"][b].reshape(1, T).astype(np.int32))
    return d


_SHARED = {}


def _shared_layout(inp):
    f = np.float32
    s = {}
    cst = np.zeros((128, NCST), f)

    def colmajor(v, n):
        return np.asarray(v, f).reshape(n, 128).T

    cst[:, 0:16] = colmajor(inp["mix_norm_gain"][0], 16)
    cst[:, 16:32] = colmajor(inp["ffn_norm_gain"][0], 16)
    cst[:, 32:48] = colmajor(inp["ple_norm_gain"][0], 16)
    cst[:, 48:52] = colmajor(inp["q_lat_gain"][0], 4)
    cst[:, 52:56] = colmajor(inp["kv_lat_gain"][0], 4)
    cst[:, 56:64] = colmajor(inp["pool_scale"][0], 8)
    qg = np.asarray(inp["q_norm_gain"][0], f)
    kg = np.asarray(inp["k_norm_gain"][0], f)
    cst[:, 64] = qg[0:128]
    cst[:, 65] = np.tile(qg[128:192], 2)
    cst[:, 66] = kg[0:128]
    cst[:, 67] = np.tile(kg[128:192], 2)
    inv_freq = (np.float32(10000.0) ** (-np.arange(0, 64, 2, dtype=np.float32) / np.float32(64))).astype(f)
    cst[:, 68] = np.tile(inv_freq, 4)
    cst[:, 69] = np.tile(np.concatenate([-np.ones(32, f), np.ones(32, f)]), 2)
    cst[:, 70:86] = (1.0 / np.arange(1, 17, dtype=f))[None, :]
    cst[:, 86] = EPS
    s["cst"] = cst
    mats = np.zeros((128, 6, 128), f)
    mats[:, 0, :] = np.eye(128, dtype=f)
    mats[:, 1, :] = 1.0
    mats[0:64, 2, :] = 1.0
    mats[64:128, 3, :] = 1.0
    for m in range(128):
        partner = m + 32 if (m % 64) < 32 else m - 32
        mats[partner, 4, m] = 1.0
    mats[:, 5, :] = np.arange(128, dtype=f)[None, :]
    s["mats"] = mats.reshape(128, 768)
    ntt = TG // 128
    mk = np.zeros((128, ntt, TG), f)
    kk = np.arange(128)[:, None]
    qq = np.arange(TG)[None, :]
    for j in range(ntt):
        mk[:, j, :] = ((qq // 64) >= ((128 * j + kk) // 64)).astype(f)
    s["maskc"] = mk.reshape(128, ntt * TG)
    w_in = np.asarray(inp["w_in"][0], f)
    w_ext = np.concatenate([w_in, w_in[:, 2048:2112]], axis=1)
    s["Win"] = np.ascontiguousarray(w_ext.reshape(16, 128, 17, 128).transpose(2, 1, 0, 3)).reshape(17, 128, 2048)
    wp = np.asarray(inp["w_pool"][0], f)
    s["Wpool"] = np.ascontiguousarray(wp.reshape(4, 2, 128, 256).transpose(2, 0, 1, 3)).reshape(128, 2048)
    wuq = np.asarray(inp["w_uq"][0], f).reshape(512, 8, 192)
    wuq_r = np.concatenate([wuq[:, :, 0:128].reshape(512, 1024), wuq[:, :, 128:192].reshape(512, 512)], axis=1)
    s["Wuq"] = np.ascontiguousarray(wuq_r.reshape(4, 128, 1536).transpose(1, 0, 2)).reshape(128, 4 * 1536)
    wukv = np.asarray(inp["w_ukv"][0], f).reshape(512, 8, 256)
    wuk = wukv[:, :, 0:128].reshape(512, 1024)
    wv = wukv[:, :, 128:256].reshape(512, 1024)
    s["Wuk"] = np.ascontiguousarray(wuk.reshape(4, 128, 1024).transpose(1, 0, 2)).reshape(128, 4096)
    s["Wv"] = np.ascontiguousarray(wv.reshape(4, 128, 1024).transpose(1, 0, 2)).reshape(128, 4096)
    wo = np.asarray(inp["w_out"][0], f)
    s["Wout"] = np.ascontiguousarray(wo.reshape(16, 128, 4, 512).transpose(2, 1, 0, 3)).reshape(4, 128, 8192)
    wpq = np.asarray(inp["w_pq"][0], f)
    s["Wpq"] = np.ascontiguousarray(wpq.reshape(16, 128, 16, 128).transpose(2, 1, 0, 3)).reshape(16, 128, 2048)
    sk = np.stack([np.asarray(inp["sub_k1"][0], f), np.asarray(inp["sub_k2"][0], f)], axis=1)
    s["subk"] = np.ascontiguousarray(sk.transpose(3, 0, 1, 2)).reshape(128, 2048)
    eu = np.asarray(inp["expert_u"][0], f)
    s["UT"] = np.ascontiguousarray(eu.reshape(128, 128, 16, 128).transpose(0, 3, 2, 1)).reshape(128, 128, 2048)
    ev = np.asarray(inp["expert_v"][0], f)
    evl = np.ascontiguousarray(ev.reshape(128, 128, 2, 1024).transpose(2, 0, 1, 3))
    s["EV"] = evl.reshape(256, 128, 1024)
    wg = np.asarray(inp["w_ple_gate"][0], f)
    s["Wg"] = np.ascontiguousarray(wg.reshape(16, 128, 4, 512).transpose(2, 1, 0, 3)).reshape(4, 128, 8192)
    wpp = np.asarray(inp["w_ple_proj"][0], f)
    s["Wpp"] = np.ascontiguousarray(wpp.reshape(2, 128, 2048).transpose(1, 0, 2)).reshape(128, 4096)
    return s


def kernel(**inputs):
    shared = _shared_layout(inputs)
    nc, _, _ = build()
    in_maps = []
    for b in range(8):
        m = dict(shared)
        m.update(_host_layout(inputs, b))
        in_maps.append(m)
    res = run_bass_kernel_spmd(nc, in_maps, core_ids=list(range(8)))
    return np.stack([r["out"] for r in res.results], axis=0).astype(np.float32)
```

```python
import math
from contextlib import ExitStack

import numpy as np
import concourse.bass as bass
import concourse.mybir as mybir
from concourse.bass_utils import run_bass_kernel_spmd

F32 = mybir.dt.float32
BF16 = mybir.dt.bfloat16
I32 = mybir.dt.int32
U32 = mybir.dt.uint32
AF = mybir.ActivationFunctionType
ALU = mybir.AluOpType
AX = mybir.AxisListType

T = 2048
D = 2048
EPS = 1e-6
TG = 256
NG = T // TG
NCST = 88
DBG_CUT = 99
TWO_PI = 2.0 * math.pi
CW1 = 6.28125
_c2 = np.array([TWO_PI - CW1], np.float32).view(np.uint32) & np.uint32(0xFFFFF000)
CW2 = float(_c2.view(np.float32)[0])
CW3 = float(TWO_PI - CW1 - CW2)


class Res:
    __slots__ = ("name", "w", "rd")

    def __init__(self, name):
        self.name = name
        self.w = None
        self.rd = []


class Op:
    __slots__ = ("eng", "fn", "deps", "signal", "sigval", "isdma", "sem", "semval", "prev")


class Prog:
    ENG = {"pe": "tensor", "act": "scalar", "dve": "vector", "pool": "gpsimd", "sp": "sync"}

    def __init__(self, nc, st, ring=8):
        self.nc = nc
        self.sems = {e: st.enter_context(nc.semaphore("s_" + e)) for e in self.ENG}
        self.cnt = {e: 0 for e in self.ENG}
        self.ring = ring
        self.rings = {q: [st.enter_context(nc.semaphore("d_%s%d" % (q, i))) for i in range(ring)]
                      for q in ("sp", "pool", "act")}
        self.ringcnt = {q: [0] * ring for q in self.rings}
        self.ringpos = {q: 0 for q in self.rings}
        self.ops = []
        self.waited = {}
        self.allres = []

    def res(self, name):
        r = Res(name)
        self.allres.append(r)
        return r

    def _add(self, eng, fn, reads, writes, isdma, indep):
        o = Op()
        o.eng = eng
        o.fn = fn
        o.isdma = isdma
        o.signal = False
        o.sigval = None
        deps = []
        for r in reads:
            if r.w is not None:
                deps.append(r.w)
        for w in writes:
            if w.w is not None:
                deps.append(w.w)
            deps.extend(w.rd)
        seen = set()
        out = []
        for d in deps:
            if id(d) in seen:
                continue
            seen.add(id(d))
            if (not d.isdma) and (not isdma) and d.eng == eng and (eng == "pe" or indep):
                continue
            out.append(d)
            if not d.isdma:
                d.signal = True
        o.deps = out
        if isdma:
            q = eng
            slot = self.ringpos[q] % self.ring
            self.ringpos[q] += 1
            o.sem = self.rings[q][slot]
            o.prev = self.ringcnt[q][slot]
            self.ringcnt[q][slot] += 16
            o.semval = self.ringcnt[q][slot]
        for w in writes:
            w.w = o
            w.rd = []
        for r in reads:
            if r in writes:
                continue
            if not isdma:
                r.rd = [x for x in r.rd if x.isdma or x.eng != eng]
            r.rd.append(o)
        self.ops.append(o)
        return o

    _defer = None

    def defer_begin(self):
        self._defer = []

    def defer_end(self):
        d = self._defer
        self._defer = None
        return d

    def op(self, eng, fn, reads=(), writes=(), indep=False):
        if self._defer is not None:
            self._defer.append(lambda: self._add(eng, fn, list(reads), list(writes), False, indep))
            return None
        return self._add(eng, fn, list(reads), list(writes), False, indep)

    def dma(self, q, fn, reads=(), writes=()):
        if self._defer is not None:
            self._defer.append(lambda: self._add(q, fn, list(reads), list(writes), True, False))
            return None
        return self._add(q, fn, list(reads), list(writes), True, False)

    def _wait(self, engobj, e, sem, key, val):
        if val <= 0:
            return
        k = (e, key)
        if self.waited.get(k, 0) >= val:
            return
        self.waited[k] = val
        engobj.wait_ge(sem, val)

    def emit(self):
        ops = self.ops
        self.ops = []
        for o in ops:
            if (not o.isdma) and o.signal:
                self.cnt[o.eng] += 1
                o.sigval = self.cnt[o.eng]
        with self.nc.Block() as block:
            for e, attr in self.ENG.items():
                mine = [o for o in ops if o.eng == e]
                if not mine:
                    continue

                def body(engobj, mine=mine, e=e):
                    for o in mine:
                        for d in o.deps:
                            if d.isdma:
                                self._wait(engobj, e, d.sem, id(d.sem), d.semval)
                            else:
                                self._wait(engobj, e, self.sems[d.eng], d.eng, d.sigval)
                        if o.isdma:
                            self._wait(engobj, e, o.sem, id(o.sem), o.prev)
                            o.fn(engobj).then_inc(o.sem, 16)
                        else:
                            ins = o.fn(engobj)
                            if o.signal:
                                ins.then_inc(self.sems[e], 1)
                    if e in self.rings:
                        for i, s in enumerate(self.rings[e]):
                            self._wait(engobj, e, s, id(s), self.ringcnt[e][i])

                getattr(block, attr)(body)
        for r in self.allres:
            r.w = None
            r.rd = []


def build(stop_after=None, dbg=()):
    nc = bass.Bass("TRN2", target_bir_lowering=False)

    def din(name, shape, dt=F32):
        return nc.dram_tensor(name, list(shape), dt, kind="ExternalInput").ap()

    def dscr(name, shape, dt):
        kind = "ExternalOutput" if name in dbg else "Internal"
        return nc.dram_tensor(name, list(shape), dt, kind=kind).ap()

    x = din("x", [T, D])
    pin = din("p", [T, 256])
    pos = din("pos", [1, T], I32)
    cst = din("cst", [128, NCST])
    mats = din("mats", [128, 6 * 128])
    maskc = din("maskc", [128, 2 * TG])
    Win = din("Win", [17, 128, 2048])
    Wpool = din("Wpool", [128, 2048])
    Wuq = din("Wuq", [128, 4 * 1536])
    Wuk = din("Wuk", [128, 4096])
    Wv = din("Wv", [128, 4096])
    Wout = din("Wout", [4, 128, 8192])
    Wpq = din("Wpq", [16, 128, 2048])
    subk = din("subk", [128, 2048])
    UT = din("UT", [128, 128, 2048])
    EV = din("EV", [256, 128, 1024])
    Wg = din("Wg", [4, 128, 8192])
    Wpp = din("Wpp", [128, 4096])
    out = nc.dram_tensor("out", [T, D], F32, kind="ExternalOutput").ap()

    Winb = dscr("Winb", [17, 128, 2048], BF16)
    Woutb = dscr("Woutb", [4, 128, 8192], BF16)
    Wpqb = dscr("Wpqb", [16, 128, 2048], BF16)
    Wgb = dscr("Wgb", [4, 128, 8192], BF16)
    UTb = dscr("UTb", [128, 128, 2048], BF16)
    EVb = dscr("EVb", [256, 128, 1024], BF16)
    CQ = dscr("CQ", [NG, 128, 4 * TG], BF16)
    YP = dscr("YP", [NG, 128, 8 * TG], BF16)
    H1 = dscr("H1", [T, D], F32)
    H2 = dscr("H2", [T, D], F32)
    XN2 = dscr("XN2", [NG, 128, 16 * TG], BF16)
    IRs = dscr("IRs", [3, 128, T], F32)

    st0 = ExitStack()
    with st0:
        P = Prog(nc, st0)

        def sbuf(st, name, shape, dt):
            return st.enter_context(nc.sbuf_tensor(name, list(shape), dt))

        def psum(st, name, shape, dt=F32):
            return st.enter_context(nc.psum_tensor(name, list(shape), dt))

        cst_sb = sbuf(st0, "cst_sb", [128, NCST], F32)
        mats_sb = sbuf(st0, "mats_sb", [128, 6 * 128], F32)
        ident_bf = sbuf(st0, "ident_bf", [128, 128], BF16)
        ones_bf = sbuf(st0, "ones_bf", [128, 128], BF16)
        iota_bf = sbuf(st0, "iota_bf", [128, 128], BF16)
        stKV = ExitStack()
        cosT = sbuf(stKV, "cosT", [128, T], F32)
        sinT = sbuf(stKV, "sinT", [128, T], F32)
        R_const = P.res("const")
        R_rope = P.res("rope")
        ident_f = mats_sb[:, 0:128]
        ones_f = mats_sb[:, 128:256]
        Llo_f = mats_sb[:, 256:384]
        Lhi_f = mats_sb[:, 384:512]
        perm_f = mats_sb[:, 512:640]
        iota_f = mats_sb[:, 640:768]

        RWin = [P.res("Win%d" % c) for c in range(17)]
        RWout = [P.res("Wout%d" % c) for c in range(4)]
        RWpq = [P.res("Wpq%d" % c) for c in range(16)]
        RWg = [P.res("Wg%d" % c) for c in range(4)]
        pro_list = []
        for c in range(17):
            pro_list.append((Winb[c], Win[c], RWin[c]))
        for nb in range(4):
            for q in range(4):
                pro_list.append((Woutb[nb, 32 * q:32 * q + 32, :], Wout[nb, 32 * q:32 * q + 32, :], RWout[nb]))
        for c in range(16):
            pro_list.append((Wpqb[c], Wpq[c], RWpq[c]))
        for nb in range(4):
            for q in range(4):
                pro_list.append((Wgb[nb, 32 * q:32 * q + 32, :], Wg[nb, 32 * q:32 * q + 32, :], RWg[nb]))
        for i in range(128):
            pro_list.append((UTb[i], UT[i], None))
            pro_list.append((EVb[2 * i:2 * i + 2], EV[2 * i:2 * i + 2], None))
        pro_state = {"i": 0}

        def pro_some(n):
            for _ in range(n):
                if pro_state["i"] >= len(pro_list):
                    return
                o_, i_, r_ = pro_list[pro_state["i"]]
                pro_state["i"] += 1
                P.dma("pool", lambda e, o_=o_, i_=i_: e.dma_start(out=o_, in_=i_, max_dma_last_dim=4096),
                      writes=([r_] if r_ is not None else []))

        with ExitStack() as st:
            posi = sbuf(st, "posi", [128, T], I32)
            ang = sbuf(st, "ang", [128, T], F32)
            kf = sbuf(st, "kf", [128, T], F32)
            ki = sbuf(st, "ki", [128, T], I32)
            rr = sbuf(st, "rr", [128, T], F32)
            r2 = sbuf(st, "r2", [128, T], F32)
            Rp, Ra, Rk, Rki, Rr, Rr2 = [P.res(n) for n in ("posi", "ang", "kf", "ki", "rr", "r2")]
            P.dma("sp", lambda e: e.dma_start(out=cst_sb[:], in_=cst), writes=[R_const])
            P.dma("sp", lambda e: e.dma_start(out=mats_sb[:], in_=mats), writes=[R_const])
            P.dma("sp", lambda e: e.dma_start(out=posi[:], in_=pos.to_broadcast([128, T])), writes=[Rp])
            P.op("dve", lambda e: e.tensor_copy(out=ident_bf[:], in_=ident_f), reads=[R_const], writes=[R_const])
            P.op("dve", lambda e: e.tensor_copy(out=ones_bf[:], in_=ones_f), reads=[R_const], writes=[R_const])
            P.op("dve", lambda e: e.tensor_copy(out=iota_bf[:], in_=iota_f), reads=[R_const], writes=[R_const])
            P.op("dve", lambda e: e.tensor_copy(out=ang[:], in_=posi[:]), reads=[Rp], writes=[Ra])
            P.op("dve", lambda e: e.tensor_scalar(out=ang[:], in0=ang[:], scalar1=cst_sb[:, 68:69], scalar2=None,
                                                  op0=ALU.mult), reads=[Ra, R_const], writes=[Ra])
            P.op("dve", lambda e: e.tensor_scalar(out=kf[:], in0=ang[:], scalar1=1.0 / TWO_PI, scalar2=None,
                                                  op0=ALU.mult), reads=[Ra], writes=[Rk])
            P.op("dve", lambda e: e.tensor_copy(out=ki[:], in_=kf[:]), reads=[Rk], writes=[Rki])
            P.op("dve", lambda e: e.tensor_copy(out=kf[:], in_=ki[:]), reads=[Rki], writes=[Rk])
            P.op("dve", lambda e: e.scalar_tensor_tensor(out=rr[:], in0=kf[:], scalar=-CW1, in1=ang[:],
                                                         op0=ALU.mult, op1=ALU.add), reads=[Rk, Ra], writes=[Rr])
            P.op("dve", lambda e: e.scalar_tensor_tensor(out=r2[:], in0=kf[:], scalar=-CW2, in1=rr[:],
                                                         op0=ALU.mult, op1=ALU.add), reads=[Rk, Rr], writes=[Rr2])
            P.op("dve", lambda e: e.scalar_tensor_tensor(out=rr[:], in0=kf[:], scalar=-CW3, in1=r2[:],
                                                         op0=ALU.mult, op1=ALU.add), reads=[Rk, Rr2], writes=[Rr])
            def wrap_sin(dst, shift, sign_col):
                P.op("dve", lambda e: e.tensor_scalar(out=r2[:], in0=rr[:], scalar1=shift, scalar2=None, op0=ALU.add),
                     reads=[Rr], writes=[Rr2])
                P.op("dve", lambda e: e.tensor_scalar(out=kf[:], in0=r2[:], scalar1=math.pi, scalar2=-TWO_PI,
                                                      op0=ALU.is_gt, op1=ALU.mult), reads=[Rr2], writes=[Rk])
                P.op("dve", lambda e: e.tensor_tensor(out=r2[:], in0=r2[:], in1=kf[:], op=ALU.add),
                     reads=[Rr2, Rk], writes=[Rr2])
                P.op("dve", lambda e: e.tensor_scalar(out=kf[:], in0=r2[:], scalar1=-math.pi, scalar2=TWO_PI,
                                                      op0=ALU.is_lt, op1=ALU.mult), reads=[Rr2], writes=[Rk])
                P.op("dve", lambda e: e.tensor_tensor(out=r2[:], in0=r2[:], in1=kf[:], op=ALU.add),
                     reads=[Rr2, Rk], writes=[Rr2])
                P.op("dve", lambda e: e.tensor_scalar(out=r2[:], in0=r2[:], scalar1=math.pi, scalar2=-math.pi,
                                                      op0=ALU.min, op1=ALU.max), reads=[Rr2], writes=[Rr2])
                P.op("act", lambda e: e.activation(out=dst[:], in_=r2[:], func=AF.Sin), reads=[Rr2], writes=[R_rope])
                if sign_col is not None:
                    P.op("dve", lambda e: e.tensor_scalar(out=dst[:], in0=dst[:], scalar1=cst_sb[:, sign_col:sign_col + 1],
                                                          scalar2=None, op0=ALU.mult),
                         reads=[R_rope, R_const], writes=[R_rope])

            wrap_sin(sinT, 0.0, 69)
            wrap_sin(cosT, math.pi / 2, None)
            pro_some(17)
            P.emit()

        def rmsnorm_multi(items, g0, all_act=False):
            for (src, Rsrc, dstfn, Rdst, bufs) in items:
                xs, Rxs, ss, ms, Rst, pst, Rpst = bufs
                P.op("act", lambda e, xs=xs, src=src, ss=ss: e.activation(out=xs[:], in_=src, func=AF.Square,
                                                                          accum_out=ss[:, 0:1]),
                     reads=[Rsrc], writes=[Rxs, Rst])
            for (src, Rsrc, dstfn, Rdst, bufs) in items:
                xs, Rxs, ss, ms, Rst, pst, Rpst = bufs
                P.op("act", lambda e, ss=ss, ms=ms: e.activation(out=ms[:, 0:1], in_=ss[:, 0:1], func=AF.Ln,
                                                                 scale=1.0 / D, bias=cst_sb[:, 86:87]),
                     reads=[Rst, R_const], writes=[Rst])
            for (src, Rsrc, dstfn, Rdst, bufs) in items:
                xs, Rxs, ss, ms, Rst, pst, Rpst = bufs
                P.op("act", lambda e, ms=ms: e.activation(out=ms[:, 2:3], in_=ms[:, 0:1], func=AF.Exp, scale=-0.5),
                     reads=[Rst], writes=[Rst])
            for (src, Rsrc, dstfn, Rdst, bufs) in items:
                xs, Rxs, ss, ms, Rst, pst, Rpst = bufs
                P.op("act", lambda e, xs=xs, src=src, ms=ms: e.activation(out=xs[:], in_=src, func=AF.Copy,
                                                                          scale=ms[:, 2:3]),
                     reads=[Rsrc, Rst], writes=[Rxs])
            for half in range(2):
                for (src, Rsrc, dstfn, Rdst, bufs) in items:
                    xs, Rxs, ss, ms, Rst, pst, Rpst = bufs
                    for j in range(8):
                        dc = half * 8 + j
                        P.op("pe", lambda e, dc=dc, j=j, half=half, pst=pst, xs=xs: e.transpose(
                            out=pst[half][:, j * 128:(j + 1) * 128], in_=xs[:, dc * 128:(dc + 1) * 128],
                            identity=ident_bf[:]), reads=[Rxs, R_const], writes=[Rpst[half]])
                for (src, Rsrc, dstfn, Rdst, bufs) in items:
                    xs, Rxs, ss, ms, Rst, pst, Rpst = bufs
                    for j in range(8):
                        dc = half * 8 + j
                        if j % 2 == 0 and not all_act:
                            P.op("dve", lambda e, dc=dc, j=j, half=half, pst=pst, dstfn=dstfn: e.tensor_scalar(
                                out=dstfn(dc), in0=pst[half][:, j * 128:(j + 1) * 128],
                                scalar1=cst_sb[:, g0 + dc:g0 + dc + 1], scalar2=None, op0=ALU.mult),
                                reads=[Rpst[half], R_const], writes=[Rdst], indep=True)
                        else:
                            P.op("act", lambda e, dc=dc, j=j, half=half, pst=pst, dstfn=dstfn: e.activation(
                                out=dstfn(dc), in_=pst[half][:, j * 128:(j + 1) * 128], func=AF.Copy,
                                scale=cst_sb[:, g0 + dc:g0 + dc + 1]),
                                reads=[Rpst[half], R_const], writes=[Rdst], indep=True)

        def rmsnorm_T(src, Rsrc, g0, dstfn, Rdst, bufs, tag):
            rmsnorm_multi([(src, Rsrc, dstfn, Rdst, bufs)], g0)

        def rms_bufs(st, tag):
            xs = sbuf(st, "rn_xs" + tag, [128, D], BF16)
            ss = sbuf(st, "rn_ss" + tag, [128, 1], F32)
            ms = sbuf(st, "rn_ms" + tag, [128, 4], F32)
            pst = [psum(st, "rn_ps%d%s" % (h, tag), [128, 1024], BF16) for h in range(2)]
            return (xs, P.res("rn_xs" + tag), ss, ms, P.res("rn_st" + tag), pst,
                    [P.res("rn_ps0" + tag), P.res("rn_ps1" + tag)])

        KT = sbuf(stKV, "KT", [128, 8, T], BF16)
        KRT = sbuf(stKV, "KRT", [128, T], BF16)
        Vsb = sbuf(stKV, "Vsb", [128, 16, 1024], BF16)
        rkcol = sbuf(stKV, "rkcol", [128, 16, 8], F32)
        R_K = [P.res("K%d" % g) for g in range(NG)]
        R_V = [P.res("V%d" % g) for g in range(NG)]

        with ExitStack() as st:
            wuk_sb = sbuf(st, "wuk_sb", [128, 4, 1024], BF16)
            wv_sb = sbuf(st, "wv_sb", [128, 4, 1024], BF16)
            wpool_sb = sbuf(st, "wpool_sb", [128, 4, 2, 256], BF16)
            R_w = P.res("m1w")
            P.dma("pool", lambda e: e.dma_start(out=wuk_sb[:], in_=Wuk.rearrange("p (a b) -> p a b", a=4),
                                                max_dma_last_dim=4096), writes=[R_w])
            P.dma("pool", lambda e: e.dma_start(out=wv_sb[:], in_=Wv.rearrange("p (a b) -> p a b", a=4),
                                                max_dma_last_dim=4096), writes=[R_w])
            P.dma("pool", lambda e: e.dma_start(out=wpool_sb[:], in_=Wpool.rearrange("p (a b c) -> p a b c", a=4, b=2),
                                                max_dma_last_dim=1024), writes=[R_w])
            xt = [sbuf(st, "m1_xt%d" % i, [128, D], F32) for i in range(2)]
            Rxt = [P.res("m1_xt%d" % i) for i in range(2)]
            rbs = [rms_bufs(st, "m1_%d" % i) for i in range(2)]
            xnT = sbuf(st, "xnT", [128, 16, TG], BF16)
            RxnT = P.res("xnT")
            wbuf = [sbuf(st, "m1_wb%d" % i, [128, 16, 128], BF16) for i in range(6)]
            Rwb = [P.res("m1_wb%d" % i) for i in range(6)]
            psz = [psum(st, "psz%d" % i, [128, 512], F32) for i in range(2)]
            Rpsz = [P.res("psz%d" % i) for i in range(2)]
            pz_ring = [psz[0][:, 0:TG], psz[1][:, 0:TG]]
            Rpz_ring = [Rpsz[0], Rpsz[1]]
            for k_ in range(2):
                for h_ in range(2):
                    pz_ring.append(rbs[k_][5][h_][:].bitcast(F32)[:, 0:TG])
                    Rpz_ring.append(rbs[k_][6][h_])
            psm = [psum(st, "psm%d" % i, [128, 512], F32) for i in range(2)]
            Rpsm = [P.res("psm%d" % i) for i in range(2)]
            halo = sbuf(st, "halo", [128, 8, 16], F32)
            Rhalo = [P.res("halo%d" % c) for c in range(8)]
            zpad = [sbuf(st, "zpad%d" % i, [128, 16 + TG], F32) for i in range(2)]
            Rzp = [P.res("zpad%d" % i) for i in range(2)]
            abuf = [sbuf(st, "abuf%d" % i, [128, 16 + TG], F32) for i in range(2)]
            Rab = [P.res("abuf%d" % i) for i in range(2)]
            fix16 = sbuf(st, "fix16", [128, 16], F32)
            Rfix = P.res("fix16")
            dbuf = sbuf(st, "dbuf", [128, 2, TG], BF16)
            Rdb = [P.res("dbuf%d" % i) for i in range(2)]
            ypo = sbuf(st, "ypo", [128, 8, TG], BF16)
            Rypo = P.res("ypo")
            zlat = sbuf(st, "zlat", [128, 4, TG], F32)
            Rzlat = P.res("zlat")
            sq = [sbuf(st, "m1_sq%d" % i, [128, TG], F32) for i in range(2)]
            Rsq = [P.res("m1_sq%d" % i) for i in range(2)]
            rlat = sbuf(st, "rlat", [128, TG], F32)
            Rrlat = P.res("rlat")
            cqn = sbuf(st, "cqn", [128, 4, TG], BF16)
            Rcqn = P.res("cqn")
            ckvn = sbuf(st, "ckvn", [128, 4, TG], BF16)
            Rckvn = P.res("ckvn")
            krg = sbuf(st, "krg", [128, TG], F32)
            krsq = sbuf(st, "krsq", [128, TG], F32)
            Rkr = P.res("kr")
            t1 = sbuf(st, "m1_t1", [128, TG], F32)
            t2 = sbuf(st, "m1_t2", [128, TG], F32)
            Rt1, Rt2 = P.res("m1_t1"), P.res("m1_t2")
            colb = sbuf(st, "colb", [128, 3, 16], F32)
            Rcolb = P.res("colb")

            P.op("dve", lambda e: e.memset(halo[:], 0.0), writes=Rhalo)
            dq = []

            def dq_tick():
                for it in dq:
                    it[0] -= 1
                while dq and dq[0][0] <= 0:
                    for f_ in dq.pop(0)[1]:
                        f_()

            def dq_flush():
                while dq:
                    for f_ in dq.pop(0)[1]:
                        f_()

            wcnt = [0]
            pcnt = [0]
            mcnt = [0]

            for g in range(NG):
                t0 = g * TG
                items = []
                for tt in range(TG // 128):
                    k = tt % 2
                    row0 = t0 + tt * 128
                    P.dma("sp", lambda e, k=k, row0=row0: e.dma_start(out=xt[k][:], in_=x[row0:row0 + 128, :]),
                          writes=[Rxt[k]])
                    items.append((xt[k][:], Rxt[k], (lambda dc, tt=tt: xnT[:, dc, tt * 128:(tt + 1) * 128]), RxnT, rbs[k]))
                rmsnorm_multi(items, 0)
                pro_some(8)
                for c in range(17):
                    wk = wcnt[0] % 6
                    wcnt[0] += 1
                    P.dma("sp", lambda e, wk=wk, c=c: e.dma_start(
                        out=wbuf[wk][:], in_=Winb[c].rearrange("p (a b) -> p a b", a=16)),
                        reads=[RWin[c]], writes=[Rwb[wk]])
                    pk = pcnt[0] % len(pz_ring)
                    pcnt[0] += 1
                    pz = pz_ring[pk]
                    for dc in range(16):
                        P.op("pe", lambda e, wk=wk, dc=dc, pz=pz: e.matmul(
                            out=pz, lhsT=wbuf[wk][:, dc, :], rhs=xnT[:, dc, :], start=(dc == 0), stop=(dc == 15)),
                            reads=[Rwb[wk], RxnT], writes=[Rpz_ring[pk]])
                    dq_tick()
                    if c < 8:
                        grp = c // 2
                        S = grp + 1
                        w = 2 ** S
                        zk = c % 2
                        zp = zpad[zk]
                        P.op("act", lambda e, zp=zp, pz=pz: e.activation(out=zp[:, 16:16 + TG], in_=pz, func=AF.Copy),
                             reads=[Rpz_ring[pk]], writes=[Rzp[zk]])
                        P.op("dve", lambda e, zp=zp, c=c: e.tensor_copy(out=zp[:, 0:16], in_=halo[:, c, :]),
                             reads=[Rhalo[c]], writes=[Rzp[zk]])
                        P.op("dve", lambda e, zp=zp, c=c: e.tensor_copy(out=halo[:, c, :], in_=zp[:, TG:TG + 16]),
                             reads=[Rzp[zk]], writes=[Rhalo[c]])
                        prev, Rprev = zp, Rzp[zk]
                        L = 16 + TG
                        for s in range(1, S + 1):
                            sh = 2 ** (s - 1)
                            lo = 2 ** s - 1
                            ab = abuf[s % 2]
                            P.op("dve", lambda e, ab=ab, prev=prev, lo=lo, sh=sh, L=L: e.tensor_tensor(
                                out=ab[:, lo:L], in0=prev[:, lo:L], in1=prev[:, lo - sh:L - sh], op=ALU.add),
                                reads=[Rprev], writes=[Rab[s % 2]])
                            prev, Rprev = ab, Rab[s % 2]
                        P.op("dve", lambda e, prev=prev, zp=zp, zk=zk, w=w: e.scalar_tensor_tensor(
                            out=dbuf[:, zk, :], in0=prev[:, 16:16 + TG], scalar=1.0 / w, in1=zp[:, 16:16 + TG],
                            op0=ALU.mult, op1=ALU.subtract), reads=[Rprev, Rzp[zk]], writes=[Rdb[zk]])
                        if g == 0:
                            P.op("dve", lambda e, prev=prev, w=w: e.tensor_tensor(
                                out=fix16[:, 0:w - 1], in0=prev[:, 16:16 + w - 1], in1=cst_sb[:, 70:70 + w - 1],
                                op=ALU.mult), reads=[Rprev, R_const], writes=[Rfix])
                            P.op("dve", lambda e, zp=zp, zk=zk, w=w: e.tensor_tensor(
                                out=dbuf[:, zk, 0:w - 1], in0=fix16[:, 0:w - 1], in1=zp[:, 16:16 + w - 1],
                                op=ALU.subtract), reads=[Rfix, Rzp[zk]], writes=[Rdb[zk]])
                        if c % 2 == 1:
                            P.defer_begin()
                            for oc in range(2):
                                mk = mcnt[0] % 2
                                mcnt[0] += 1
                                pm = psm[mk][:, 0:TG]
                                for ic in range(2):
                                    P.op("pe", lambda e, pm=pm, grp=grp, ic=ic, oc=oc: e.matmul(
                                        out=pm, lhsT=wpool_sb[:, grp, ic, oc * 128:(oc + 1) * 128], rhs=dbuf[:, ic, :],
                                        start=(ic == 0), stop=(ic == 1)), reads=[R_w, Rdb[ic]], writes=[Rpsm[mk]])
                                cc = 2 * grp + oc
                                P.op("act", lambda e, pm=pm, cc=cc: e.activation(
                                    out=ypo[:, cc, :], in_=pm, func=AF.Copy, scale=cst_sb[:, 56 + cc:57 + cc]),
                                    reads=[Rpsm[mk], R_const], writes=[Rypo], indep=True)
                            dq.append([1, P.defer_end()])
                    elif c < 16:
                        j = (c - 8) % 4
                        isq = c < 12
                        sk = c % 2
                        P.op("act", lambda e, j=j, pz=pz: e.activation(out=zlat[:, j, :], in_=pz, func=AF.Copy),
                             reads=[Rpz_ring[pk]], writes=[Rzlat], indep=True)
                        P.op("act", lambda e, sk=sk, pz=pz: e.activation(out=sq[sk][:], in_=pz, func=AF.Square),
                             reads=[Rpz_ring[pk]], writes=[Rsq[sk]])
                        P.defer_begin()
                        if j == 0:
                            smk = mcnt[0] % 2
                            mcnt[0] += 1
                        pm = psm[smk][:, 0:TG]
                        P.op("pe", lambda e, pm=pm, sk=sk, j=j: e.matmul(out=pm, lhsT=ones_f, rhs=sq[sk][:],
                                                                          start=(j == 0), stop=(j == 3)),
                             reads=[Rsq[sk], R_const], writes=[Rpsm[smk]])
                        if j == 3:
                            P.op("act", lambda e, pm=pm: e.activation(out=rlat[:], in_=pm, func=AF.Ln, scale=1.0 / 512,
                                                                        bias=cst_sb[:, 86:87]),
                                 reads=[Rpsm[smk], R_const], writes=[Rrlat])
                            P.op("act", lambda e: e.activation(out=rlat[:], in_=rlat[:], func=AF.Exp, scale=-0.5),
                                 reads=[Rrlat], writes=[Rrlat])
                            dst, Rd, gc = (cqn, Rcqn, 48) if isq else (ckvn, Rckvn, 52)
                            for jj in range(4):
                                P.op("dve", lambda e, dst=dst, jj=jj, gc=gc: e.scalar_tensor_tensor(
                                    out=dst[:, jj, :], in0=zlat[:, jj, :], scalar=cst_sb[:, gc + jj:gc + jj + 1],
                                    in1=rlat[:], op0=ALU.mult, op1=ALU.mult),
                                    reads=[Rzlat, Rrlat, R_const], writes=[Rd], indep=(jj > 0))
                            if isq:
                                P.dma("sp", lambda e, g=g: e.dma_start(
                                    out=CQ[g].rearrange("p (a b) -> p a b", a=4), in_=cqn[:]), reads=[Rcqn])
                        dq.append([1, P.defer_end()])
                    else:
                        P.op("act", lambda e, pz=pz: e.activation(out=krg[:], in_=pz, func=AF.Copy,
                                                                    scale=cst_sb[:, 67:68]),
                             reads=[Rpz_ring[pk], R_const], writes=[Rkr])
                        P.op("act", lambda e, pz=pz: e.activation(out=krsq[:], in_=pz, func=AF.Square),
                             reads=[Rpz_ring[pk]], writes=[Rkr])
                dq_flush()
                P.dma("sp", lambda e, g=g: e.dma_start(out=YP[g].rearrange("p (a b) -> p a b", a=8), in_=ypo[:]),
                      reads=[Rypo])
                pro_some(6)
                mkc = mcnt[0] % 2
                mcnt[0] += 1
                pcol = psm[mkc]
                ntt = TG // 128
                for h in range(8):
                    pk = pcnt[0] % len(pz_ring)
                    pcnt[0] += 1
                    pz = pz_ring[pk]
                    for fc in range(4):
                        P.op("pe", lambda e, pz=pz, fc=fc, h=h: e.matmul(
                            out=pz, lhsT=wuk_sb[:, fc, h * 128:(h + 1) * 128], rhs=ckvn[:, fc, :],
                            start=(fc == 0), stop=(fc == 3)), reads=[R_w, Rckvn], writes=[Rpz_ring[pk]])
                    dq_tick()
                    P.op("act", lambda e, pz=pz, h=h, t0=t0: e.activation(
                        out=KT[:, h, t0:t0 + TG], in_=pz, func=AF.Copy, scale=cst_sb[:, 66:67]),
                        reads=[Rpz_ring[pk], R_const], writes=[R_K[g]], indep=True)
                    sk = h % 2
                    P.op("act", lambda e, pz=pz, sk=sk: e.activation(out=sq[sk][:], in_=pz, func=AF.Square),
                         reads=[Rpz_ring[pk]], writes=[Rsq[sk]])
                    P.defer_begin()
                    for tt in range(ntt):
                        col = (h * ntt + tt) * 2
                        P.op("pe", lambda e, sk=sk, tt=tt, col=col: e.matmul(
                            out=pcol[:, col:col + 2], lhsT=sq[sk][:, tt * 128:(tt + 1) * 128], rhs=ones_f[:, 0:2],
                            start=True, stop=False), reads=[Rsq[sk], R_const], writes=[Rpsm[mkc]])
                        P.op("pe", lambda e, tt=tt, col=col: e.matmul(
                            out=pcol[:, col:col + 2], lhsT=krsq[0:64, tt * 128:(tt + 1) * 128], rhs=ones_f[0:64, 0:2],
                            start=False, stop=True), reads=[Rkr, R_const], writes=[Rpsm[mkc]])
                    dq.append([1, P.defer_end()])
                dq_flush()
                nc_ = 8 * ntt
                P.op("dve", lambda e, nc_=nc_: e.tensor_scalar(
                    out=colb[:, 0, 0:nc_], in0=pcol[:, 0:2 * nc_].rearrange("p (a b) -> p a b", b=2)[:, :, 0],
                    scalar1=1.0, scalar2=192.0 * EPS, op0=ALU.mult, op1=ALU.add), reads=[Rpsm[mkc]], writes=[Rcolb])
                P.op("act", lambda e, nc_=nc_: e.activation(out=colb[:, 1, 0:nc_], in_=colb[:, 0, 0:nc_], func=AF.Ln),
                     reads=[Rcolb], writes=[Rcolb])
                P.op("act", lambda e, nc_=nc_: e.activation(out=colb[:, 2, 0:nc_], in_=colb[:, 1, 0:nc_], func=AF.Exp,
                                                            scale=-0.5),
                     reads=[Rcolb], writes=[Rcolb])
                P.op("dve", lambda e, g=g, ntt=ntt, nc_=nc_: e.tensor_copy(
                    out=rkcol[:, g * ntt:(g + 1) * ntt, :].rearrange("p t h -> p h t"),
                    in_=colb[:, 2, 0:nc_].rearrange("p (h t) -> p h t", t=ntt)), reads=[Rcolb], writes=[R_K[g]])
                mk = mcnt[0] % 2
                mcnt[0] += 1
                pm = psm[mk][:, 0:TG]
                P.op("pe", lambda e, pm=pm: e.matmul(out=pm, lhsT=perm_f, rhs=krg[:], start=True, stop=True),
                     reads=[Rkr, R_const], writes=[Rpsm[mk]])
                P.op("dve", lambda e, t0=t0: e.tensor_tensor(out=t1[:], in0=krg[:], in1=cosT[:, t0:t0 + TG], op=ALU.mult),
                     reads=[Rkr, R_rope], writes=[Rt1])
                P.op("dve", lambda e, pm=pm, t0=t0: e.tensor_tensor(out=t2[:], in0=pm, in1=sinT[:, t0:t0 + TG],
                                                                     op=ALU.mult),
                     reads=[Rpsm[mk], R_rope], writes=[Rt2])
                P.op("dve", lambda e, t0=t0: e.tensor_tensor(out=KRT[:, t0:t0 + TG], in0=t1[:], in1=t2[:], op=ALU.add),
                     reads=[Rt1, Rt2], writes=[R_K[g]])
                for tt in range(ntt):
                    for nb in range(2):
                        mk = mcnt[0] % 2
                        mcnt[0] += 1
                        for fc in range(4):
                            P.op("pe", lambda e, mk=mk, fc=fc, tt=tt, nb=nb: e.matmul(
                                out=psm[mk][:], lhsT=ckvn[:, fc, tt * 128:(tt + 1) * 128],
                                rhs=wv_sb[:, fc, nb * 512:(nb + 1) * 512], start=(fc == 0), stop=(fc == 3)),
                                reads=[Rckvn, R_w], writes=[Rpsm[mk]])
                        P.op("act", lambda e, mk=mk, tt=tt, nb=nb, g=g, ntt=ntt: e.activation(
                            out=Vsb[:, g * ntt + tt, nb * 512:(nb + 1) * 512], in_=psm[mk][:], func=AF.Copy),
                            reads=[Rpsm[mk]], writes=[R_V[g]], indep=True)
            P.emit()
        if stop_after == "M1":
            stKV.close()
            return nc, None, st0

        with ExitStack() as st:
            wuq_sb = sbuf(st, "wuq_sb", [128, 4, 1536], BF16)
            mask_f = sbuf(st, "mask_f", [128, 2, TG], F32)
            mask_b = sbuf(st, "mask_b", [128, 2, TG], BF16)
            R_w = P.res("m2w")
            P.dma("pool", lambda e: e.dma_start(out=wuq_sb[:], in_=Wuq.rearrange("p (a b) -> p a b", a=4),
                                                max_dma_last_dim=2048), writes=[R_w])
            P.dma("sp", lambda e: e.dma_start(out=mask_f[:], in_=maskc.rearrange("p (a b) -> p a b", a=2)),
                  writes=[R_w])
            P.op("dve", lambda e: e.tensor_copy(out=mask_b[:], in_=mask_f[:]), reads=[R_w], writes=[R_w])
            cqn = sbuf(st, "m2_cqn", [128, 4, TG], BF16)
            Rcqn = P.res("m2_cqn")
            ymix = sbuf(st, "ymix", [128, 16, TG], BF16)
            Rypool = P.res("ymix_pool")
            Rymla = P.res("ymix_mla")
            psq = [psum(st, "psq%d" % i, [128, 512], F32) for i in range(2)]
            Rpsq = [P.res("psq%d" % i) for i in range(2)]
            pss = [psum(st, "pss%d" % i, [128, 512], F32) for i in range(2)]
            Rpss = [P.res("pss%d" % i) for i in range(2)]
            pso = psum(st, "pso", [128, 512], F32)
            psd = psum(st, "psd", [128, 512], F32)
            Rpso, Rpsd = P.res("pso"), P.res("psd")
            psh = [psum(st, "psh%d" % i, [128, 512], F32) for i in range(2)]
            Rpsh = [P.res("psh%d" % i) for i in range(2)]
            qtmp = [sbuf(st, "qtmp%d" % i, [128, TG], F32) for i in range(2)]
            sqq = [sbuf(st, "sqq%d" % i, [128, TG], F32) for i in range(2)]
            rq = [sbuf(st, "rq%d" % i, [128, TG], F32) for i in range(2)]
            Rqtmp = [P.res("qtmp%d" % i) for i in range(2)]
            Rsqq = [P.res("sqq%d" % i) for i in range(2)]
            Rrq = [P.res("rq%d" % i) for i in range(2)]
            qrg = sbuf(st, "qrg", [128, TG], F32)
            sqr = sbuf(st, "sqr", [128, TG], F32)
            Rqrg, Rsqr = P.res("qrg"), P.res("sqr")
            t1 = sbuf(st, "m2_t1", [128, TG], F32)
            t2 = sbuf(st, "m2_t2", [128, TG], F32)
            Rt1, Rt2 = P.res("m2_t1"), P.res("m2_t2")
            QTs = [sbuf(st, "QT%d" % i, [128, TG], BF16) for i in range(4)]
            RQTs = [P.res("QT%d" % i) for i in range(4)]
            QRTs = [sbuf(st, "QRT%d" % i, [128, TG], BF16) for i in range(2)]
            RQRTs = [P.res("QRT%d" % i) for i in range(2)]
            pT = [sbuf(st, "pT%d" % i, [128, TG], BF16) for i in range(3)]
            RpT = [P.res("pT%d" % i) for i in range(3)]
            rec = sbuf(st, "rec", [128, TG], F32)
            Rrec = P.res("rec")
            wob = [sbuf(st, "wob%d" % i, [128, 16, 512], BF16) for i in range(3)]
            Rwob = [P.res("wob%d" % i) for i in range(3)]
            xb = [sbuf(st, "m2_xb%d" % i, [128, 512], F32) for i in range(3)]
            Rxb = [P.res("m2_xb%d" % i) for i in range(3)]
            hb = [sbuf(st, "m2_hb%d" % i, [128, 512], F32) for i in range(3)]
            Rhb = [P.res("m2_hb%d" % i) for i in range(3)]
            qcnt = [0]
            scnt = [0]
            ptc = [0]
            wcnt = [0]
            hcnt = [0]
            ntt = TG // 128

            for g in range(NG):
                t0 = g * TG
                P.dma("sp", lambda e, g=g: e.dma_start(out=cqn[:], in_=CQ[g].rearrange("p (a b) -> p a b", a=4)),
                      writes=[Rcqn])
                P.dma("sp", lambda e, g=g: e.dma_start(out=ymix[:, 0:8, :], in_=YP[g].rearrange("p (a b) -> p a b", a=8)),
                      writes=[Rypool])
                pro_some(8)
                nkt = ntt * (g + 1)
                def prep(j, t0=t0):
                    QT, RQT = QTs[2 * (j % 2):2 * (j % 2) + 2], RQTs[2 * (j % 2):2 * (j % 2) + 2]
                    QRT, RQRT = QRTs[j % 2], RQRTs[j % 2]
                    qk = qcnt[0] % 2
                    qcnt[0] += 1
                    pqr = psq[qk][:, 0:TG]
                    for fc in range(4):
                        P.op("pe", lambda e, pqr=pqr, fc=fc, j=j: e.matmul(
                            out=pqr, lhsT=wuq_sb[:, fc, 1024 + j * 128:1024 + (j + 1) * 128], rhs=cqn[:, fc, :],
                            start=(fc == 0), stop=(fc == 3)), reads=[R_w, Rcqn], writes=[Rpsq[qk]])
                    P.op("act", lambda e, pqr=pqr: e.activation(out=qrg[:], in_=pqr, func=AF.Copy, scale=cst_sb[:, 65:66]),
                         reads=[Rpsq[qk], R_const], writes=[Rqrg])
                    P.op("act", lambda e, pqr=pqr: e.activation(out=sqr[:], in_=pqr, func=AF.Square),
                         reads=[Rpsq[qk]], writes=[Rsqr])
                    for hh in range(2):
                        h = 2 * j + hh
                        qk2 = qcnt[0] % 2
                        qcnt[0] += 1
                        pq = psq[qk2][:, 0:TG]
                        for fc in range(4):
                            P.op("pe", lambda e, pq=pq, fc=fc, h=h: e.matmul(
                                out=pq, lhsT=wuq_sb[:, fc, h * 128:(h + 1) * 128], rhs=cqn[:, fc, :],
                                start=(fc == 0), stop=(fc == 3)), reads=[R_w, Rcqn], writes=[Rpsq[qk2]])
                        P.op("act", lambda e, pq=pq, hh=hh: e.activation(out=qtmp[hh][:], in_=pq, func=AF.Copy,
                                                                           scale=cst_sb[:, 64:65]),
                             reads=[Rpsq[qk2], R_const], writes=[Rqtmp[hh]])
                        P.op("act", lambda e, pq=pq, hh=hh: e.activation(out=sqq[hh][:], in_=pq, func=AF.Square),
                             reads=[Rpsq[qk2]], writes=[Rsqq[hh]])
                        P.op("pe", lambda e, pq=pq, hh=hh: e.matmul(out=pq, lhsT=ones_f, rhs=sqq[hh][:], start=True,
                                                                      stop=False),
                             reads=[Rsqq[hh], R_const, Rqtmp[hh]], writes=[Rpsq[qk2]])
                        P.op("pe", lambda e, pq=pq, hh=hh: e.matmul(out=pq, lhsT=(Llo_f if hh == 0 else Lhi_f),
                                                                      rhs=sqr[:], start=False, stop=True),
                             reads=[Rsqr, R_const], writes=[Rpsq[qk2]])
                        P.op("act", lambda e, pq=pq, hh=hh: e.activation(out=rq[hh][:], in_=pq, func=AF.Ln,
                                                                           scale=1.0 / 192, bias=cst_sb[:, 86:87]),
                             reads=[Rpsq[qk2], R_const], writes=[Rrq[hh]])
                        P.op("act", lambda e, hh=hh: e.activation(out=rq[hh][:], in_=rq[hh][:], func=AF.Exp, scale=-0.5),
                             reads=[Rrq[hh]], writes=[Rrq[hh]])
                        P.op("dve", lambda e, hh=hh: e.tensor_tensor(out=QT[hh][:], in0=qtmp[hh][:], in1=rq[hh][:],
                                                                       op=ALU.mult),
                             reads=[Rqtmp[hh], Rrq[hh]], writes=[RQT[hh]])
                    qk3 = qcnt[0] % 2
                    qcnt[0] += 1
                    pr = psq[qk3][:, 0:TG]
                    P.op("pe", lambda e, pr=pr: e.matmul(out=pr, lhsT=perm_f, rhs=qrg[:], start=True, stop=True),
                         reads=[Rqrg, R_const], writes=[Rpsq[qk3]])
                    P.op("dve", lambda e, t0=t0: e.tensor_tensor(out=t1[:], in0=qrg[:], in1=cosT[:, t0:t0 + TG],
                                                                  op=ALU.mult), reads=[Rqrg, R_rope], writes=[Rt1])
                    P.op("dve", lambda e, pr=pr, t0=t0: e.tensor_tensor(out=t2[:], in0=pr, in1=sinT[:, t0:t0 + TG],
                                                                         op=ALU.mult),
                         reads=[Rpsq[qk3], R_rope], writes=[Rt2])
                    P.op("dve", lambda e: e.tensor_tensor(out=t1[:], in0=t1[:], in1=t2[:], op=ALU.add),
                         reads=[Rt1, Rt2], writes=[Rt1])
                    P.op("dve", lambda e: e.tensor_tensor(out=QRT[0:64, :], in0=t1[0:64, :], in1=rq[0][0:64, :],
                                                          op=ALU.mult), reads=[Rt1, Rrq[0]], writes=[RQRT])
                    P.op("dve", lambda e: e.tensor_tensor(out=QRT[64:128, :], in0=t1[64:128, :], in1=rq[1][64:128, :],
                                                          op=ALU.mult), reads=[Rt1, Rrq[1]], writes=[RQRT], indep=True)

                prep(0)
                for j in range(4):
                    QT, RQT = QTs[2 * (j % 2):2 * (j % 2) + 2], RQTs[2 * (j % 2):2 * (j % 2) + 2]
                    QRT, RQRT = QRTs[j % 2], RQRTs[j % 2]
                    pending = []
                    if j + 1 < 4:
                        P.defer_begin()
                        prep(j + 1)
                        pending = P.defer_end()
                    kstep = -(-len(pending) // max(1, 2 * nkt - 1))
                    for hh in range(2):
                        h = 2 * j + hh
                        hp = 64 * hh
                        def emit_S(kt, h=h, hh=hh, hp=hp, QT=QT, QRT=QRT):
                            sk = scnt[0] % 2
                            scnt[0] += 1
                            ps_ = pss[sk][:, 0:TG]
                            gk = kt // ntt
                            P.op("pe", lambda e, ps_=ps_, h=h, kt=kt, hh=hh: e.matmul(
                                out=ps_, lhsT=KT[:, h, kt * 128:(kt + 1) * 128], rhs=QT[hh][:], start=True, stop=False),
                                reads=[R_K[gk], RQT[hh]], writes=[Rpss[sk]])
                            P.op("pe", lambda e, ps_=ps_, kt=kt, hp=hp: e.matmul(
                                out=ps_, lhsT=KRT[hp:hp + 64, kt * 128:(kt + 1) * 128], rhs=QRT[hp:hp + 64, :],
                                start=False, stop=True), reads=[R_K[gk], RQRT], writes=[Rpss[sk]])
                            return sk, ps_

                        nxt = emit_S(0)
                        for kt in range(nkt):
                            sk, ps_ = nxt
                            gk = kt // ntt
                            if kt + 1 < nkt:
                                nxt = emit_S(kt + 1)
                            pk_ = ptc[0] % 3
                            ptc[0] += 1
                            P.op("act", lambda e, ps_=ps_, pk_=pk_, kt=kt, h=h: e.activation(
                                out=pT[pk_][:], in_=ps_, func=AF.Exp, scale=rkcol[:, kt, h:h + 1]),
                                reads=[Rpss[sk], R_K[gk]], writes=[RpT[pk_]])
                            if kt >= ntt * g:
                                jm = kt - ntt * g
                                P.op("dve", lambda e, pk_=pk_, jm=jm: e.tensor_tensor(
                                    out=pT[pk_][:], in0=pT[pk_][:], in1=mask_b[:, jm, :], op=ALU.mult),
                                    reads=[RpT[pk_], R_w], writes=[RpT[pk_]])
                            P.op("pe", lambda e, pk_=pk_, kt=kt, h=h, nkt=nkt: e.matmul(
                                out=pso[:, 0:TG], lhsT=Vsb[:, kt, h * 128:(h + 1) * 128], rhs=pT[pk_][:],
                                start=(kt == 0), stop=(kt == nkt - 1)), reads=[R_V[gk], RpT[pk_]], writes=[Rpso])
                            P.op("pe", lambda e, pk_=pk_, kt=kt, nkt=nkt: e.matmul(
                                out=psd[:, 0:TG], lhsT=ones_bf[:], rhs=pT[pk_][:],
                                start=(kt == 0), stop=(kt == nkt - 1)), reads=[R_const, RpT[pk_]], writes=[Rpsd])
                            for _ in range(kstep):
                                if pending:
                                    pending.pop(0)()
                        P.op("dve", lambda e: e.reciprocal(out=rec[:], in_=psd[:, 0:TG]), reads=[Rpsd], writes=[Rrec])
                        P.op("dve", lambda e, h=h: e.tensor_tensor(out=ymix[:, 8 + h, :], in0=pso[:, 0:TG], in1=rec[:],
                                                                    op=ALU.mult),
                             reads=[Rpso, Rrec], writes=[Rymla])
                    while pending:
                        pending.pop(0)()
                for nb in range(4):
                    wk = wcnt[0] % 3
                    wcnt[0] += 1
                    P.dma("sp", lambda e, wk=wk, nb=nb: e.dma_start(
                        out=wob[wk][:], in_=Woutb[nb].rearrange("p (a b) -> p a b", a=16)),
                        reads=[RWout[nb]], writes=[Rwob[wk]])
                    for tt in range(ntt):
                        hk = hcnt[0] % 3
                        hk2 = hcnt[0] % 2
                        hcnt[0] += 1
                        row0 = t0 + tt * 128
                        P.dma("sp", lambda e, hk=hk, row0=row0, nb=nb: e.dma_start(
                            out=xb[hk][:], in_=x[row0:row0 + 128, nb * 512:(nb + 1) * 512]), writes=[Rxb[hk]])
                        for fc in range(16):
                            P.op("pe", lambda e, hk2=hk2, fc=fc, tt=tt, wk=wk: e.matmul(
                                out=psh[hk2][:], lhsT=ymix[:, fc, tt * 128:(tt + 1) * 128], rhs=wob[wk][:, fc, :],
                                start=(fc == 0), stop=(fc == 15)),
                                reads=[Rypool, Rymla, Rwob[wk]], writes=[Rpsh[hk2]])
                        P.op("dve", lambda e, hk=hk, hk2=hk2: e.tensor_tensor(out=hb[hk][:], in0=psh[hk2][:],
                                                                                in1=xb[hk][:], op=ALU.add),
                             reads=[Rpsh[hk2], Rxb[hk]], writes=[Rhb[hk]])
                        P.dma("sp", lambda e, hk=hk, row0=row0, nb=nb: e.dma_start(
                            out=H1[row0:row0 + 128, nb * 512:(nb + 1) * 512], in_=hb[hk][:]), reads=[Rhb[hk]])
            P.emit()
        stKV.close()
        if stop_after == "M2":
            return nc, None, st0

        ntt = TG // 128
        IRv = IRs.rearrange("k p t -> p k t")
        with ExitStack() as st:
            subk_sb = sbuf(st, "subk_sb", [128, 16, 128], F32)
            iota16 = sbuf(st, "iota16", [128, 16], BF16)
            R_w = P.res("f1w")
            P.dma("sp", lambda e: e.dma_start(out=subk_sb[:], in_=subk.rearrange("p (a b) -> p a b", a=16)), writes=[R_w])
            P.op("dve", lambda e: e.tensor_copy(out=iota16[:], in_=iota_f[:, 0:16]), reads=[R_const], writes=[R_w])
            ht = [sbuf(st, "f1_ht%d" % i, [128, D], F32) for i in range(2)]
            Rht = [P.res("f1_ht%d" % i) for i in range(2)]
            rbs = [rms_bufs(st, "f1_%d" % i) for i in range(2)]
            xn2 = sbuf(st, "xn2", [128, 16, TG], BF16)
            Rxn2 = P.res("xn2")
            wbuf = [sbuf(st, "f1_wb%d" % i, [128, 16, 128], BF16) for i in range(4)]
            Rwb = [P.res("f1_wb%d" % i) for i in range(4)]
            psq = [psum(st, "f1_psq%d" % i, [128, 512], F32) for i in range(2)]
            Rpsq = [P.res("f1_psq%d" % i) for i in range(2)]
            pssc = [psum(st, "f1_pssc%d" % i, [128, 512], F32) for i in range(2)]
            Rpssc = [P.res("f1_pssc%d" % i) for i in range(2)]
            qpTs = [sbuf(st, "qpT%d" % i, [128, 16, TG], F32) for i in range(2)]
            RqpTs = [P.res("qpT%d" % i) for i in range(2)]
            sc = sbuf(st, "sc", [128, 16, 128], F32)
            sc2 = sbuf(st, "sc2", [128, 16, 128], F32)
            Rsc = [P.res("sc%d" % i) for i in range(16)]
            Rsc2 = [P.res("sc2_%d" % i) for i in range(16)]
            v16 = sbuf(st, "v16", [128, 16, 16], F32)
            i16 = sbuf(st, "i16", [128, 16, 16], U32)
            i16f = sbuf(st, "i16f", [128, 16, 16], BF16)
            Rv16 = [P.res("v16_%d" % i) for i in range(16)]
            Ri16 = [P.res("i16_%d" % i) for i in range(16)]
            Ri16f = P.res("i16f")
            cand = sbuf(st, "cand", [128, 8, 256], F32)
            cand2 = sbuf(st, "cand2", [128, 8, 256], F32)
            Rcand = [P.res("cand%d" % i) for i in range(8)]
            Rcand2 = [P.res("cand2_%d" % i) for i in range(8)]
            vs = sbuf(st, "vs", [128, 8, 16], F32)
            ci = sbuf(st, "ci", [128, 8, 16], U32)
            Rvs = [P.res("vs%d" % i) for i in range(8)]
            Rci = [P.res("ci%d" % i) for i in range(8)]
            abi = sbuf(st, "abi", [128, 2, 128], U32)
            abf = sbuf(st, "abf", [128, 2, 128], BF16)
            Rabi, Rabf = P.res("abi"), P.res("abf")
            eqb = [sbuf(st, "eqb%d" % i, [128, 8, 16, 16], BF16) for i in range(2)]
            Reqb = [P.res("eqb%d" % i) for i in range(2)]
            tris = [sbuf(st, "tri%d" % i, [128, 3, 128], F32) for i in range(4)]
            Rtris = [P.res("tri%d" % i) for i in range(4)]
            gsm = sbuf(st, "gsm", [128, 2, 8], F32)
            Rgsm = P.res("gsm")
            pstr = pssc[1]
            Rpstr = Rpssc[1]
            trT = sbuf(st, "trT", [128, 3, 128], F32)
            RtrT = P.res("trT")
            wcnt = [0]
            qcnt = [0]
            tcnt = [0]

            def top16(src, Rsrc, src2, Rsrc2, vout, Rvout, iout, Riout, n):
                for k in range(n):
                    P.op("dve", lambda e, k=k: e.max(out=vout(k)[:, 0:8], in_=src(k)), reads=[Rsrc[k]], writes=[Rvout[k]])
                for k in range(n):
                    P.op("dve", lambda e, k=k: e.max_index(out=iout(k)[:, 0:8], in_max=vout(k)[:, 0:8], in_values=src(k)),
                         reads=[Rsrc[k], Rvout[k]], writes=[Riout[k]])
                for k in range(n):
                    P.op("dve", lambda e, k=k: e.match_replace(out=src2(k), in_to_replace=vout(k)[:, 0:8],
                                                               in_values=src(k), imm_value=-1e30),
                         reads=[Rsrc[k], Rvout[k]], writes=[Rsrc2[k]])
                for k in range(n):
                    P.op("dve", lambda e, k=k: e.max(out=vout(k)[:, 8:16], in_=src2(k)), reads=[Rsrc2[k]],
                         writes=[Rvout[k]])
                for k in range(n):
                    P.op("dve", lambda e, k=k: e.max_index(out=iout(k)[:, 8:16], in_max=vout(k)[:, 8:16],
                                                           in_values=src2(k)),
                         reads=[Rsrc2[k], Rvout[k]], writes=[Riout[k]])

            def f1_front(g):
                t0 = g * TG
                qpT, RqpT = qpTs[g % 2], RqpTs[g % 2]
                items = []
                for tt in range(ntt):
                    k = tt % 2
                    row0 = t0 + tt * 128
                    P.dma("sp", lambda e, k=k, row0=row0: e.dma_start(out=ht[k][:], in_=H1[row0:row0 + 128, :]),
                          writes=[Rht[k]])
                    items.append((ht[k][:], Rht[k], (lambda dc, tt=tt: xn2[:, dc, tt * 128:(tt + 1) * 128]), Rxn2, rbs[k]))
                rmsnorm_multi(items, 16, all_act=True)
                P.dma("sp", lambda e, g=g: e.dma_start(out=XN2[g].rearrange("p (a b) -> p a b", a=16), in_=xn2[:]),
                      reads=[Rxn2])
                pro_some(40)
                for c in range(16):
                    wk = wcnt[0] % 4
                    wcnt[0] += 1
                    P.dma("sp", lambda e, wk=wk, c=c: e.dma_start(
                        out=wbuf[wk][:], in_=Wpqb[c].rearrange("p (a b) -> p a b", a=16)),
                        reads=[RWpq[c]], writes=[Rwb[wk]])
                    qk = qcnt[0] % 2
                    qcnt[0] += 1
                    pq = psq[qk][:, 0:TG]
                    for dc in range(16):
                        P.op("pe", lambda e, wk=wk, dc=dc, pq=pq: e.matmul(
                            out=pq, lhsT=wbuf[wk][:, dc, :], rhs=xn2[:, dc, :], start=(dc == 0), stop=(dc == 15)),
                            reads=[Rwb[wk], Rxn2], writes=[Rpsq[qk]])
                    P.op("act", lambda e, pq=pq, c=c: e.activation(out=qpT[:, c, :], in_=pq, func=AF.Copy),
                         reads=[Rpsq[qk]], writes=[RqpT], indep=True)

            def f1_main(g, tt):
                t0 = g * TG
                qpT, RqpT = qpTs[g % 2], RqpTs[g % 2]
                tri, Rtri = tris[2 * (g % 2) + tt], Rtris[2 * (g % 2) + tt]
                if True:
                    for c in range(16):
                        bq = (c // 4) % 2
                        P.op("pe", lambda e, c=c, tt=tt, bq=bq: e.matmul(
                            out=pssc[bq][:, (c % 4) * 128:(c % 4 + 1) * 128],
                            lhsT=qpT[:, c, tt * 128:(tt + 1) * 128], rhs=subk_sb[:, c, :], start=True, stop=True),
                            reads=[RqpT, R_w], writes=[Rpssc[bq]])
                        if c % 4 == 3:
                            b4 = c // 4
                            P.op("dve", lambda e, b4=b4, bq=bq: e.tensor_copy(
                                out=sc[:, 4 * b4:4 * b4 + 4, :], in_=pssc[bq][:].rearrange("p (a b) -> p a b", a=4)),
                                reads=[Rpssc[bq]], writes=Rsc[4 * b4:4 * b4 + 4])
                    top16(lambda k: sc[:, k, :], Rsc, lambda k: sc2[:, k, :], Rsc2,
                          lambda k: v16[:, k, :], Rv16, lambda k: i16[:, k, :], Ri16, 16)
                    P.op("dve", lambda e: e.tensor_copy(out=i16f[:], in_=i16[:]), reads=Ri16, writes=[Ri16f])
                    v16v = v16[:].rearrange("p (h s) a -> p h s a", s=2)
                    i16v = i16f[:].rearrange("p (h s) a -> p h s a", s=2)
                    P.op("dve", lambda e, v16v=v16v: e.tensor_tensor(
                        out=cand[:].rearrange("p h (a b) -> p h a b", a=16),
                        in0=v16v[:, :, 0, :].unsqueeze(3).to_broadcast([128, 8, 16, 16]),
                        in1=v16v[:, :, 1, :].unsqueeze(2).to_broadcast([128, 8, 16, 16]), op=ALU.add),
                        reads=Rv16, writes=Rcand)
                    top16(lambda k: cand[:, k, :], Rcand, lambda k: cand2[:, k, :], Rcand2,
                          lambda k: vs[:, k, :], Rvs, lambda k: ci[:, k, :], Rci, 8)
                    civ = ci[:].rearrange("p h r -> p (h r)")
                    P.op("dve", lambda e, civ=civ: e.tensor_single_scalar(out=abi[:, 0, :], in_=civ, scalar=4,
                                                                          op=ALU.logical_shift_right),
                         reads=Rci, writes=[Rabi])
                    P.op("dve", lambda e, civ=civ: e.tensor_single_scalar(out=abi[:, 1, :], in_=civ, scalar=15,
                                                                          op=ALU.bitwise_and),
                         reads=Rci, writes=[Rabi], indep=True)
                    P.op("dve", lambda e: e.tensor_copy(out=abf[:], in_=abi[:]), reads=[Rabi], writes=[Rabf])
                    for s_ in range(2):
                        eb = eqb[s_]
                        P.op("dve", lambda e, eb=eb, s_=s_: e.tensor_tensor(
                            out=eb[:],
                            in0=iota16[:].unsqueeze(1).unsqueeze(1).to_broadcast([128, 8, 16, 16]),
                            in1=abf[:, s_, :].rearrange("p (h r) -> p h r", h=8).unsqueeze(3).to_broadcast([128, 8, 16, 16]),
                            op=ALU.is_equal), reads=[Rabf, R_w], writes=[Reqb[s_]])
                        P.op("dve", lambda e, eb=eb, s_=s_, i16v=i16v: e.tensor_tensor(
                            out=eb[:], in0=eb[:],
                            in1=i16v[:, :, s_, :].unsqueeze(2).to_broadcast([128, 8, 16, 16]), op=ALU.mult),
                            reads=[Reqb[s_], Ri16f], writes=[Reqb[s_]])
                        P.op("dve", lambda e, eb=eb, s_=s_: e.tensor_reduce(
                            out=tri[:, s_, :].rearrange("p (h r) -> p h r", h=8), in_=eb[:], axis=AX.X, op=ALU.add),
                            reads=[Reqb[s_]], writes=[Rtri])
                    gv = tri[:, 2, :].rearrange("p (h r) -> p h r", h=8)
                    P.op("dve", lambda e, gv=gv: e.tensor_tensor(
                        out=gv, in0=vs[:], in1=vs[:, :, 0:1].to_broadcast([128, 8, 16]), op=ALU.subtract),
                        reads=Rvs, writes=[Rtri])

            def f1_tail(g, tt):
                t0 = g * TG
                tri, Rtri = tris[2 * (g % 2) + tt], Rtris[2 * (g % 2) + tt]
                if True:
                    gv = tri[:, 2, :].rearrange("p (h r) -> p h r", h=8)
                    P.op("act", lambda e, gv=gv: e.activation(out=gv, in_=gv, func=AF.Exp), reads=[Rtri], writes=[Rtri])
                    P.op("dve", lambda e, gv=gv: e.tensor_reduce(out=gsm[:, 0, :], in_=gv, axis=AX.X, op=ALU.add),
                         reads=[Rtri], writes=[Rgsm])
                    P.op("dve", lambda e: e.reciprocal(out=gsm[:, 1, :], in_=gsm[:, 0, :]), reads=[Rgsm], writes=[Rgsm])
                    P.op("dve", lambda e, gv=gv: e.tensor_tensor(
                        out=gv, in0=gv, in1=gsm[:, 1, :].unsqueeze(2).to_broadcast([128, 8, 16]), op=ALU.mult),
                        reads=[Rtri, Rgsm], writes=[Rtri])
                    for q_ in range(3):
                        P.op("pe", lambda e, q_=q_: e.transpose(out=pstr[:, q_ * 128:(q_ + 1) * 128], in_=tri[:, q_, :],
                                                                identity=ident_f), reads=[Rtri, R_const], writes=[Rpstr])
                    P.op("act", lambda e: e.activation(out=trT[:], in_=pstr[:, 0:384].rearrange("p (a b) -> p a b", a=3),
                                                       func=AF.Copy), reads=[Rpstr], writes=[RtrT])
                    row0 = t0 + tt * 128
                    P.dma("sp", lambda e, row0=row0: e.dma_start(out=IRv[:, :, row0:row0 + 128], in_=trT[:]),
                          reads=[RtrT])

            f1_front(0)
            if NG > 1:
                f1_front(1)
            for g in range(NG):
                for tt in range(ntt):
                    f1_main(g, tt)
                if g + 2 < NG:
                    f1_front(g + 2)
                if g >= 1:
                    for tt in range(ntt):
                        f1_tail(g - 1, tt)
            for tt in range(ntt):
                f1_tail(NG - 1, tt)
            P.emit()
        if stop_after == "F1":
            return nc, None, st0

        with ExitStack() as st:
            GT = sbuf(st, "GT", [128, TG, 128], BF16)
            RGT = [P.res("GT%d" % i) for i in range(128)]
            xn2 = sbuf(st, "f2_xn2", [128, 16, TG], BF16)
            Rxn2 = P.res("f2_xn2")
            trg = sbuf(st, "trg", [128, 3, TG], F32)
            Rtrg = P.res("trg")
            NSUB = 32
            Pb = [sbuf(st, "Pb%d" % i, [128, NSUB, 128], BF16) for i in range(2)]
            Qb = [sbuf(st, "Qb%d" % i, [128, NSUB, 128], BF16) for i in range(2)]
            RPb = [P.res("Pb%d" % i) for i in range(2)]
            RQb = [P.res("Qb%d" % i) for i in range(2)]
            psg = [psum(st, "psg%d" % i, [128, 512], F32) for i in range(2)]
            Rpsg = [P.res("psg%d" % i) for i in range(2)]
            pss = [psum(st, "f2_pss%d" % i, [128, 512], F32) for i in range(2)]
            Rpss = [P.res("f2_pss%d" % i) for i in range(2)]
            pso = [psum(st, "f2_pso%d" % i, [128, 512], F32) for i in range(4)]
            Rpso = [P.res("f2_pso%d" % i) for i in range(4)]
            ub = [sbuf(st, "ub%d" % i, [128, 16, 128], BF16) for i in range(10)]
            Rub = [P.res("ub%d" % i) for i in range(10)]
            vb = [sbuf(st, "vb%d" % i, [128, 2, 1024], BF16) for i in range(6)]
            Rvb = [P.res("vb%d" % i) for i in range(6)]
            gl = [sbuf(st, "gl%d" % i, [128, TG], F32) for i in range(3)]
            Rgl = [P.res("gl%d" % i) for i in range(3)]
            hb = [sbuf(st, "f2_hb%d" % i, [128, 512], F32) for i in range(3)]
            Rhb = [P.res("f2_hb%d" % i) for i in range(3)]
            ob = [sbuf(st, "f2_ob%d" % i, [128, 512], F32) for i in range(3)]
            Rob = [P.res("f2_ob%d" % i) for i in range(3)]
            gcnt = [0]
            scnt = [0]
            ucnt = [0]
            vcnt = [0]
            glc = [0]
            ocnt = [0]
            hcnt = [0]
            sbc = [0]
            for g in range(NG):
                t0 = g * TG
                P.dma("sp", lambda e, g=g: e.dma_start(out=xn2[:], in_=XN2[g].rearrange("p (a b) -> p a b", a=16)),
                      writes=[Rxn2])
                P.dma("sp", lambda e, t0=t0: e.dma_start(out=trg[:], in_=IRv[:, :, t0:t0 + TG]), writes=[Rtrg])
                for sub in range(TG // NSUB):
                    bk = sbc[0] % 2
                    sbc[0] += 1
                    for tl in range(NSUB):
                        t = sub * NSUB + tl
                        P.op("dve", lambda e, bk=bk, tl=tl, t=t: e.tensor_scalar(
                            out=Pb[bk][:, tl, :], in0=iota_bf[:], scalar1=trg[:, 0, t:t + 1], scalar2=trg[:, 2, t:t + 1],
                            op0=ALU.is_equal, op1=ALU.mult), reads=[Rtrg, R_const], writes=[RPb[bk]], indep=True)
                        P.op("dve", lambda e, bk=bk, tl=tl, t=t: e.tensor_scalar(
                            out=Qb[bk][:, tl, :], in0=iota_bf[:], scalar1=trg[:, 1, t:t + 1], scalar2=None,
                            op0=ALU.is_equal), reads=[Rtrg, R_const], writes=[RQb[bk]], indep=True)
                    for q4 in range(NSUB // 4):
                        gk = gcnt[0] % 2
                        gcnt[0] += 1
                        tok0 = sub * NSUB + q4 * 4
                        for u in range(4):
                            tl = q4 * 4 + u
                            P.op("pe", lambda e, gk=gk, u=u, bk=bk, tl=tl: e.matmul(
                                out=psg[gk][:, u * 128:(u + 1) * 128], lhsT=Qb[bk][:, tl, :], rhs=Pb[bk][:, tl, :],
                                start=True, stop=True), reads=[RPb[bk], RQb[bk]], writes=[Rpsg[gk]])
                        P.op("act", lambda e, gk=gk, tok0=tok0: e.activation(
                            out=GT[:, tok0:tok0 + 4, :],
                            in_=psg[gk][:].rearrange("p (t i) -> p t i", t=4), func=AF.Copy),
                            reads=[Rpsg[gk]], writes=RGT, indep=True)
                for i in range(128):
                    uk = ucnt[0] % 10
                    ucnt[0] += 1
                    P.dma("sp", lambda e, uk=uk, i=i: e.dma_start(out=ub[uk][:], in_=UTb[i].rearrange("p (a b) -> p a b", a=16)),
                          writes=[Rub[uk]])
                    sk = scnt[0] % 2
                    scnt[0] += 1
                    ps_ = pss[sk][:, 0:TG]
                    for dc in range(16):
                        P.op("pe", lambda e, ps_=ps_, uk=uk, dc=dc: e.matmul(
                            out=ps_, lhsT=ub[uk][:, dc, :], rhs=xn2[:, dc, :], start=(dc == 0), stop=(dc == 15)),
                            reads=[Rub[uk], Rxn2], writes=[Rpss[sk]])
                    lk = glc[0] % 3
                    glc[0] += 1
                    P.op("act", lambda e, ps_=ps_, lk=lk: e.activation(out=gl[lk][:], in_=ps_, func=AF.Gelu_apprx_tanh),
                         reads=[Rpss[sk]], writes=[Rgl[lk]])
                    P.op("dve", lambda e, lk=lk, i=i: e.tensor_tensor(out=GT[:, :, i], in0=gl[lk][:], in1=GT[:, :, i],
                                                                        op=ALU.mult),
                         reads=[Rgl[lk], RGT[i]], writes=[RGT[i]])
                for nbp in range(2):
                    for i2 in range(64):
                        vk = vcnt[0] % 6
                        vcnt[0] += 1
                        r0 = nbp * 128 + 2 * i2
                        P.dma("sp", lambda e, vk=vk, r0=r0: e.dma_start(
                            out=vb[vk][:], in_=EVb[r0:r0 + 2].rearrange("i e n -> e i n")), writes=[Rvb[vk]])
                        for ii in range(2):
                            i = 2 * i2 + ii
                            for nbl in range(2):
                                for tt in range(ntt):
                                    pk = 2 * nbl + tt
                                    P.op("pe", lambda e, pk=pk, tt=tt, i=i, ii=ii, vk=vk, nbl=nbl: e.matmul(
                                        out=pso[pk][:], lhsT=GT[:, tt * 128:(tt + 1) * 128, i],
                                        rhs=vb[vk][:, ii, nbl * 512:(nbl + 1) * 512],
                                        start=(i == 0), stop=(i == 127)), reads=[RGT[i], Rvb[vk]], writes=[Rpso[pk]])
                    for nbl in range(2):
                        nb = 2 * nbp + nbl
                        for tt in range(ntt):
                            pk = 2 * nbl + tt
                            hk = hcnt[0] % 3
                            hcnt[0] += 1
                            row0 = t0 + tt * 128
                            P.dma("sp", lambda e, hk=hk, row0=row0, nb=nb: e.dma_start(
                                out=hb[hk][:], in_=H1[row0:row0 + 128, nb * 512:(nb + 1) * 512]), writes=[Rhb[hk]])
                            P.op("dve", lambda e, hk=hk, pk=pk: e.tensor_tensor(
                                out=ob[hk][:], in0=pso[pk][:], in1=hb[hk][:], op=ALU.add),
                                reads=[Rpso[pk], Rhb[hk]], writes=[Rob[hk]])
                            P.dma("sp", lambda e, hk=hk, row0=row0, nb=nb: e.dma_start(
                                out=H2[row0:row0 + 128, nb * 512:(nb + 1) * 512], in_=ob[hk][:]), reads=[Rob[hk]])
            P.emit()
        if stop_after == "F2":
            return nc, None, st0

        with ExitStack() as st:
            NTILE = T // 128
            wpp_sb = sbuf(st, "wpp_sb", [128, 2, 2048], BF16)
            R_w = P.res("gw")
            P.dma("pool", lambda e: e.dma_start(out=wpp_sb[:], in_=Wpp.rearrange("p (a b) -> p a b", a=2),
                                                max_dma_last_dim=4096), writes=[R_w])
            ht = [sbuf(st, "g_ht%d" % i, [128, D], F32) for i in range(2)]
            Rht = [P.res("g_ht%d" % i) for i in range(2)]
            rbs = [rms_bufs(st, "g%d" % i) for i in range(2)]
            xn3 = sbuf(st, "xn3", [128, 16, T], BF16)
            Rxn3 = [P.res("xn3_%d" % i) for i in range(NTILE)]
            pt_f = sbuf(st, "pt_f", [128, 256], F32)
            pt_b = sbuf(st, "pt_b", [128, 256], BF16)
            Rptf, Rptb = P.res("pt_f"), P.res("pt_b")
            pTb = sbuf(st, "pTb", [128, 2, T], BF16)
            RpTb = [P.res("pTb%d" % i) for i in range(NTILE)]
            pstp = psum(st, "g_pstp", [128, 1024], BF16)
            Rpstp = P.res("g_pstp")
            wgb = [sbuf(st, "wgb%d" % i, [128, 16, 512], BF16) for i in range(3)]
            Rwgb = [P.res("wgb%d" % i) for i in range(3)]
            psgt = [psum(st, "g_psg%d" % i, [128, 512], F32) for i in range(2)]
            Rpsgt = [P.res("g_psg%d" % i) for i in range(2)]
            pspp = psum(st, "g_psp", [128, 512], F32)
            Rpspp = P.res("g_psp")
            sg = [sbuf(st, "sg%d" % i, [128, 512], F32) for i in range(2)]
            Rsg = [P.res("sg%d" % i) for i in range(2)]
            hb = [sbuf(st, "g_hb%d" % i, [128, 512], F32) for i in range(3)]
            Rhb = [P.res("g_hb%d" % i) for i in range(3)]
            ob = [sbuf(st, "g_ob%d" % i, [128, 512], F32) for i in range(3)]
            Rob = [P.res("g_ob%d" % i) for i in range(3)]
            kcnt = [0]
            ocnt = [0]

            def g_front2(t_first):
                items = []
                for t in (t_first, t_first + 1):
                    k = t % 2
                    row0 = t * 128
                    P.dma("sp", lambda e, k=k, row0=row0: e.dma_start(out=ht[k][:], in_=H2[row0:row0 + 128, :]),
                          writes=[Rht[k]])
                    items.append((ht[k][:], Rht[k], (lambda dc, row0=row0: xn3[:, dc, row0:row0 + 128]), Rxn3[t], rbs[k]))
                rmsnorm_multi(items, 32)
                for t in (t_first, t_first + 1):
                    row0 = t * 128
                    P.dma("sp", lambda e, row0=row0: e.dma_start(out=pt_f[:], in_=pin[row0:row0 + 128, :]), writes=[Rptf])
                    P.op("act", lambda e: e.activation(out=pt_b[:], in_=pt_f[:], func=AF.Copy), reads=[Rptf], writes=[Rptb])
                    for fc in range(2):
                        P.op("pe", lambda e, fc=fc: e.transpose(out=pstp[:, fc * 128:(fc + 1) * 128],
                                                                in_=pt_b[:, fc * 128:(fc + 1) * 128], identity=ident_bf[:]),
                             reads=[Rptb, R_const], writes=[Rpstp])
                    P.op("dve", lambda e, row0=row0: e.tensor_copy(
                        out=pTb[:, :, row0:row0 + 128], in_=pstp[:, 0:256].rearrange("p (a b) -> p a b", a=2)),
                        reads=[Rpstp], writes=[RpTb[t]])

            def g_back(t, nb, wk):
                row0 = t * 128
                kk = kcnt[0] % 2
                kcnt[0] += 1
                okk = ocnt[0] % 3
                ocnt[0] += 1
                P.dma("sp", lambda e: e.dma_start(out=hb[okk][:], in_=H2[row0:row0 + 128, nb * 512:(nb + 1) * 512]),
                      writes=[Rhb[okk]])
                for fc in range(16):
                    P.op("pe", lambda e, fc=fc: e.matmul(
                        out=psgt[kk][:], lhsT=xn3[:, fc, row0:row0 + 128], rhs=wgb[wk][:, fc, :],
                        start=(fc == 0), stop=(fc == 15)), reads=[Rxn3[t], Rwgb[wk]], writes=[Rpsgt[kk]])
                for fc in range(2):
                    P.op("pe", lambda e, fc=fc: e.matmul(
                        out=pspp[:], lhsT=pTb[:, fc, row0:row0 + 128], rhs=wpp_sb[:, fc, nb * 512:(nb + 1) * 512],
                        start=(fc == 0), stop=(fc == 1)), reads=[RpTb[t], R_w], writes=[Rpspp])
                P.op("act", lambda e: e.activation(out=sg[kk][:], in_=psgt[kk][:], func=AF.Sigmoid),
                     reads=[Rpsgt[kk]], writes=[Rsg[kk]])
                P.op("dve", lambda e: e.tensor_tensor(out=sg[kk][:], in0=sg[kk][:], in1=pspp[:], op=ALU.mult),
                     reads=[Rsg[kk], Rpspp], writes=[Rsg[kk]])
                P.op("dve", lambda e: e.tensor_tensor(out=ob[okk][:], in0=sg[kk][:], in1=hb[okk][:], op=ALU.add),
                     reads=[Rsg[kk], Rhb[okk]], writes=[Rob[okk]])
                P.dma("sp", lambda e: e.dma_start(out=out[row0:row0 + 128, nb * 512:(nb + 1) * 512], in_=ob[okk][:]),
                      reads=[Rob[okk]])

            def load_w(nb):
                wk = nb % 3
                P.dma("sp", lambda e: e.dma_start(out=wgb[wk][:], in_=Wgb[nb].rearrange("p (a b) -> p a b", a=16)),
                      reads=[RWg[nb]], writes=[Rwgb[wk]])

            load_w(0)
            g_front2(0)
            load_w(1)
            load_w(2)
            for t in range(0, NTILE, 2):
                if t + 2 < NTILE:
                    g_front2(t + 2)
                g_back(t, 0, 0)
                g_back(t + 1, 0, 0)
            for nb in range(1, 4):
                if nb + 2 < 4:
                    load_w(nb + 2)
                for t in range(NTILE):
                    g_back(t, nb, nb % 3)
            P.emit()
        return nc, None, st0


def _host_layout(inp, b):
    f = np.float32
    d = {}
    d["x"] = np.ascontiguousarray(inp["x"][b], f)
    d["p"] = np.ascontiguousarray(inp["p"][0, b], f)
    d["pos"] = np.ascontiguousarray(inp["positions"][b].reshape(1, T).astype(np.int32))
    return d


_SHARED = {}


def _shared_layout(inp):
    f = np.float32
    s = {}
    cst = np.zeros((128, NCST), f)

    def colmajor(v, n):
        return np.asarray(v, f).reshape(n, 128).T

    cst[:, 0:16] = colmajor(inp["mix_norm_gain"][0], 16)
    cst[:, 16:32] = colmajor(inp["ffn_norm_gain"][0], 16)
    cst[:, 32:48] = colmajor(inp["ple_norm_gain"][0], 16)
    cst[:, 48:52] = colmajor(inp["q_lat_gain"][0], 4)
    cst[:, 52:56] = colmajor(inp["kv_lat_gain"][0], 4)
    cst[:, 56:64] = colmajor(inp["pool_scale"][0], 8)
    qg = np.asarray(inp["q_norm_gain"][0], f)
    kg = np.asarray(inp["k_norm_gain"][0], f)
    cst[:, 64] = qg[0:128]
    cst[:, 65] = np.tile(qg[128:192], 2)
    cst[:, 66] = kg[0:128]
    cst[:, 67] = np.tile(kg[128:192], 2)
    inv_freq = (np.float32(10000.0) ** (-np.arange(0, 64, 2, dtype=np.float32) / np.float32(64))).astype(f)
    cst[:, 68] = np.tile(inv_freq, 4)
    cst[:, 69] = np.tile(np.concatenate([-np.ones(32, f), np.ones(32, f)]), 2)
    cst[:, 70:86] = (1.0 / np.arange(1, 17, dtype=f))[None, :]
    cst[:, 86] = EPS
    s["cst"] = cst
    mats = np.zeros((128, 6, 128), f)
    mats[:, 0, :] = np.eye(128, dtype=f)
    mats[:, 1, :] = 1.0
    mats[0:64, 2, :] = 1.0
    mats[64:128, 3, :] = 1.0
    for m in range(128):
        partner = m + 32 if (m % 64) < 32 else m - 32
        mats[partner, 4, m] = 1.0
    mats[:, 5, :] = np.arange(128, dtype=f)[None, :]
    s["mats"] = mats.reshape(128, 768)
    ntt = TG // 128
    mk = np.zeros((128, ntt, TG), f)
    kk = np.arange(128)[:, None]
    qq = np.arange(TG)[None, :]
    for j in range(ntt):
        mk[:, j, :] = ((qq // 64) >= ((128 * j + kk) // 64)).astype(f)
    s["maskc"] = mk.reshape(128, ntt * TG)
    w_in = np.asarray(inp["w_in"][0], f)
    w_ext = np.concatenate([w_in, w_in[:, 2048:2112]], axis=1)
    s["Win"] = np.ascontiguousarray(w_ext.reshape(16, 128, 17, 128).transpose(2, 1, 0, 3)).reshape(17, 128, 2048)
    wp = np.asarray(inp["w_pool"][0], f)
    s["Wpool"] = np.ascontiguousarray(wp.reshape(4, 2, 128, 256).transpose(2, 0, 1, 3)).reshape(128, 2048)
    wuq = np.asarray(inp["w_uq"][0], f).reshape(512, 8, 192)
    wuq_r = np.concatenate([wuq[:, :, 0:128].reshape(512, 1024), wuq[:, :, 128:192].reshape(512, 512)], axis=1)
    s["Wuq"] = np.ascontiguousarray(wuq_r.reshape(4, 128, 1536).transpose(1, 0, 2)).reshape(128, 4 * 1536)
    wukv = np.asarray(inp["w_ukv"][0], f).reshape(512, 8, 256)
    wuk = wukv[:, :, 0:128].reshape(512, 1024)
    wv = wukv[:, :, 128:256].reshape(512, 1024)
    s["Wuk"] = np.ascontiguousarray(wuk.reshape(4, 128, 1024).transpose(1, 0, 2)).reshape(128, 4096)
    s["Wv"] = np.ascontiguousarray(wv.reshape(4, 128, 1024).transpose(1, 0, 2)).reshape(128, 4096)
    wo = np.asarray(inp["w_out"][0], f)
    s["Wout"] = np.ascontiguousarray(wo.reshape(16, 128, 4, 512).transpose(2, 1, 0, 3)).reshape(4, 128, 8192)
    wpq = np.asarray(inp["w_pq"][0], f)
    s["Wpq"] = np.ascontiguousarray(wpq.reshape(16, 128, 16, 128).transpose(2, 1, 0, 3)).reshape(16, 128, 2048)
    sk = np.stack([np.asarray(inp["sub_k1"][0], f), np.asarray(inp["sub_k2"][0], f)], axis=1)
    s["subk"] = np.ascontiguousarray(sk.transpose(3, 0, 1, 2)).reshape(128, 2048)
    eu = np.asarray(inp["expert_u"][0], f)
    s["UT"] = np.ascontiguousarray(eu.reshape(128, 128, 16, 128).transpose(0, 3, 2, 1)).reshape(128, 128, 2048)
    ev = np.asarray(inp["expert_v"][0], f)
    evl = np.ascontiguousarray(ev.reshape(128, 128, 2, 1024).transpose(2, 0, 1, 3))
    s["EV"] = evl.reshape(256, 128, 1024)
    wg = np.asarray(inp["w_ple_gate"][0], f)
    s["Wg"] = np.ascontiguousarray(wg.reshape(16, 128, 4, 512).transpose(2, 1, 0, 3)).reshape(4, 128, 8192)
    wpp = np.asarray(inp["w_ple_proj"][0], f)
    s["Wpp"] = np.ascontiguousarray(wpp.reshape(2, 128, 2048).transpose(1, 0, 2)).reshape(128, 4096)
    return s


def kernel(**inputs):
    shared = _shared_layout(inputs)
    nc, _, _ = build()
    in_maps = []
    for b in range(8):
        m = dict(shared)
        m.update(_host_layout(inputs, b))
        in_maps.append(m)
    res = run_bass_kernel_spmd(nc, in_maps, core_ids=list(range(8)))
    return np.stack([r["out"] for r in res.results], axis=0).astype(np.float32)
```

```python
import math
from contextlib import ExitStack

import numpy as np
import concourse.bass as bass
import concourse.mybir as mybir
from concourse.bass_utils import run_bass_kernel_spmd

F32 = mybir.dt.float32
BF16 = mybir.dt.bfloat16
I32 = mybir.dt.int32
U32 = mybir.dt.uint32
AF = mybir.ActivationFunctionType
ALU = mybir.AluOpType
AX = mybir.AxisListType

T = 2048
D = 2048
EPS = 1e-6
TG = 256
NG = T // TG
NCST = 88
DBG_CUT = 99
TWO_PI = 2.0 * math.pi
CW1 = 6.28125
_c2 = np.array([TWO_PI - CW1], np.float32).view(np.uint32) & np.uint32(0xFFFFF000)
CW2 = float(_c2.view(np.float32)[0])
CW3 = float(TWO_PI - CW1 - CW2)


class Res:
    __slots__ = ("name", "w", "rd")

    def __init__(self, name):
        self.name = name
        self.w = None
        self.rd = []


class Op:
    __slots__ = ("eng", "fn", "deps", "signal", "sigval", "isdma", "sem", "semval", "prev")


class Prog:
    ENG = {"pe": "tensor", "act": "scalar", "dve": "vector", "pool": "gpsimd", "sp": "sync"}

    def __init__(self, nc, st, ring=8):
        self.nc = nc
        self.sems = {e: st.enter_context(nc.semaphore("s_" + e)) for e in self.ENG}
        self.cnt = {e: 0 for e in self.ENG}
        self.ring = ring
        self.rings = {q: [st.enter_context(nc.semaphore("d_%s%d" % (q, i))) for i in range(ring)]
                      for q in ("sp", "pool", "act")}
        self.ringcnt = {q: [0] * ring for q in self.rings}
        self.ringpos = {q: 0 for q in self.rings}
        self.ops = []
        self.waited = {}
        self.allres = []

    def res(self, name):
        r = Res(name)
        self.allres.append(r)
        return r

    def _add(self, eng, fn, reads, writes, isdma, indep):
        o = Op()
        o.eng = eng
        o.fn = fn
        o.isdma = isdma
        o.signal = False
        o.sigval = None
        deps = []
        for r in reads:
            if r.w is not None:
                deps.append(r.w)
        for w in writes:
            if w.w is not None:
                deps.append(w.w)
            deps.extend(w.rd)
        seen = set()
        out = []
        for d in deps:
            if id(d) in seen:
                continue
            seen.add(id(d))
            if (not d.isdma) and (not isdma) and d.eng == eng and (eng == "pe" or indep):
                continue
            out.append(d)
            if not d.isdma:
                d.signal = True
        o.deps = out
        if isdma:
            q = eng
            slot = self.ringpos[q] % self.ring
            self.ringpos[q] += 1
            o.sem = self.rings[q][slot]
            o.prev = self.ringcnt[q][slot]
            self.ringcnt[q][slot] += 16
            o.semval = self.ringcnt[q][slot]
        for w in writes:
            w.w = o
            w.rd = []
        for r in reads:
            if r in writes:
                continue
            if not isdma:
                r.rd = [x for x in r.rd if x.isdma or x.eng != eng]
            r.rd.append(o)
        self.ops.append(o)
        return o

    _defer = None

    def defer_begin(self):
        self._defer = []

    def defer_end(self):
        d = self._defer
        self._defer = None
        return d

    def op(self, eng, fn, reads=(), writes=(), indep=False):
        if self._defer is not None:
            self._defer.append(lambda: self._add(eng, fn, list(reads), list(writes), False, indep))
            return None
        return self._add(eng, fn, list(reads), list(writes), False, indep)

    def dma(self, q, fn, reads=(), writes=()):
        if self._defer is not None:
            self._defer.append(lambda: self._add(q, fn, list(reads), list(writes), True, False))
            return None
        return self._add(q, fn, list(reads), list(writes), True, False)

    def _wait(self, engobj, e, sem, key, val):
        if val <= 0:
            return
        k = (e, key)
        if self.waited.get(k, 0) >= val:
            return
        self.waited[k] = val
        engobj.wait_ge(sem, val)

    def emit(self):
        ops = self.ops
        self.ops = []
        for o in ops:
            if (not o.isdma) and o.signal:
                self.cnt[o.eng] += 1
                o.sigval = self.cnt[o.eng]
        with self.nc.Block() as block:
            for e, attr in self.ENG.items():
                mine = [o for o in ops if o.eng == e]
                if not mine:
                    continue

                def body(engobj, mine=mine, e=e):
                    for o in mine:
                        for d in o.deps:
                            if d.isdma:
                                self._wait(engobj, e, d.sem, id(d.sem), d.semval)
                            else:
                                self._wait(engobj, e, self.sems[d.eng], d.eng, d.sigval)
                        if o.isdma:
                            self._wait(engobj, e, o.sem, id(o.sem), o.prev)
                            o.fn(engobj).then_inc(o.sem, 16)
                        else:
                            ins = o.fn(engobj)
                            if o.signal:
                                ins.then_inc(self.sems[e], 1)
                    if e in self.rings:
                        for i, s in enumerate(self.rings[e]):
                            self._wait(engobj, e, s, id(s), self.ringcnt[e][i])

                getattr(block, attr)(body)
        for r in self.allres:
            r.w = None
            r.rd = []


def build(stop_after=None, dbg=()):
    nc = bass.Bass("TRN2", target_bir_lowering=False)

    def din(name, shape, dt=F32):
        return nc.dram_tensor(name, list(shape), dt, kind="ExternalInput").ap()

    def dscr(name, shape, dt):
        kind = "ExternalOutput" if name in dbg else "Internal"
        return nc.dram_tensor(name, list(shape), dt, kind=kind).ap()

    x = din("x", [T, D])
    pin = din("p", [T, 256])
    pos = din("pos", [1, T], I32)
    cst = din("cst", [128, NCST])
    mats = din("mats", [128, 6 * 128])
    maskc = din("maskc", [128, 2 * TG])
    Win = din("Win", [17, 128, 2048])
    Wpool = din("Wpool", [128, 2048])
    Wuq = din("Wuq", [128, 4 * 1536])
    Wuk = din("Wuk", [128, 4096])
    Wv = din("Wv", [128, 4096])
    Wout = din("Wout", [4, 128, 8192])
    Wpq = din("Wpq", [16, 128, 2048])
    subk = din("subk", [128, 2048])
    UT = din("UT", [128, 128, 2048])
    EV = din("EV", [256, 128, 1024])
    Wg = din("Wg", [4, 128, 8192])
    Wpp = din("Wpp", [128, 4096])
    out = nc.dram_tensor("out", [T, D], F32, kind="ExternalOutput").ap()

    Winb = dscr("Winb", [17, 128, 2048], BF16)
    Woutb = dscr("Woutb", [4, 128, 8192], BF16)
    Wpqb = dscr("Wpqb", [16, 128, 2048], BF16)
    Wgb = dscr("Wgb", [4, 128, 8192], BF16)
    UTb = dscr("UTb", [128, 128, 2048], BF16)
    EVb = dscr("EVb", [256, 128, 1024], BF16)
    CQ = dscr("CQ", [NG, 128, 4 * TG], BF16)
    YP = dscr("YP", [NG, 128, 8 * TG], BF16)
    H1 = dscr("H1", [T, D], F32)
    H2 = dscr("H2", [T, D], F32)
    XN2 = dscr("XN2", [NG, 128, 16 * TG], BF16)
    IRs = dscr("IRs", [3, 128, T], F32)

    st0 = ExitStack()
    with st0:
        P = Prog(nc, st0)

        def sbuf(st, name, shape, dt):
            return st.enter_context(nc.sbuf_tensor(name, list(shape), dt))

        def psum(st, name, shape, dt=F32):
            return st.enter_context(nc.psum_tensor(name, list(shape), dt))

        cst_sb = sbuf(st0, "cst_sb", [128, NCST], F32)
        mats_sb = sbuf(st0, "mats_sb", [128, 6 * 128], F32)
        ident_bf = sbuf(st0, "ident_bf", [128, 128], BF16)
        ones_bf = sbuf(st0, "ones_bf", [128, 128], BF16)
        iota_bf = sbuf(st0, "iota_bf", [128, 128], BF16)
        stKV = ExitStack()
        cosT = sbuf(stKV, "cosT", [128, T], F32)
        sinT = sbuf(stKV, "sinT", [128, T], F32)
        R_const = P.res("const")
        R_rope = P.res("rope")
        ident_f = mats_sb[:, 0:128]
        ones_f = mats_sb[:, 128:256]
        Llo_f = mats_sb[:, 256:384]
        Lhi_f = mats_sb[:, 384:512]
        perm_f = mats_sb[:, 512:640]
        iota_f = mats_sb[:, 640:768]

        RWin = [P.res("Win%d" % c) for c in range(17)]
        RWout = [P.res("Wout%d" % c) for c in range(4)]
        RWpq = [P.res("Wpq%d" % c) for c in range(16)]
        RWg = [P.res("Wg%d" % c) for c in range(4)]
        pro_list = []
        for c in range(17):
            pro_list.append((Winb[c], Win[c], RWin[c]))
        for nb in range(4):
            for q in range(4):
                pro_list.append((Woutb[nb, 32 * q:32 * q + 32, :], Wout[nb, 32 * q:32 * q + 32, :], RWout[nb]))
        for c in range(16):
            pro_list.append((Wpqb[c], Wpq[c], RWpq[c]))
        for nb in range(4):
            for q in range(4):
                pro_list.append((Wgb[nb, 32 * q:32 * q + 32, :], Wg[nb, 32 * q:32 * q + 32, :], RWg[nb]))
        for i in range(128):
            pro_list.append((UTb[i], UT[i], None))
            pro_list.append((EVb[2 * i:2 * i + 2], EV[2 * i:2 * i + 2], None))
        pro_state = {"i": 0}

        def pro_some(n):
            for _ in range(n):
                if pro_state["i"] >= len(pro_list):
                    return
                o_, i_, r_ = pro_list[pro_state["i"]]
                pro_state["i"] += 1
                P.dma("pool", lambda e, o_=o_, i_=i_: e.dma_start(out=o_, in_=i_, max_dma_last_dim=4096),
                      writes=([r_] if r_ is not None else []))

        with ExitStack() as st:
            posi = sbuf(st, "posi", [128, T], I32)
            ang = sbuf(st, "ang", [128, T], F32)
            kf = sbuf(st, "kf", [128, T], F32)
            ki = sbuf(st, "ki", [128, T], I32)
            rr = sbuf(st, "rr", [128, T], F32)
            r2 = sbuf(st, "r2", [128, T], F32)
            Rp, Ra, Rk, Rki, Rr, Rr2 = [P.res(n) for n in ("posi", "ang", "kf", "ki", "rr", "r2")]
            P.dma("sp", lambda e: e.dma_start(out=cst_sb[:], in_=cst), writes=[R_const])
            P.dma("sp", lambda e: e.dma_start(out=mats_sb[:], in_=mats), writes=[R_const])
            P.dma("sp", lambda e: e.dma_start(out=posi[:], in_=pos.to_broadcast([128, T])), writes=[Rp])
            P.op("dve", lambda e: e.tensor_copy(out=ident_bf[:], in_=ident_f), reads=[R_const], writes=[R_const])
            P.op("dve", lambda e: e.tensor_copy(out=ones_bf[:], in_=ones_f), reads=[R_const], writes=[R_const])
            P.op("dve", lambda e: e.tensor_copy(out=iota_bf[:], in_=iota_f), reads=[R_const], writes=[R_const])
            P.op("dve", lambda e: e.tensor_copy(out=ang[:], in_=posi[:]), reads=[Rp], writes=[Ra])
            P.op("dve", lambda e: e.tensor_scalar(out=ang[:], in0=ang[:], scalar1=cst_sb[:, 68:69], scalar2=None,
                                                  op0=ALU.mult), reads=[Ra, R_const], writes=[Ra])
            P.op("dve", lambda e: e.tensor_scalar(out=kf[:], in0=ang[:], scalar1=1.0 / TWO_PI, scalar2=None,
                                                  op0=ALU.mult), reads=[Ra], writes=[Rk])
            P.op("dve", lambda e: e.tensor_copy(out=ki[:], in_=kf[:]), reads=[Rk], writes=[Rki])
            P.op("dve", lambda e: e.tensor_copy(out=kf[:], in_=ki[:]), reads=[Rki], writes=[Rk])
            P.op("dve", lambda e: e.scalar_tensor_tensor(out=rr[:], in0=kf[:], scalar=-CW1, in1=ang[:],
                                                         op0=ALU.mult, op1=ALU.add), reads=[Rk, Ra], writes=[Rr])
            P.op("dve", lambda e: e.scalar_tensor_tensor(out=r2[:], in0=kf[:], scalar=-CW2, in1=rr[:],
                                                         op0=ALU.mult, op1=ALU.add), reads=[Rk, Rr], writes=[Rr2])
            P.op("dve", lambda e: e.scalar_tensor_tensor(out=rr[:], in0=kf[:], scalar=-CW3, in1=r2[:],
                                                         op0=ALU.mult, op1=ALU.add), reads=[Rk, Rr2], writes=[Rr])
            def wrap_sin(dst, shift, sign_col):
                P.op("dve", lambda e: e.tensor_scalar(out=r2[:], in0=rr[:], scalar1=shift, scalar2=None, op0=ALU.add),
                     reads=[Rr], writes=[Rr2])
                P.op("dve", lambda e: e.tensor_scalar(out=kf[:], in0=r2[:], scalar1=math.pi, scalar2=-TWO_PI,
                                                      op0=ALU.is_gt, op1=ALU.mult), reads=[Rr2], writes=[Rk])
                P.op("dve", lambda e: e.tensor_tensor(out=r2[:], in0=r2[:], in1=kf[:], op=ALU.add),
                     reads=[Rr2, Rk], writes=[Rr2])
                P.op("dve", lambda e: e.tensor_scalar(out=kf[:], in0=r2[:], scalar1=-math.pi, scalar2=TWO_PI,
                                                      op0=ALU.is_lt, op1=ALU.mult), reads=[Rr2], writes=[Rk])
                P.op("dve", lambda e: e.tensor_tensor(out=r2[:], in0=r2[:], in1=kf[:], op=ALU.add),
                     reads=[Rr2, Rk], writes=[Rr2])
                P.op("dve", lambda e: e.tensor_scalar(out=r2[:], in0=r2[:], scalar1=math.pi, scalar2=-math.pi,
                                                      op0=ALU.min, op1=ALU.max), reads=[Rr2], writes=[Rr2])
                P.op("act", lambda e: e.activation(out=dst[:], in_=r2[:], func=AF.Sin), reads=[Rr2], writes=[R_rope])
                if sign_col is not None:
                    P.op("dve", lambda e: e.tensor_scalar(out=dst[:], in0=dst[:], scalar1=cst_sb[:, sign_col:sign_col + 1],
                                                          scalar2=None, op0=ALU.mult),
                         reads=[R_rope, R_const], writes=[R_rope])

            wrap_sin(sinT, 0.0, 69)
            wrap_sin(cosT, math.pi / 2, None)
            pro_some(17)
            P.emit()

        def rmsnorm_multi(items, g0, all_act=False):
            for (src, Rsrc, dstfn, Rdst, bufs) in items:
                xs, Rxs, ss, ms, Rst, pst, Rpst = bufs
                P.op("act", lambda e, xs=xs, src=src, ss=ss: e.activation(out=xs[:], in_=src, func=AF.Square,
                                                                          accum_out=ss[:, 0:1]),
                     reads=[Rsrc], writes=[Rxs, Rst])
            for (src, Rsrc, dstfn, Rdst, bufs) in items:
                xs, Rxs, ss, ms, Rst, pst, Rpst = bufs
                P.op("act", lambda e, ss=ss, ms=ms: e.activation(out=ms[:, 0:1], in_=ss[:, 0:1], func=AF.Ln,
                                                                 scale=1.0 / D, bias=cst_sb[:, 86:87]),
                     reads=[Rst, R_const], writes=[Rst])
            for (src, Rsrc, dstfn, Rdst, bufs) in items:
                xs, Rxs, ss, ms, Rst, pst, Rpst = bufs
                P.op("act", lambda e, ms=ms: e.activation(out=ms[:, 2:3], in_=ms[:, 0:1], func=AF.Exp, scale=-0.5),
                     reads=[Rst], writes=[Rst])
            for (src, Rsrc, dstfn, Rdst, bufs) in items:
                xs, Rxs, ss, ms, Rst, pst, Rpst = bufs
                P.op("act", lambda e, xs=xs, src=src, ms=ms: e.activation(out=xs[:], in_=src, func=AF.Copy,
                                                                          scale=ms[:, 2:3]),
                     reads=[Rsrc, Rst], writes=[Rxs])
            for half in range(2):
                for (src, Rsrc, dstfn, Rdst, bufs) in items:
                    xs, Rxs, ss, ms, Rst, pst, Rpst = bufs
                    for j in range(8):
                        dc = half * 8 + j
                        P.op("pe", lambda e, dc=dc, j=j, half=half, pst=pst, xs=xs: e.transpose(
                            out=pst[half][:, j * 128:(j + 1) * 128], in_=xs[:, dc * 128:(dc + 1) * 128],
                            identity=ident_bf[:]), reads=[Rxs, R_const], writes=[Rpst[half]])
                for (src, Rsrc, dstfn, Rdst, bufs) in items:
                    xs, Rxs, ss, ms, Rst, pst, Rpst = bufs
                    for j in range(8):
                        dc = half * 8 + j
                        if j % 2 == 0 and not all_act:
                            P.op("dve", lambda e, dc=dc, j=j, half=half, pst=pst, dstfn=dstfn: e.tensor_scalar(
                                out=dstfn(dc), in0=pst[half][:, j * 128:(j + 1) * 128],
                                scalar1=cst_sb[:, g0 + dc:g0 + dc + 1], scalar2=None, op0=ALU.mult),
                                reads=[Rpst[half], R_const], writes=[Rdst], indep=True)
                        else:
                            P.op("act", lambda e, dc=dc, j=j, half=half, pst=pst, dstfn=dstfn: e.activation(
                                out=dstfn(dc), in_=pst[half][:, j * 128:(j + 1) * 128], func=AF.Copy,
                                scale=cst_sb[:, g0 + dc:g0 + dc + 1]),
                                reads=[Rpst[half], R_const], writes=[Rdst], indep=True)

        def rmsnorm_T(src, Rsrc, g0, dstfn, Rdst, bufs, tag):
            rmsnorm_multi([(src, Rsrc, dstfn, Rdst, bufs)], g0)

        def rms_bufs(st, tag):
            xs = sbuf(st, "rn_xs" + tag, [128, D], BF16)
            ss = sbuf(st, "rn_ss" + tag, [128, 1], F32)
            ms = sbuf(st, "rn_ms" + tag, [128, 4], F32)
            pst = [psum(st, "rn_ps%d%s" % (h, tag), [128, 1024], BF16) for h in range(2)]
            return (xs, P.res("rn_xs" + tag), ss, ms, P.res("rn_st" + tag), pst,
                    [P.res("rn_ps0" + tag), P.res("rn_ps1" + tag)])

        KT = sbuf(stKV, "KT", [128, 8, T], BF16)
        KRT = sbuf(stKV, "KRT", [128, T], BF16)
        Vsb = sbuf(stKV, "Vsb", [128, 16, 1024], BF16)
        rkcol = sbuf(stKV, "rkcol", [128, 16, 8], F32)
        R_K = [P.res("K%d" % g) for g in range(NG)]
        R_V = [P.res("V%d" % g) for g in range(NG)]

        with ExitStack() as st:
            wuk_sb = sbuf(st, "wuk_sb", [128, 4, 1024], BF16)
            wv_sb = sbuf(st, "wv_sb", [128, 4, 1024], BF16)
            wpool_sb = sbuf(st, "wpool_sb", [128, 4, 2, 256], BF16)
            R_w = P.res("m1w")
            P.dma("pool", lambda e: e.dma_start(out=wuk_sb[:], in_=Wuk.rearrange("p (a b) -> p a b", a=4),
                                                max_dma_last_dim=4096), writes=[R_w])
            P.dma("pool", lambda e: e.dma_start(out=wv_sb[:], in_=Wv.rearrange("p (a b) -> p a b", a=4),
                                                max_dma_last_dim=4096), writes=[R_w])
            P.dma("pool", lambda e: e.dma_start(out=wpool_sb[:], in_=Wpool.rearrange("p (a b c) -> p a b c", a=4, b=2),
                                                max_dma_last_dim=1024), writes=[R_w])
            xt = [sbuf(st, "m1_xt%d" % i, [128, D], F32) for i in range(2)]
            Rxt = [P.res("m1_xt%d" % i) for i in range(2)]
            rbs = [rms_bufs(st, "m1_%d" % i) for i in range(2)]
            xnT = sbuf(st, "xnT", [128, 16, TG], BF16)
            RxnT = P.res("xnT")
            wbuf = [sbuf(st, "m1_wb%d" % i, [128, 16, 128], BF16) for i in range(6)]
            Rwb = [P.res("m1_wb%d" % i) for i in range(6)]
            psz = [psum(st, "psz%d" % i, [128, 512], F32) for i in range(2)]
            Rpsz = [P.res("psz%d" % i) for i in range(2)]
            pz_ring = [psz[0][:, 0:TG], psz[1][:, 0:TG]]
            Rpz_ring = [Rpsz[0], Rpsz[1]]
            for k_ in range(2):
                for h_ in range(2):
                    pz_ring.append(rbs[k_][5][h_][:].bitcast(F32)[:, 0:TG])
                    Rpz_ring.append(rbs[k_][6][h_])
            psm = [psum(st, "psm%d" % i, [128, 512], F32) for i in range(2)]
            Rpsm = [P.res("psm%d" % i) for i in range(2)]
            halo = sbuf(st, "halo", [128, 8, 16], F32)
            Rhalo = [P.res("halo%d" % c) for c in range(8)]
            zpad = [sbuf(st, "zpad%d" % i, [128, 16 + TG], F32) for i in range(2)]
            Rzp = [P.res("zpad%d" % i) for i in range(2)]
            abuf = [sbuf(st, "abuf%d" % i, [128, 16 + TG], F32) for i in range(2)]
            Rab = [P.res("abuf%d" % i) for i in range(2)]
            fix16 = sbuf(st, "fix16", [128, 16], F32)
            Rfix = P.res("fix16")
            dbuf = sbuf(st, "dbuf", [128, 2, TG], BF16)
            Rdb = [P.res("dbuf%d" % i) for i in range(2)]
            ypo = sbuf(st, "ypo", [128, 8, TG], BF16)
            Rypo = P.res("ypo")
            zlat = sbuf(st, "zlat", [128, 4, TG], F32)
            Rzlat = P.res("zlat")
            sq = [sbuf(st, "m1_sq%d" % i, [128, TG], F32) for i in range(2)]
            Rsq = [P.res("m1_sq%d" % i) for i in range(2)]
            rlat = sbuf(st, "rlat", [128, TG], F32)
            Rrlat = P.res("rlat")
            cqn = sbuf(st, "cqn", [128, 4, TG], BF16)
            Rcqn = P.res("cqn")
            ckvn = sbuf(st, "ckvn", [128, 4, TG], BF16)
            Rckvn = P.res("ckvn")
            krg = sbuf(st, "krg", [128, TG], F32)
            krsq = sbuf(st, "krsq", [128, TG], F32)
            Rkr = P.res("kr")
            t1 = sbuf(st, "m1_t1", [128, TG], F32)
            t2 = sbuf(st, "m1_t2", [128, TG], F32)
            Rt1, Rt2 = P.res("m1_t1"), P.res("m1_t2")
            colb = sbuf(st, "colb", [128, 3, 16], F32)
            Rcolb = P.res("colb")

            P.op("dve", lambda e: e.memset(halo[:], 0.0), writes=Rhalo)
            dq = []

            def dq_tick():
                for it in dq:
                    it[0] -= 1
                while dq and dq[0][0] <= 0:
                    for f_ in dq.pop(0)[1]:
                        f_()

            def dq_flush():
                while dq:
                    for f_ in dq.pop(0)[1]:
                        f_()

            wcnt = [0]
            pcnt = [0]
            mcnt = [0]

            for g in range(NG):
                t0 = g * TG
                items = []
                for tt in range(TG // 128):
                    k = tt % 2
                    row0 = t0 + tt * 128
                    P.dma("sp", lambda e, k=k, row0=row0: e.dma_start(out=xt[k][:], in_=x[row0:row0 + 128, :]),
                          writes=[Rxt[k]])
                    items.append((xt[k][:], Rxt[k], (lambda dc, tt=tt: xnT[:, dc, tt * 128:(tt + 1) * 128]), RxnT, rbs[k]))
                rmsnorm_multi(items, 0)
                pro_some(8)
                for c in range(17):
                    wk = wcnt[0] % 6
                    wcnt[0] += 1
                    P.dma("sp", lambda e, wk=wk, c=c: e.dma_start(
                        out=wbuf[wk][:], in_=Winb[c].rearrange("p (a b) -> p a b", a=16)),
                        reads=[RWin[c]], writes=[Rwb[wk]])
                    pk = pcnt[0] % len(pz_ring)
                    pcnt[0] += 1
                    pz = pz_ring[pk]
                    for dc in range(16):
                        P.op("pe", lambda e, wk=wk, dc=dc, pz=pz: e.matmul(
                            out=pz, lhsT=wbuf[wk][:, dc, :], rhs=xnT[:, dc, :], start=(dc == 0), stop=(dc == 15)),
                            reads=[Rwb[wk], RxnT], writes=[Rpz_ring[pk]])
                    dq_tick()
                    if c < 8:
                        grp = c // 2
                        S = grp + 1
                        w = 2 ** S
                        zk = c % 2
                        zp = zpad[zk]
                        P.op("act", lambda e, zp=zp, pz=pz: e.activation(out=zp[:, 16:16 + TG], in_=pz, func=AF.Copy),
                             reads=[Rpz_ring[pk]], writes=[Rzp[zk]])
                        P.op("dve", lambda e, zp=zp, c=c: e.tensor_copy(out=zp[:, 0:16], in_=halo[:, c, :]),
                             reads=[Rhalo[c]], writes=[Rzp[zk]])
                        P.op("dve", lambda e, zp=zp, c=c: e.tensor_copy(out=halo[:, c, :], in_=zp[:, TG:TG + 16]),
                             reads=[Rzp[zk]], writes=[Rhalo[c]])
                        prev, Rprev = zp, Rzp[zk]
                        L = 16 + TG
                        for s in range(1, S + 1):
                            sh = 2 ** (s - 1)
                            lo = 2 ** s - 1
                            ab = abuf[s % 2]
                            P.op("dve", lambda e, ab=ab, prev=prev, lo=lo, sh=sh, L=L: e.tensor_tensor(
                                out=ab[:, lo:L], in0=prev[:, lo:L], in1=prev[:, lo - sh:L - sh], op=ALU.add),
                                reads=[Rprev], writes=[Rab[s % 2]])
                            prev, Rprev = ab, Rab[s % 2]
                        P.op("dve", lambda e, prev=prev, zp=zp, zk=zk, w=w: e.scalar_tensor_tensor(
                            out=dbuf[:, zk, :], in0=prev[:, 16:16 + TG], scalar=1.0 / w, in1=zp[:, 16:16 + TG],
                            op0=ALU.mult, op1=ALU.subtract), reads=[Rprev, Rzp[zk]], writes=[Rdb[zk]])
                        if g == 0:
                            P.op("dve", lambda e, prev=prev, w=w: e.tensor_tensor(
                                out=fix16[:, 0:w - 1], in0=prev[:, 16:16 + w - 1], in1=cst_sb[:, 70:70 + w - 1],
                                op=ALU.mult), reads=[Rprev, R_const], writes=[Rfix])
                            P.op("dve", lambda e, zp=zp, zk=zk, w=w: e.tensor_tensor(
                                out=dbuf[:, zk, 0:w - 1], in0=fix16[:, 0:w - 1], in1=zp[:, 16:16 + w - 1],
                                op=ALU.subtract), reads=[Rfix, Rzp[zk]], writes=[Rdb[zk]])
                        if c % 2 == 1:
                            P.defer_begin()
                            for oc in range(2):
                                mk = mcnt[0] % 2
                                mcnt[0] += 1
                                pm = psm[mk][:, 0:TG]
                                for ic in range(2):
                                    P.op("pe", lambda e, pm=pm, grp=grp, ic=ic, oc=oc: e.matmul(
                                        out=pm, lhsT=wpool_sb[:, grp, ic, oc * 128:(oc + 1) * 128], rhs=dbuf[:, ic, :],
                                        start=(ic == 0), stop=(ic == 1)), reads=[R_w, Rdb[ic]], writes=[Rpsm[mk]])
                                cc = 2 * grp + oc
                                P.op("act", lambda e, pm=pm, cc=cc: e.activation(
                                    out=ypo[:, cc, :], in_=pm, func=AF.Copy, scale=cst_sb[:, 56 + cc:57 + cc]),
                                    reads=[Rpsm[mk], R_const], writes=[Rypo], indep=True)
                            dq.append([1, P.defer_end()])
                    elif c < 16:
                        j = (c - 8) % 4
                        isq = c < 12
                        sk = c % 2
                        P.op("act", lambda e, j=j, pz=pz: e.activation(out=zlat[:, j, :], in_=pz, func=AF.Copy),
                             reads=[Rpz_ring[pk]], writes=[Rzlat], indep=True)
                        P.op("act", lambda e, sk=sk, pz=pz: e.activation(out=sq[sk][:], in_=pz, func=AF.Square),
                             reads=[Rpz_ring[pk]], writes=[Rsq[sk]])
                        P.defer_begin()
                        if j == 0:
                            smk = mcnt[0] % 2
                            mcnt[0] += 1
                        pm = psm[smk][:, 0:TG]
                        P.op("pe", lambda e, pm=pm, sk=sk, j=j: e.matmul(out=pm, lhsT=ones_f, rhs=sq[sk][:],
                                                                          start=(j == 0), stop=(j == 3)),
                             reads=[Rsq[sk], R_const], writes=[Rpsm[smk]])
                        if j == 3:
                            P.op("act", lambda e, pm=pm: e.activation(out=rlat[:], in_=pm, func=AF.Ln, scale=1.0 / 512,
                                                                        bias=cst_sb[:, 86:87]),
                                 reads=[Rpsm[smk], R_const], writes=[Rrlat])
                            P.op("act", lambda e: e.activation(out=rlat[:], in_=rlat[:], func=AF.Exp, scale=-0.5),
                                 reads=[Rrlat], writes=[Rrlat])
                            dst, Rd, gc = (cqn, Rcqn, 48) if isq else (ckvn, Rckvn, 52)
                            for jj in range(4):
                                P.op("dve", lambda e, dst=dst, jj=jj, gc=gc: e.scalar_tensor_tensor(
                                    out=dst[:, jj, :], in0=zlat[:, jj, :], scalar=cst_sb[:, gc + jj:gc + jj + 1],
                                    in1=rlat[:], op0=ALU.mult, op1=ALU.mult),
                                    reads=[Rzlat, Rrlat, R_const], writes=[Rd], indep=(jj > 0))
                            if isq:
                                P.dma("sp", lambda e, g=g: e.dma_start(
                                    out=CQ[g].rearrange("p (a b) -> p a b", a=4), in_=cqn[:]), reads=[Rcqn])
                        dq.append([1, P.defer_end()])
                    else:
                        P.op("act", lambda e, pz=pz: e.activation(out=krg[:], in_=pz, func=AF.Copy,
                                                                    scale=cst_sb[:, 67:68]),
                             reads=[Rpz_ring[pk], R_const], writes=[Rkr])
                        P.op("act", lambda e, pz=pz: e.activation(out=krsq[:], in_=pz, func=AF.Square),
                             reads=[Rpz_ring[pk]], writes=[Rkr])
                dq_flush()
                P.dma("sp", lambda e, g=g: e.dma_start(out=YP[g].rearrange("p (a b) -> p a b", a=8), in_=ypo[:]),
                      reads=[Rypo])
                pro_some(6)
                mkc = mcnt[0] % 2
                mcnt[0] += 1
                pcol = psm[mkc]
                ntt = TG // 128
                for h in range(8):
                    pk = pcnt[0] % len(pz_ring)
                    pcnt[0] += 1
                    pz = pz_ring[pk]
                    for fc in range(4):
                        P.op("pe", lambda e, pz=pz, fc=fc, h=h: e.matmul(
                            out=pz, lhsT=wuk_sb[:, fc, h * 128:(h + 1) * 128], rhs=ckvn[:, fc, :],
                            start=(fc == 0), stop=(fc == 3)), reads=[R_w, Rckvn], writes=[Rpz_ring[pk]])
                    dq_tick()
                    P.op("act", lambda e, pz=pz, h=h, t0=t0: e.activation(
                        out=KT[:, h, t0:t0 + TG], in_=pz, func=AF.Copy, scale=cst_sb[:, 66:67]),
                        reads=[Rpz_ring[pk], R_const], writes=[R_K[g]], indep=True)
                    sk = h % 2
                    P.op("act", lambda e, pz=pz, sk=sk: e.activation(out=sq[sk][:], in_=pz, func=AF.Square),
                         reads=[Rpz_ring[pk]], writes=[Rsq[sk]])
                    P.defer_begin()
                    for tt in range(ntt):
                        col = (h * ntt + tt) * 2
                        P.op("pe", lambda e, sk=sk, tt=tt, col=col: e.matmul(
                            out=pcol[:, col:col + 2], lhsT=sq[sk][:, tt * 128:(tt + 1) * 128], rhs=ones_f[:, 0:2],
                            start=True, stop=False), reads=[Rsq[sk], R_const], writes=[Rpsm[mkc]])
                        P.op("pe", lambda e, tt=tt, col=col: e.matmul(
                            out=pcol[:, col:col + 2], lhsT=krsq[0:64, tt * 128:(tt + 1) * 128], rhs=ones_f[0:64, 0:2],
                            start=False, stop=True), reads=[Rkr, R_const], writes=[Rpsm[mkc]])
                    dq.append([1, P.defer_end()])
                dq_flush()
                nc_ = 8 * ntt
                P.op("dve", lambda e, nc_=nc_: e.tensor_scalar(
                    out=colb[:, 0, 0:nc_], in0=pcol[:, 0:2 * nc_].rearrange("p (a b) -> p a b", b=2)[:, :, 0],
                    scalar1=1.0, scalar2=192.0 * EPS, op0=ALU.mult, op1=ALU.add), reads=[Rpsm[mkc]], writes=[Rcolb])
                P.op("act", lambda e, nc_=nc_: e.activation(out=colb[:, 1, 0:nc_], in_=colb[:, 0, 0:nc_], func=AF.Ln),
                     reads=[Rcolb], writes=[Rcolb])
                P.op("act", lambda e, nc_=nc_: e.activation(out=colb[:, 2, 0:nc_], in_=colb[:, 1, 0:nc_], func=AF.Exp,
                                                            scale=-0.5),
                     reads=[Rcolb], writes=[Rcolb])
                P.op("dve", lambda e, g=g, ntt=ntt, nc_=nc_: e.tensor_copy(
                    out=rkcol[:, g * ntt:(g + 1) * ntt, :].rearrange("p t h -> p h t"),
                    in_=colb[:, 2, 0:nc_].rearrange("p (h t) -> p h t", t=ntt)), reads=[Rcolb], writes=[R_K[g]])
                mk = mcnt[0] % 2
                mcnt[0] += 1
                pm = psm[mk][:, 0:TG]
                P.op("pe", lambda e, pm=pm: e.matmul(out=pm, lhsT=perm_f, rhs=krg[:], start=True, stop=True),
                     reads=[Rkr, R_const], writes=[Rpsm[mk]])
                P.op("dve", lambda e, t0=t0: e.tensor_tensor(out=t1[:], in0=krg[:], in1=cosT[:, t0:t0 + TG], op=ALU.mult),
                     reads=[Rkr, R_rope], writes=[Rt1])
                P.op("dve", lambda e, pm=pm, t0=t0: e.tensor_tensor(out=t2[:], in0=pm, in1=sinT[:, t0:t0 + TG],
                                                                     op=ALU.mult),
                     reads=[Rpsm[mk], R_rope], writes=[Rt2])
                P.op("dve", lambda e, t0=t0: e.tensor_tensor(out=KRT[:, t0:t0 + TG], in0=t1[:], in1=t2[:], op=ALU.add),
                     reads=[Rt1, Rt2], writes=[R_K[g]])
                for tt in range(ntt):
                    for nb in range(2):
                        mk = mcnt[0] % 2
                        mcnt[0] += 1
                        for fc in range(4):
                            P.op("pe", lambda e, mk=mk, fc=fc, tt=tt, nb=nb: e.matmul(
                                out=psm[mk][:], lhsT=ckvn[:, fc, tt * 128:(tt + 1) * 128],
                                rhs=wv_sb[:, fc, nb * 512:(nb + 1) * 512], start=(fc == 0), stop=(fc == 3)),
                                reads=[Rckvn, R_w], writes=[Rpsm[mk]])
                        P.op("act", lambda e, mk=mk, tt=tt, nb=nb, g=g, ntt=ntt: e.activation(
                            out=Vsb[:, g * ntt + tt, nb * 512:(nb + 1) * 512], in_=psm[mk][:], func=AF.Copy),
                            reads=[Rpsm[mk]], writes=[R_V[g]], indep=True)
            P.emit()
        if stop_after == "M1":
            stKV.close()
            return nc, None, st0

        with ExitStack() as st:
            wuq_sb = sbuf(st, "wuq_sb", [128, 4, 1536], BF16)
            mask_f = sbuf(st, "mask_f", [128, 2, TG], F32)
            mask_b = sbuf(st, "mask_b", [128, 2, TG], BF16)
            R_w = P.res("m2w")
            P.dma("pool", lambda e: e.dma_start(out=wuq_sb[:], in_=Wuq.rearrange("p (a b) -> p a b", a=4),
                                                max_dma_last_dim=2048), writes=[R_w])
            P.dma("sp", lambda e: e.dma_start(out=mask_f[:], in_=maskc.rearrange("p (a b) -> p a b", a=2)),
                  writes=[R_w])
            P.op("dve", lambda e: e.tensor_copy(out=mask_b[:], in_=mask_f[:]), reads=[R_w], writes=[R_w])
            cqn = sbuf(st, "m2_cqn", [128, 4, TG], BF16)
            Rcqn = P.res("m2_cqn")
            ymix = sbuf(st, "ymix", [128, 16, TG], BF16)
            Rypool = P.res("ymix_pool")
            Rymla = P.res("ymix_mla")
            psq = [psum(st, "psq%d" % i, [128, 512], F32) for i in range(2)]
            Rpsq = [P.res("psq%d" % i) for i in range(2)]
            pss = [psum(st, "pss%d" % i, [128, 512], F32) for i in range(2)]
            Rpss = [P.res("pss%d" % i) for i in range(2)]
            pso = psum(st, "pso", [128, 512], F32)
            psd = psum(st, "psd", [128, 512], F32)
            Rpso, Rpsd = P.res("pso"), P.res("psd")
            psh = [psum(st, "psh%d" % i, [128, 512], F32) for i in range(2)]
            Rpsh = [P.res("psh%d" % i) for i in range(2)]
            qtmp = [sbuf(st, "qtmp%d" % i, [128, TG], F32) for i in range(2)]
            sqq = [sbuf(st, "sqq%d" % i, [128, TG], F32) for i in range(2)]
            rq = [sbuf(st, "rq%d" % i, [128, TG], F32) for i in range(2)]
            Rqtmp = [P.res("qtmp%d" % i) for i in range(2)]
            Rsqq = [P.res("sqq%d" % i) for i in range(2)]
            Rrq = [P.res("rq%d" % i) for i in range(2)]
            qrg = sbuf(st, "qrg", [128, TG], F32)
            sqr = sbuf(st, "sqr", [128, TG], F32)
            Rqrg, Rsqr = P.res("qrg"), P.res("sqr")
            t1 = sbuf(st, "m2_t1", [128, TG], F32)
            t2 = sbuf(st, "m2_t2", [128, TG], F32)
            Rt1, Rt2 = P.res("m2_t1"), P.res("m2_t2")
            QTs = [sbuf(st, "QT%d" % i, [128, TG], BF16) for i in range(4)]
            RQTs = [P.res("QT%d" % i) for i in range(4)]
            QRTs = [sbuf(st, "QRT%d" % i, [128, TG], BF16) for i in range(2)]
            RQRTs = [P.res("QRT%d" % i) for i in range(2)]
            pT = [sbuf(st, "pT%d" % i, [128, TG], BF16) for i in range(3)]
            RpT = [P.res("pT%d" % i) for i in range(3)]
            rec = sbuf(st, "rec", [128, TG], F32)
            Rrec = P.res("rec")
            wob = [sbuf(st, "wob%d" % i, [128, 16, 512], BF16) for i in range(3)]
            Rwob = [P.res("wob%d" % i) for i in range(3)]
            xb = [sbuf(st, "m2_xb%d" % i, [128, 512], F32) for i in range(3)]
            Rxb = [P.res("m2_xb%d" % i) for i in range(3)]
            hb = [sbuf(st, "m2_hb%d" % i, [128, 512], F32) for i in range(3)]
            Rhb = [P.res("m2_hb%d" % i) for i in range(3)]
            qcnt = [0]
            scnt = [0]
            ptc = [0]
            wcnt = [0]
            hcnt = [0]
            ntt = TG // 128

            for g in range(NG):
                t0 = g * TG
                P.dma("sp", lambda e, g=g: e.dma_start(out=cqn[:], in_=CQ[g].rearrange("p (a b) -> p a b", a=4)),
                      writes=[Rcqn])
                P.dma("sp", lambda e, g=g: e.dma_start(out=ymix[:, 0:8, :], in_=YP[g].rearrange("p (a b) -> p a b", a=8)),
                      writes=[Rypool])
                pro_some(8)
                nkt = ntt * (g + 1)
                def prep(j, t0=t0):
                    QT, RQT = QTs[2 * (j % 2):2 * (j % 2) + 2], RQTs[2 * (j % 2):2 * (j % 2) + 2]
                    QRT, RQRT = QRTs[j % 2], RQRTs[j % 2]
                    qk = qcnt[0] % 2
                    qcnt[0] += 1
                    pqr = psq[qk][:, 0:TG]
                    for fc in range(4):
                        P.op("pe", lambda e, pqr=pqr, fc=fc, j=j: e.matmul(
                            out=pqr, lhsT=wuq_sb[:, fc, 1024 + j * 128:1024 + (j + 1) * 128], rhs=cqn[:, fc, :],
                            start=(fc == 0), stop=(fc == 3)), reads=[R_w, Rcqn], writes=[Rpsq[qk]])
                    P.op("act", lambda e, pqr=pqr: e.activation(out=qrg[:], in_=pqr, func=AF.Copy, scale=cst_sb[:, 65:66]),
                         reads=[Rpsq[qk], R_const], writes=[Rqrg])
                    P.op("act", lambda e, pqr=pqr: e.activation(out=sqr[:], in_=pqr, func=AF.Square),
                         reads=[Rpsq[qk]], writes=[Rsqr])
                    for hh in range(2):
                        h = 2 * j + hh
                        qk2 = qcnt[0] % 2
                        qcnt[0] += 1
                        pq = psq[qk2][:, 0:TG]
                        for fc in range(4):
                            P.op("pe", lambda e, pq=pq, fc=fc, h=h: e.matmul(
                                out=pq, lhsT=wuq_sb[:, fc, h * 128:(h + 1) * 128], rhs=cqn[:, fc, :],
                                start=(fc == 0), stop=(fc == 3)), reads=[R_w, Rcqn], writes=[Rpsq[qk2]])
                        P.op("act", lambda e, pq=pq, hh=hh: e.activation(out=qtmp[hh][:], in_=pq, func=AF.Copy,
                                                                           scale=cst_sb[:, 64:65]),
                             reads=[Rpsq[qk2], R_const], writes=[Rqtmp[hh]])
                        P.op("act", lambda e, pq=pq, hh=hh: e.activation(out=sqq[hh][:], in_=pq, func=AF.Square),
                             reads=[Rpsq[qk2]], writes=[Rsqq[hh]])
                        P.op("pe", lambda e, pq=pq, hh=hh: e.matmul(out=pq, lhsT=ones_f, rhs=sqq[hh][:], start=True,
                                                                      stop=False),
                             reads=[Rsqq[hh], R_const, Rqtmp[hh]], writes=[Rpsq[qk2]])
                        P.op("pe", lambda e, pq=pq, hh=hh: e.matmul(out=pq, lhsT=(Llo_f if hh == 0 else Lhi_f),
                                                                      rhs=sqr[:], start=False, stop=True),
                             reads=[Rsqr, R_const], writes=[Rpsq[qk2]])
                        P.op("act", lambda e, pq=pq, hh=hh: e.activation(out=rq[hh][:], in_=pq, func=AF.Ln,
                                                                           scale=1.0 / 192, bias=cst_sb[:, 86:87]),
                             reads=[Rpsq[qk2], R_const], writes=[Rrq[hh]])
                        P.op("act", lambda e, hh=hh: e.activation(out=rq[hh][:], in_=rq[hh][:], func=AF.Exp, scale=-0.5),
                             reads=[Rrq[hh]], writes=[Rrq[hh]])
                        P.op("dve", lambda e, hh=hh: e.tensor_tensor(out=QT[hh][:], in0=qtmp[hh][:], in1=rq[hh][:],
                                                                       op=ALU.mult),
                             reads=[Rqtmp[hh], Rrq[hh]], writes=[RQT[hh]])
                    qk3 = qcnt[0] % 2
                    qcnt[0] += 1
                    pr = psq[qk3][:, 0:TG]
                    P.op("pe", lambda e, pr=pr: e.matmul(out=pr, lhsT=perm_f, rhs=qrg[:], start=True, stop=True),
                         reads=[Rqrg, R_const], writes=[Rpsq[qk3]])
                    P.op("dve", lambda e, t0=t0: e.tensor_tensor(out=t1[:], in0=qrg[:], in1=cosT[:, t0:t0 + TG],
                                                                  op=ALU.mult), reads=[Rqrg, R_rope], writes=[Rt1])
                    P.op("dve", lambda e, pr=pr, t0=t0: e.tensor_tensor(out=t2[:], in0=pr, in1=sinT[:, t0:t0 + TG],
                                                                         op=ALU.mult),
                         reads=[Rpsq[qk3], R_rope], writes=[Rt2])
                    P.op("dve", lambda e: e.tensor_tensor(out=t1[:], in0=t1[:], in1=t2[:], op=ALU.add),
                         reads=[Rt1, Rt2], writes=[Rt1])
                    P.op("dve", lambda e: e.tensor_tensor(out=QRT[0:64, :], in0=t1[0:64, :], in1=rq[0][0:64, :],
                                                          op=ALU.mult), reads=[Rt1, Rrq[0]], writes=[RQRT])
                    P.op("dve", lambda e: e.tensor_tensor(out=QRT[64:128, :], in0=t1[64:128, :], in1=rq[1][64:128, :],
                                                          op=ALU.mult), reads=[Rt1, Rrq[1]], writes=[RQRT], indep=True)

                prep(0)
                for j in range(4):
                    QT, RQT = QTs[2 * (j % 2):2 * (j % 2) + 2], RQTs[2 * (j % 2):2 * (j % 2) + 2]
                    QRT, RQRT = QRTs[j % 2], RQRTs[j % 2]
                    pending = []
                    if j + 1 < 4:
                        P.defer_begin()
                        prep(j + 1)
                        pending = P.defer_end()
                    kstep = -(-len(pending) // max(1, 2 * nkt - 1))
                    for hh in range(2):
                        h = 2 * j + hh
                        hp = 64 * hh
                        def emit_S(kt, h=h, hh=hh, hp=hp, QT=QT, QRT=QRT):
                            sk = scnt[0] % 2
                            scnt[0] += 1
                            ps_ = pss[sk][:, 0:TG]
                            gk = kt // ntt
                            P.op("pe", lambda e, ps_=ps_, h=h, kt=kt, hh=hh: e.matmul(
                                out=ps_, lhsT=KT[:, h, kt * 128:(kt + 1) * 128], rhs=QT[hh][:], start=True, stop=False),
                                reads=[R_K[gk], RQT[hh]], writes=[Rpss[sk]])
                            P.op("pe", lambda e, ps_=ps_, kt=kt, hp=hp: e.matmul(
                                out=ps_, lhsT=KRT[hp:hp + 64, kt * 128:(kt + 1) * 128], rhs=QRT[hp:hp + 64, :],
                                start=False, stop=True), reads=[R_K[gk], RQRT], writes=[Rpss[sk]])
                            return sk, ps_

                        nxt = emit_S(0)
                        for kt in range(nkt):
                            sk, ps_ = nxt
                            gk = kt // ntt
                            if kt + 1 < nkt:
                                nxt = emit_S(kt + 1)
                            pk_ = ptc[0] % 3
                            ptc[0] += 1
                            P.op("act", lambda e, ps_=ps_, pk_=pk_, kt=kt, h=h: e.activation(
                                out=pT[pk_][:], in_=ps_, func=AF.Exp, scale=rkcol[:, kt, h:h + 1]),
                                reads=[Rpss[sk], R_K[gk]], writes=[RpT[pk_]])
                            if kt >= ntt * g:
                                jm = kt - ntt * g
                                P.op("dve", lambda e, pk_=pk_, jm=jm: e.tensor_tensor(
                                    out=pT[pk_][:], in0=pT[pk_][:], in1=mask_b[:, jm, :], op=ALU.mult),
                                    reads=[RpT[pk_], R_w], writes=[RpT[pk_]])
                            P.op("pe", lambda e, pk_=pk_, kt=kt, h=h, nkt=nkt: e.matmul(
                                out=pso[:, 0:TG], lhsT=Vsb[:, kt, h * 128:(h + 1) * 128], rhs=pT[pk_][:],
                                start=(kt == 0), stop=(kt == nkt - 1)), reads=[R_V[gk], RpT[pk_]], writes=[Rpso])
                            P.op("pe", lambda e, pk_=pk_, kt=kt, nkt=nkt: e.matmul(
                                out=psd[:, 0:TG], lhsT=ones_bf[:], rhs=pT[pk_][:],
                                start=(kt == 0), stop=(kt == nkt - 1)), reads=[R_const, RpT[pk_]], writes=[Rpsd])
                            for _ in range(kstep):
                                if pending:
                                    pending.pop(0)()
                        P.op("dve", lambda e: e.reciprocal(out=rec[:], in_=psd[:, 0:TG]), reads=[Rpsd], writes=[Rrec])
                        P.op("dve", lambda e, h=h: e.tensor_tensor(out=ymix[:, 8 + h, :], in0=pso[:, 0:TG], in1=rec[:],
                                                                    op=ALU.mult),
                             reads=[Rpso, Rrec], writes=[Rymla])
                    while pending:
                        pending.pop(0)()
                for nb in range(4):
                    wk = wcnt[0] % 3
                    wcnt[0] += 1
                    P.dma("sp", lambda e, wk=wk, nb=nb: e.dma_start(
                        out=wob[wk][:], in_=Woutb[nb].rearrange("p (a b) -> p a b", a=16)),
                        reads=[RWout[nb]], writes=[Rwob[wk]])
                    for tt in range(ntt):
                        hk = hcnt[0] % 3
                        hk2 = hcnt[0] % 2
                        hcnt[0] += 1
                        row0 = t0 + tt * 128
                        P.dma("sp", lambda e, hk=hk, row0=row0, nb=nb: e.dma_start(
                            out=xb[hk][:], in_=x[row0:row0 + 128, nb * 512:(nb + 1) * 512]), writes=[Rxb[hk]])
                        for fc in range(16):
                            P.op("pe", lambda e, hk2=hk2, fc=fc, tt=tt, wk=wk: e.matmul(
                                out=psh[hk2][:], lhsT=ymix[:, fc, tt * 128:(tt + 1) * 128], rhs=wob[wk][:, fc, :],
                                start=(fc == 0), stop=(fc == 15)),
                                reads=[Rypool, Rymla, Rwob[wk]], writes=[Rpsh[hk2]])
                        P.op("dve", lambda e, hk=hk, hk2=hk2: e.tensor_tensor(out=hb[hk][:], in0=psh[hk2][:],
                                                                                in1=xb[hk][:], op=ALU.add),
                             reads=[Rpsh[hk2], Rxb[hk]], writes=[Rhb[hk]])
                        P.dma("sp", lambda e, hk=hk, row0=row0, nb=nb: e.dma_start(
                            out=H1[row0:row0 + 128, nb * 512:(nb + 1) * 512], in_=hb[hk][:]), reads=[Rhb[hk]])
            P.emit()
        stKV.close()
        if stop_after == "M2":
            return nc, None, st0

        ntt = TG // 128
        IRv = IRs.rearrange("k p t -> p k t")
        with ExitStack() as st:
            subk_sb = sbuf(st, "subk_sb", [128, 16, 128], F32)
            iota16 = sbuf(st, "iota16", [128, 16], BF16)
            R_w = P.res("f1w")
            P.dma("sp", lambda e: e.dma_start(out=subk_sb[:], in_=subk.rearrange("p (a b) -> p a b", a=16)), writes=[R_w])
            P.op("dve", lambda e: e.tensor_copy(out=iota16[:], in_=iota_f[:, 0:16]), reads=[R_const], writes=[R_w])
            ht = [sbuf(st, "f1_ht%d" % i, [128, D], F32) for i in range(2)]
            Rht = [P.res("f1_ht%d" % i) for i in range(2)]
            rbs = [rms_bufs(st, "f1_%d" % i) for i in range(2)]
            xn2 = sbuf(st, "xn2", [128, 16, TG], BF16)
            Rxn2 = P.res("xn2")
            wbuf = [sbuf(st, "f1_wb%d" % i, [128, 16, 128], BF16) for i in range(4)]
            Rwb = [P.res("f1_wb%d" % i) for i in range(4)]
            psq = [psum(st, "f1_psq%d" % i, [128, 512], F32) for i in range(2)]
            Rpsq = [P.res("f1_psq%d" % i) for i in range(2)]
            pssc = [psum(st, "f1_pssc%d" % i, [128, 512], F32) for i in range(2)]
            Rpssc = [P.res("f1_pssc%d" % i) for i in range(2)]
            qpTs = [sbuf(st, "qpT%d" % i, [128, 16, TG], F32) for i in range(2)]
            RqpTs = [P.res("qpT%d" % i) for i in range(2)]
            def f1_bufset(u):
                u_ = "_%d" % u
                sc = sbuf(st, u_[1:] + "sc", [128, 16, 128], F32)
                sc2 = sbuf(st, u_[1:] + "sc2", [128, 16, 128], F32)
                Rsc = [P.res(u_[1:] + "sc%d" % i) for i in range(16)]
                Rsc2 = [P.res(u_[1:] + "sc2_%d" % i) for i in range(16)]
                v16 = sbuf(st, u_[1:] + "v16", [128, 16, 16], F32)
                i16 = sbuf(st, u_[1:] + "i16", [128, 16, 16], U32)
                i16f = sbuf(st, u_[1:] + "i16f", [128, 16, 16], BF16)
                Rv16 = [P.res(u_[1:] + "v16_%d" % i) for i in range(16)]
                Ri16 = [P.res(u_[1:] + "i16_%d" % i) for i in range(16)]
                Ri16f = P.res(u_[1:] + "i16f")
                cand = sbuf(st, u_[1:] + "cand", [128, 8, 256], F32)
                cand2 = sbuf(st, u_[1:] + "cand2", [128, 8, 256], F32)
                Rcand = [P.res(u_[1:] + "cand%d" % i) for i in range(8)]
                Rcand2 = [P.res(u_[1:] + "cand2_%d" % i) for i in range(8)]
                vs = sbuf(st, u_[1:] + "vs", [128, 8, 16], F32)
                ci = sbuf(st, u_[1:] + "ci", [128, 8, 16], U32)
                Rvs = [P.res(u_[1:] + "vs%d" % i) for i in range(8)]
                Rci = [P.res(u_[1:] + "ci%d" % i) for i in range(8)]
                abi = sbuf(st, u_[1:] + "abi", [128, 2, 128], U32)
                abf = sbuf(st, u_[1:] + "abf", [128, 2, 128], BF16)
                Rabi, Rabf = P.res(u_[1:] + "abi"), P.res(u_[1:] + "abf")
                eqb = [sbuf(st, u_[1:] + "eqb%d" % i, [128, 8, 16, 16], BF16) for i in range(2)]
                Reqb = [P.res(u_[1:] + "eqb%d" % i) for i in range(2)]
                gsm = sbuf(st, u_[1:] + "gsm", [128, 2, 8], F32)
                Rgsm = P.res(u_[1:] + "gsm")
                return dict(sc=sc, sc2=sc2, Rsc=Rsc, Rsc2=Rsc2, v16=v16, i16=i16, i16f=i16f, Rv16=Rv16, Ri16=Ri16, Ri16f=Ri16f, cand=cand, cand2=cand2, Rcand=Rcand, Rcand2=Rcand2, vs=vs, ci=ci, Rvs=Rvs, Rci=Rci, abi=abi, abf=abf, Rabi=Rabi, Rabf=Rabf, eqb=eqb, Reqb=Reqb, gsm=gsm, Rgsm=Rgsm)

            bsets = [f1_bufset(0), f1_bufset(1)]
            tris = [sbuf(st, "tri%d" % i, [128, 3, 128], F32) for i in range(4)]
            Rtris = [P.res("tri%d" % i) for i in range(4)]
            pstr = pssc[1]
            Rpstr = Rpssc[1]
            trT = sbuf(st, "trT", [128, 3, 128], F32)
            RtrT = P.res("trT")
            wcnt = [0]
            qcnt = [0]
            tcnt = [0]

            def top16(src, Rsrc, src2, Rsrc2, vout, Rvout, iout, Riout, n):
                for k in range(n):
                    P.op("dve", lambda e, k=k: e.max(out=vout(k)[:, 0:8], in_=src(k)), reads=[Rsrc[k]], writes=[Rvout[k]])
                for k in range(n):
                    P.op("dve", lambda e, k=k: e.max_index(out=iout(k)[:, 0:8], in_max=vout(k)[:, 0:8], in_values=src(k)),
                         reads=[Rsrc[k], Rvout[k]], writes=[Riout[k]])
                for k in range(n):
                    P.op("dve", lambda e, k=k: e.match_replace(out=src2(k), in_to_replace=vout(k)[:, 0:8],
                                                               in_values=src(k), imm_value=-1e30),
                         reads=[Rsrc[k], Rvout[k]], writes=[Rsrc2[k]])
                for k in range(n):
                    P.op("dve", lambda e, k=k: e.max(out=vout(k)[:, 8:16], in_=src2(k)), reads=[Rsrc2[k]],
                         writes=[Rvout[k]])
                for k in range(n):
                    P.op("dve", lambda e, k=k: e.max_index(out=iout(k)[:, 8:16], in_max=vout(k)[:, 8:16],
                                                           in_values=src2(k)),
                         reads=[Rsrc2[k], Rvout[k]], writes=[Riout[k]])

            def f1_front(g):
                t0 = g * TG
                qpT, RqpT = qpTs[g % 2], RqpTs[g % 2]
                items = []
                for tt in range(ntt):
                    k = tt % 2
                    row0 = t0 + tt * 128
                    P.dma("sp", lambda e, k=k, row0=row0: e.dma_start(out=ht[k][:], in_=H1[row0:row0 + 128, :]),
                          writes=[Rht[k]])
                    items.append((ht[k][:], Rht[k], (lambda dc, tt=tt: xn2[:, dc, tt * 128:(tt + 1) * 128]), Rxn2, rbs[k]))
                rmsnorm_multi(items, 16, all_act=True)
                P.dma("sp", lambda e, g=g: e.dma_start(out=XN2[g].rearrange("p (a b) -> p a b", a=16), in_=xn2[:]),
                      reads=[Rxn2])
                pro_some(40)
                for c in range(16):
                    wk = wcnt[0] % 4
                    wcnt[0] += 1
                    P.dma("sp", lambda e, wk=wk, c=c: e.dma_start(
                        out=wbuf[wk][:], in_=Wpqb[c].rearrange("p (a b) -> p a b", a=16)),
                        reads=[RWpq[c]], writes=[Rwb[wk]])
                    qk = qcnt[0] % 2
                    qcnt[0] += 1
                    pq = psq[qk][:, 0:TG]
                    for dc in range(16):
                        P.op("pe", lambda e, wk=wk, dc=dc, pq=pq: e.matmul(
                            out=pq, lhsT=wbuf[wk][:, dc, :], rhs=xn2[:, dc, :], start=(dc == 0), stop=(dc == 15)),
                            reads=[Rwb[wk], Rxn2], writes=[Rpsq[qk]])
                    P.op("act", lambda e, pq=pq, c=c: e.activation(out=qpT[:, c, :], in_=pq, func=AF.Copy),
                         reads=[Rpsq[qk]], writes=[RqpT], indep=True)

            def f1_main(g, tt):
                t0 = g * TG
                qpT, RqpT = qpTs[g % 2], RqpTs[g % 2]
                tri, Rtri = tris[2 * (g % 2) + tt], Rtris[2 * (g % 2) + tt]
                B_ = bsets[tt]
                sc, sc2, Rsc, Rsc2 = B_["sc"], B_["sc2"], B_["Rsc"], B_["Rsc2"]
                v16, i16, i16f, Rv16, Ri16, Ri16f = B_["v16"], B_["i16"], B_["i16f"], B_["Rv16"], B_["Ri16"], B_["Ri16f"]
                cand, cand2, Rcand, Rcand2 = B_["cand"], B_["cand2"], B_["Rcand"], B_["Rcand2"]
                vs, ci, Rvs, Rci = B_["vs"], B_["ci"], B_["Rvs"], B_["Rci"]
                abi, abf, Rabi, Rabf, eqb, Reqb = B_["abi"], B_["abf"], B_["Rabi"], B_["Rabf"], B_["eqb"], B_["Reqb"]
                if True:
                    for c in range(16):
                        bq = tt % 2
                        P.op("pe", lambda e, c=c, tt=tt, bq=bq: e.matmul(
                            out=pssc[bq][:, (c % 4) * 128:(c % 4 + 1) * 128],
                            lhsT=qpT[:, c, tt * 128:(tt + 1) * 128], rhs=subk_sb[:, c, :], start=True, stop=True),
                            reads=[RqpT, R_w], writes=[Rpssc[bq]])
                        if c % 4 == 3:
                            b4 = c // 4
                            P.op("dve", lambda e, b4=b4, bq=bq: e.tensor_copy(
                                out=sc[:, 4 * b4:4 * b4 + 4, :], in_=pssc[bq][:].rearrange("p (a b) -> p a b", a=4)),
                                reads=[Rpssc[bq]], writes=Rsc[4 * b4:4 * b4 + 4])
                    top16(lambda k: sc[:, k, :], Rsc, lambda k: sc2[:, k, :], Rsc2,
                          lambda k: v16[:, k, :], Rv16, lambda k: i16[:, k, :], Ri16, 16)
                    P.op("dve", lambda e: e.tensor_copy(out=i16f[:], in_=i16[:]), reads=Ri16, writes=[Ri16f])
                    v16v = v16[:].rearrange("p (h s) a -> p h s a", s=2)
                    i16v = i16f[:].rearrange("p (h s) a -> p h s a", s=2)
                    P.op("dve", lambda e, v16v=v16v: e.tensor_tensor(
                        out=cand[:].rearrange("p h (a b) -> p h a b", a=16),
                        in0=v16v[:, :, 0, :].unsqueeze(3).to_broadcast([128, 8, 16, 16]),
                        in1=v16v[:, :, 1, :].unsqueeze(2).to_broadcast([128, 8, 16, 16]), op=ALU.add),
                        reads=Rv16, writes=Rcand)
                    top16(lambda k: cand[:, k, :], Rcand, lambda k: cand2[:, k, :], Rcand2,
                          lambda k: vs[:, k, :], Rvs, lambda k: ci[:, k, :], Rci, 8)
                    civ = ci[:].rearrange("p h r -> p (h r)")
                    P.op("dve", lambda e, civ=civ: e.tensor_single_scalar(out=abi[:, 0, :], in_=civ, scalar=4,
                                                                          op=ALU.logical_shift_right),
                         reads=Rci, writes=[Rabi])
                    P.op("dve", lambda e, civ=civ: e.tensor_single_scalar(out=abi[:, 1, :], in_=civ, scalar=15,
                                                                          op=ALU.bitwise_and),
                         reads=Rci, writes=[Rabi], indep=True)
                    P.op("dve", lambda e: e.tensor_copy(out=abf[:], in_=abi[:]), reads=[Rabi], writes=[Rabf])
                    for s_ in range(2):
                        eb = eqb[s_]
                        P.op("dve", lambda e, eb=eb, s_=s_: e.tensor_tensor(
                            out=eb[:],
                            in0=iota16[:].unsqueeze(1).unsqueeze(1).to_broadcast([128, 8, 16, 16]),
                            in1=abf[:, s_, :].rearrange("p (h r) -> p h r", h=8).unsqueeze(3).to_broadcast([128, 8, 16, 16]),
                            op=ALU.is_equal), reads=[Rabf, R_w], writes=[Reqb[s_]])
                        P.op("dve", lambda e, eb=eb, s_=s_, i16v=i16v: e.tensor_tensor(
                            out=eb[:], in0=eb[:],
                            in1=i16v[:, :, s_, :].unsqueeze(2).to_broadcast([128, 8, 16, 16]), op=ALU.mult),
                            reads=[Reqb[s_], Ri16f], writes=[Reqb[s_]])
                        P.op("dve", lambda e, eb=eb, s_=s_: e.tensor_reduce(
                            out=tri[:, s_, :].rearrange("p (h r) -> p h r", h=8), in_=eb[:], axis=AX.X, op=ALU.add),
                            reads=[Reqb[s_]], writes=[Rtri])
                    gv = tri[:, 2, :].rearrange("p (h r) -> p h r", h=8)
                    P.op("dve", lambda e, gv=gv: e.tensor_tensor(
                        out=gv, in0=vs[:], in1=vs[:, :, 0:1].to_broadcast([128, 8, 16]), op=ALU.subtract),
                        reads=Rvs, writes=[Rtri])

            def f1_tail(g, tt):
                t0 = g * TG
                tri, Rtri = tris[2 * (g % 2) + tt], Rtris[2 * (g % 2) + tt]
                gsm, Rgsm = bsets[tt]["gsm"], bsets[tt]["Rgsm"]
                if True:
                    gv = tri[:, 2, :].rearrange("p (h r) -> p h r", h=8)
                    P.op("act", lambda e, gv=gv: e.activation(out=gv, in_=gv, func=AF.Exp), reads=[Rtri], writes=[Rtri])
                    P.op("dve", lambda e, gv=gv: e.tensor_reduce(out=gsm[:, 0, :], in_=gv, axis=AX.X, op=ALU.add),
                         reads=[Rtri], writes=[Rgsm])
                    P.op("dve", lambda e: e.reciprocal(out=gsm[:, 1, :], in_=gsm[:, 0, :]), reads=[Rgsm], writes=[Rgsm])
                    P.op("dve", lambda e, gv=gv: e.tensor_tensor(
                        out=gv, in0=gv, in1=gsm[:, 1, :].unsqueeze(2).to_broadcast([128, 8, 16]), op=ALU.mult),
                        reads=[Rtri, Rgsm], writes=[Rtri])
                    for q_ in range(3):
                        P.op("pe", lambda e, q_=q_: e.transpose(out=pstr[:, q_ * 128:(q_ + 1) * 128], in_=tri[:, q_, :],
                                                                identity=ident_f), reads=[Rtri, R_const], writes=[Rpstr])
                    P.op("act", lambda e: e.activation(out=trT[:], in_=pstr[:, 0:384].rearrange("p (a b) -> p a b", a=3),
                                                       func=AF.Copy), reads=[Rpstr], writes=[RtrT])
                    row0 = t0 + tt * 128
                    P.dma("sp", lambda e, row0=row0: e.dma_start(out=IRv[:, :, row0:row0 + 128], in_=trT[:]),
                          reads=[RtrT])

            f1_front(0)
            if NG > 1:
                f1_front(1)
            for g in range(NG):
                lists = []
                for tt in range(ntt):
                    P.defer_begin()
                    f1_main(g, tt)
                    lists.append(P.defer_end())
                for k_ in range(max(len(l_) for l_ in lists)):
                    for l_ in lists:
                        if k_ < len(l_):
                            l_[k_]()
                if g + 2 < NG:
                    f1_front(g + 2)
                if g >= 1:
                    for tt in range(ntt):
                        f1_tail(g - 1, tt)
            for tt in range(ntt):
                f1_tail(NG - 1, tt)
            P.emit()
        if stop_after == "F1":
            return nc, None, st0

        with ExitStack() as st:
            GT = sbuf(st, "GT", [128, TG, 128], BF16)
            RGT = [P.res("GT%d" % i) for i in range(128)]
            xn2 = sbuf(st, "f2_xn2", [128, 16, TG], BF16)
            Rxn2 = P.res("f2_xn2")
            trg = sbuf(st, "trg", [128, 3, TG], F32)
            Rtrg = P.res("trg")
            NSUB = 32
            Pb = [sbuf(st, "Pb%d" % i, [128, NSUB, 128], BF16) for i in range(2)]
            Qb = [sbuf(st, "Qb%d" % i, [128, NSUB, 128], BF16) for i in range(2)]
            RPb = [P.res("Pb%d" % i) for i in range(2)]
            RQb = [P.res("Qb%d" % i) for i in range(2)]
            psg = [psum(st, "psg%d" % i, [128, 512], F32) for i in range(2)]
            Rpsg = [P.res("psg%d" % i) for i in range(2)]
            pss = [psum(st, "f2_pss%d" % i, [128, 512], F32) for i in range(2)]
            Rpss = [P.res("f2_pss%d" % i) for i in range(2)]
            pso = [psum(st, "f2_pso%d" % i, [128, 512], F32) for i in range(4)]
            Rpso = [P.res("f2_pso%d" % i) for i in range(4)]
            ub = [sbuf(st, "ub%d" % i, [128, 16, 128], BF16) for i in range(10)]
            Rub = [P.res("ub%d" % i) for i in range(10)]
            vb = [sbuf(st, "vb%d" % i, [128, 2, 1024], BF16) for i in range(6)]
            Rvb = [P.res("vb%d" % i) for i in range(6)]
            gl = [sbuf(st, "gl%d" % i, [128, TG], F32) for i in range(3)]
            Rgl = [P.res("gl%d" % i) for i in range(3)]
            hb = [sbuf(st, "f2_hb%d" % i, [128, 512], F32) for i in range(3)]
            Rhb = [P.res("f2_hb%d" % i) for i in range(3)]
            ob = [sbuf(st, "f2_ob%d" % i, [128, 512], F32) for i in range(3)]
            Rob = [P.res("f2_ob%d" % i) for i in range(3)]
            gcnt = [0]
            scnt = [0]
            ucnt = [0]
            vcnt = [0]
            glc = [0]
            ocnt = [0]
            hcnt = [0]
            sbc = [0]
            for g in range(NG):
                t0 = g * TG
                P.dma("sp", lambda e, g=g: e.dma_start(out=xn2[:], in_=XN2[g].rearrange("p (a b) -> p a b", a=16)),
                      writes=[Rxn2])
                P.dma("sp", lambda e, t0=t0: e.dma_start(out=trg[:], in_=IRv[:, :, t0:t0 + TG]), writes=[Rtrg])
                for sub in range(TG // NSUB):
                    bk = sbc[0] % 2
                    sbc[0] += 1
                    for tl in range(NSUB):
                        t = sub * NSUB + tl
                        P.op("dve", lambda e, bk=bk, tl=tl, t=t: e.tensor_scalar(
                            out=Pb[bk][:, tl, :], in0=iota_bf[:], scalar1=trg[:, 0, t:t + 1], scalar2=trg[:, 2, t:t + 1],
                            op0=ALU.is_equal, op1=ALU.mult), reads=[Rtrg, R_const], writes=[RPb[bk]], indep=True)
                        P.op("dve", lambda e, bk=bk, tl=tl, t=t: e.tensor_scalar(
                            out=Qb[bk][:, tl, :], in0=iota_bf[:], scalar1=trg[:, 1, t:t + 1], scalar2=None,
                            op0=ALU.is_equal), reads=[Rtrg, R_const], writes=[RQb[bk]], indep=True)
                    for q4 in range(NSUB // 4):
                        gk = gcnt[0] % 2
                        gcnt[0] += 1
                        tok0 = sub * NSUB + q4 * 4
                        for u in range(4):
                            tl = q4 * 4 + u
                            P.op("pe", lambda e, gk=gk, u=u, bk=bk, tl=tl: e.matmul(
                                out=psg[gk][:, u * 128:(u + 1) * 128], lhsT=Qb[bk][:, tl, :], rhs=Pb[bk][:, tl, :],
                                start=True, stop=True), reads=[RPb[bk], RQb[bk]], writes=[Rpsg[gk]])
                        P.op("act", lambda e, gk=gk, tok0=tok0: e.activation(
                            out=GT[:, tok0:tok0 + 4, :],
                            in_=psg[gk][:].rearrange("p (t i) -> p t i", t=4), func=AF.Copy),
                            reads=[Rpsg[gk]], writes=RGT, indep=True)
                for i in range(128):
                    uk = ucnt[0] % 10
                    ucnt[0] += 1
                    P.dma("sp", lambda e, uk=uk, i=i: e.dma_start(out=ub[uk][:], in_=UTb[i].rearrange("p (a b) -> p a b", a=16)),
                          writes=[Rub[uk]])
                    sk = scnt[0] % 2
                    scnt[0] += 1
                    ps_ = pss[sk][:, 0:TG]
                    for dc in range(16):
                        P.op("pe", lambda e, ps_=ps_, uk=uk, dc=dc: e.matmul(
                            out=ps_, lhsT=ub[uk][:, dc, :], rhs=xn2[:, dc, :], start=(dc == 0), stop=(dc == 15)),
                            reads=[Rub[uk], Rxn2], writes=[Rpss[sk]])
                    lk = glc[0] % 3
                    glc[0] += 1
                    P.op("act", lambda e, ps_=ps_, lk=lk: e.activation(out=gl[lk][:], in_=ps_, func=AF.Gelu_apprx_tanh),
                         reads=[Rpss[sk]], writes=[Rgl[lk]])
                    P.op("dve", lambda e, lk=lk, i=i: e.tensor_tensor(out=GT[:, :, i], in0=gl[lk][:], in1=GT[:, :, i],
                                                                        op=ALU.mult),
                         reads=[Rgl[lk], RGT[i]], writes=[RGT[i]])
                for nbp in range(2):
                    for i2 in range(64):
                        vk = vcnt[0] % 6
                        vcnt[0] += 1
                        r0 = nbp * 128 + 2 * i2
                        P.dma("sp", lambda e, vk=vk, r0=r0: e.dma_start(
                            out=vb[vk][:], in_=EVb[r0:r0 + 2].rearrange("i e n -> e i n")), writes=[Rvb[vk]])
                        for ii in range(2):
                            i = 2 * i2 + ii
                            for nbl in range(2):
                                for tt in range(ntt):
                                    pk = 2 * nbl + tt
                                    P.op("pe", lambda e, pk=pk, tt=tt, i=i, ii=ii, vk=vk, nbl=nbl: e.matmul(
                                        out=pso[pk][:], lhsT=GT[:, tt * 128:(tt + 1) * 128, i],
                                        rhs=vb[vk][:, ii, nbl * 512:(nbl + 1) * 512],
                                        start=(i == 0), stop=(i == 127)), reads=[RGT[i], Rvb[vk]], writes=[Rpso[pk]])
                    for nbl in range(2):
                        nb = 2 * nbp + nbl
                        for tt in range(ntt):
                            pk = 2 * nbl + tt
                            hk = hcnt[0] % 3
                            hcnt[0] += 1
                            row0 = t0 + tt * 128
                            P.dma("sp", lambda e, hk=hk, row0=row0, nb=nb: e.dma_start(
                                out=hb[hk][:], in_=H1[row0:row0 + 128, nb * 512:(nb + 1) * 512]), writes=[Rhb[hk]])
                            P.op("dve", lambda e, hk=hk, pk=pk: e.tensor_tensor(
                                out=ob[hk][:], in0=pso[pk][:], in1=hb[hk][:], op=ALU.add),
                                reads=[Rpso[pk], Rhb[hk]], writes=[Rob[hk]])
                            P.dma("sp", lambda e, hk=hk, row0=row0, nb=nb: e.dma_start(
                                out=H2[row0:row0 + 128, nb * 512:(nb + 1) * 512], in_=ob[hk][:]), reads=[Rob[hk]])
            P.emit()
        if stop_after == "F2":
            return nc, None, st0

        with ExitStack() as st:
            NTILE = T // 128
            wpp_sb = sbuf(st, "wpp_sb", [128, 2, 2048], BF16)
            R_w = P.res("gw")
            P.dma("pool", lambda e: e.dma_start(out=wpp_sb[:], in_=Wpp.rearrange("p (a b) -> p a b", a=2),
                                                max_dma_last_dim=4096), writes=[R_w])
            ht = [sbuf(st, "g_ht%d" % i, [128, D], F32) for i in range(2)]
            Rht = [P.res("g_ht%d" % i) for i in range(2)]
            rbs = [rms_bufs(st, "g%d" % i) for i in range(2)]
            xn3 = sbuf(st, "xn3", [128, 16, T], BF16)
            Rxn3 = [P.res("xn3_%d" % i) for i in range(NTILE)]
            pt_f = sbuf(st, "pt_f", [128, 256], F32)
            pt_b = sbuf(st, "pt_b", [128, 256], BF16)
            Rptf, Rptb = P.res("pt_f"), P.res("pt_b")
            pTb = sbuf(st, "pTb", [128, 2, T], BF16)
            RpTb = [P.res("pTb%d" % i) for i in range(NTILE)]
            pstp = psum(st, "g_pstp", [128, 1024], BF16)
            Rpstp = P.res("g_pstp")
            wgb = [sbuf(st, "wgb%d" % i, [128, 16, 512], BF16) for i in range(3)]
            Rwgb = [P.res("wgb%d" % i) for i in range(3)]
            psgt = [psum(st, "g_psg%d" % i, [128, 512], F32) for i in range(2)]
            Rpsgt = [P.res("g_psg%d" % i) for i in range(2)]
            pspp = psum(st, "g_psp", [128, 512], F32)
            Rpspp = P.res("g_psp")
            sg = [sbuf(st, "sg%d" % i, [128, 512], F32) for i in range(2)]
            Rsg = [P.res("sg%d" % i) for i in range(2)]
            hb = [sbuf(st, "g_hb%d" % i, [128, 512], F32) for i in range(3)]
            Rhb = [P.res("g_hb%d" % i) for i in range(3)]
            ob = [sbuf(st, "g_ob%d" % i, [128, 512], F32) for i in range(3)]
            Rob = [P.res("g_ob%d" % i) for i in range(3)]
            kcnt = [0]
            ocnt = [0]

            def g_front2(t_first):
                items = []
                for t in (t_first, t_first + 1):
                    k = t % 2
                    row0 = t * 128
                    P.dma("sp", lambda e, k=k, row0=row0: e.dma_start(out=ht[k][:], in_=H2[row0:row0 + 128, :]),
                          writes=[Rht[k]])
                    items.append((ht[k][:], Rht[k], (lambda dc, row0=row0: xn3[:, dc, row0:row0 + 128]), Rxn3[t], rbs[k]))
                rmsnorm_multi(items, 32)
                for t in (t_first, t_first + 1):
                    row0 = t * 128
                    P.dma("sp", lambda e, row0=row0: e.dma_start(out=pt_f[:], in_=pin[row0:row0 + 128, :]), writes=[Rptf])
                    P.op("act", lambda e: e.activation(out=pt_b[:], in_=pt_f[:], func=AF.Copy), reads=[Rptf], writes=[Rptb])
                    for fc in range(2):
                        P.op("pe", lambda e, fc=fc: e.transpose(out=pstp[:, fc * 128:(fc + 1) * 128],
                                                                in_=pt_b[:, fc * 128:(fc + 1) * 128], identity=ident_bf[:]),
                             reads=[Rptb, R_const], writes=[Rpstp])
                    P.op("dve", lambda e, row0=row0: e.tensor_copy(
                        out=pTb[:, :, row0:row0 + 128], in_=pstp[:, 0:256].rearrange("p (a b) -> p a b", a=2)),
                        reads=[Rpstp], writes=[RpTb[t]])

            def g_back(t, nb, wk):
                row0 = t * 128
                kk = kcnt[0] % 2
                kcnt[0] += 1
                okk = ocnt[0] % 3
                ocnt[0] += 1
                P.dma("sp", lambda e: e.dma_start(out=hb[okk][:], in_=H2[row0:row0 + 128, nb * 512:(nb + 1) * 512]),
                      writes=[Rhb[okk]])
                for fc in range(16):
                    P.op("pe", lambda e, fc=fc: e.matmul(
                        out=psgt[kk][:], lhsT=xn3[:, fc, row0:row0 + 128], rhs=wgb[wk][:, fc, :],
                        start=(fc == 0), stop=(fc == 15)), reads=[Rxn3[t], Rwgb[wk]], writes=[Rpsgt[kk]])
                for fc in range(2):
                    P.op("pe", lambda e, fc=fc: e.matmul(
                        out=pspp[:], lhsT=pTb[:, fc, row0:row0 + 128], rhs=wpp_sb[:, fc, nb * 512:(nb + 1) * 512],
                        start=(fc == 0), stop=(fc == 1)), reads=[RpTb[t], R_w], writes=[Rpspp])
                P.op("act", lambda e: e.activation(out=sg[kk][:], in_=psgt[kk][:], func=AF.Sigmoid),
                     reads=[Rpsgt[kk]], writes=[Rsg[kk]])
                P.op("dve", lambda e: e.tensor_tensor(out=sg[kk][:], in0=sg[kk][:], in1=pspp[:], op=ALU.mult),
                     reads=[Rsg[kk], Rpspp], writes=[Rsg[kk]])
                P.op("dve", lambda e: e.tensor_tensor(out=ob[okk][:], in0=sg[kk][:], in1=hb[okk][:], op=ALU.add),
                     reads=[Rsg[kk], Rhb[okk]], writes=[Rob[okk]])
                P.dma("sp", lambda e: e.dma_start(out=out[row0:row0 + 128, nb * 512:(nb + 1) * 512], in_=ob[okk][:]),
                      reads=[Rob[okk]])

            def load_w(nb):
                wk = nb % 3
                P.dma("sp", lambda e: e.dma_start(out=wgb[wk][:], in_=Wgb[nb].rearrange("p (a b) -> p a b", a=16)),
                      reads=[RWg[nb]], writes=[Rwgb[wk]])

            load_w(0)
            g_front2(0)
            load_w(1)
            load_w(2)
            for t in range(0, NTILE, 2):
                if t + 2 < NTILE:
                    g_front2(t + 2)
                g_back(t, 0, 0)
                g_back(t + 1, 0, 0)
            for nb in range(1, 4):
                if nb + 2 < 4:
                    load_w(nb + 2)
                for t in range(NTILE):
                    g_back(t, nb, nb % 3)
            P.emit()
        return nc, None, st0


def _host_layout(inp, b):
    f = np.float32
    d = {}
    d["x"] = np.ascontiguousarray(inp["x"][b], f)
    d["p"] = np.ascontiguousarray(inp["p"][0, b], f)
    d["pos"] = np.ascontiguousarray(inp["positions"][b].reshape(1, T).astype(np.int32))
    return d


_SHARED = {}


def _shared_layout(inp):
    f = np.float32
    s = {}
    cst = np.zeros((128, NCST), f)

    def colmajor(v, n):
        return np.asarray(v, f).reshape(n, 128).T

    cst[:, 0:16] = colmajor(inp["mix_norm_gain"][0], 16)
    cst[:, 16:32] = colmajor(inp["ffn_norm_gain"][0], 16)
    cst[:, 32:48] = colmajor(inp["ple_norm_gain"][0], 16)
    cst[:, 48:52] = colmajor(inp["q_lat_gain"][0], 4)
    cst[:, 52:56] = colmajor(inp["kv_lat_gain"][0], 4)
    cst[:, 56:64] = colmajor(inp["pool_scale"][0], 8)
    qg = np.asarray(inp["q_norm_gain"][0], f)
    kg = np.asarray(inp["k_norm_gain"][0], f)
    cst[:, 64] = qg[0:128]
    cst[:, 65] = np.tile(qg[128:192], 2)
    cst[:, 66] = kg[0:128]
    cst[:, 67] = np.tile(kg[128:192], 2)
    inv_freq = (np.float32(10000.0) ** (-np.arange(0, 64, 2, dtype=np.float32) / np.float32(64))).astype(f)
    cst[:, 68] = np.tile(inv_freq, 4)
    cst[:, 69] = np.tile(np.concatenate([-np.ones(32, f), np.ones(32, f)]), 2)
    cst[:, 70:86] = (1.0 / np.arange(1, 17, dtype=f))[None, :]
    cst[:, 86] = EPS
    s["cst"] = cst
    mats = np.zeros((128, 6, 128), f)
    mats[:, 0, :] = np.eye(128, dtype=f)
    mats[:, 1, :] = 1.0
    mats[0:64, 2, :] = 1.0
    mats[64:128, 3, :] = 1.0
    for m in range(128):
        partner = m + 32 if (m % 64) < 32 else m - 32
        mats[partner, 4, m] = 1.0
    mats[:, 5, :] = np.arange(128, dtype=f)[None, :]
    s["mats"] = mats.reshape(128, 768)
    ntt = TG // 128
    mk = np.zeros((128, ntt, TG), f)
    kk = np.arange(128)[:, None]
    qq = np.arange(TG)[None, :]
    for j in range(ntt):
        mk[:, j, :] = ((qq // 64) >= ((128 * j + kk) // 64)).astype(f)
    s["maskc"] = mk.reshape(128, ntt * TG)
    w_in = np.asarray(inp["w_in"][0], f)
    w_ext = np.concatenate([w_in, w_in[:, 2048:2112]], axis=1)
    s["Win"] = np.ascontiguousarray(w_ext.reshape(16, 128, 17, 128).transpose(2, 1, 0, 3)).reshape(17, 128, 2048)
    wp = np.asarray(inp["w_pool"][0], f)
    s["Wpool"] = np.ascontiguousarray(wp.reshape(4, 2, 128, 256).transpose(2, 0, 1, 3)).reshape(128, 2048)
    wuq = np.asarray(inp["w_uq"][0], f).reshape(512, 8, 192)
    wuq_r = np.concatenate([wuq[:, :, 0:128].reshape(512, 1024), wuq[:, :, 128:192].reshape(512, 512)], axis=1)
    s["Wuq"] = np.ascontiguousarray(wuq_r.reshape(4, 128, 1536).transpose(1, 0, 2)).reshape(128, 4 * 1536)
    wukv = np.asarray(inp["w_ukv"][0], f).reshape(512, 8, 256)
    wuk = wukv[:, :, 0:128].reshape(512, 1024)
    wv = wukv[:, :, 128:256].reshape(512, 1024)
    s["Wuk"] = np.ascontiguousarray(wuk.reshape(4, 128, 1024).transpose(1, 0, 2)).reshape(128, 4096)
    s["Wv"] = np.ascontiguousarray(wv.reshape(4, 128, 1024).transpose(1, 0, 2)).reshape(128, 4096)
    wo = np.asarray(inp["w_out"][0], f)
    s["Wout"] = np.ascontiguousarray(wo.reshape(16, 128, 4, 512).transpose(2, 1, 0, 3)).reshape(4, 128, 8192)
    wpq = np.asarray(inp["w_pq"][0], f)
    s["Wpq"] = np.ascontiguousarray(wpq.reshape(16, 128, 16, 128).transpose(2, 1, 0, 3)).reshape(16, 128, 2048)
    sk = np.stack([np.asarray(inp["sub_k1"][0], f), np.asarray(inp["sub_k2"][0], f)], axis=1)
    s["subk"] = np.ascontiguousarray(sk.transpose(3, 0, 1, 2)).reshape(128, 2048)
    eu = np.asarray(inp["expert_u"][0], f)
    s["UT"] = np.ascontiguousarray(eu.reshape(128, 128, 16, 128).transpose(0, 3, 2, 1)).reshape(128, 128, 2048)
    ev = np.asarray(inp["expert_v"][0], f)
    evl = np.ascontiguousarray(ev.reshape(128, 128, 2, 1024).transpose(2, 0, 1, 3))
    s["EV"] = evl.reshape(256, 128, 1024)
    wg = np.asarray(inp["w_ple_gate"][0], f)
    s["Wg"] = np.ascontiguousarray(wg.reshape(16, 128, 4, 512).transpose(2, 1, 0, 3)).reshape(4, 128, 8192)
    wpp = np.asarray(inp["w_ple_proj"][0], f)
    s["Wpp"] = np.ascontiguousarray(wpp.reshape(2, 128, 2048).transpose(1, 0, 2)).reshape(128, 4096)
    return s


def kernel(**inputs):
    shared = _shared_layout(inputs)
    nc, _, _ = build()
    in_maps = []
    for b in range(8):
        m = dict(shared)
        m.update(_host_layout(inputs, b))
        in_maps.append(m)
    res = run_bass_kernel_spmd(nc, in_maps, core_ids=list(range(8)))
    return np.stack([r["out"] for r in res.results], axis=0).astype(np.float32)
```
